# Optimizing a Trainium2 kernel written in Bass

```python
import math
import jax, jax.numpy as jnp
from jax import lax
import numpy as np

D_MODEL = 1024
BATCH = 4
SEQ = 8192
DEPTH = 1

SSM_WIDTH = D_MODEL // 2
SSM_GROUP = 16
SSM_GROUPS = SSM_WIDTH // SSM_GROUP
SSM_STATE = 64
DT_MIN = 1e-3
DT_MAX = 1e-1
ATTN_HEADS = 8
HEAD_DIM = 64
ATTN_WIDTH = ATTN_HEADS * HEAD_DIM
MOBA_BLOCK = 256
MOBA_TOPK = 3
Q_CHUNK = 32
N_BRANCH = 2
IN_WIDTH = SSM_WIDTH + 3 * ATTN_WIDTH + N_BRANCH * D_MODEL
D_FF = -(-8 * D_MODEL // (3 * 256)) * 256
N_MOD = 6
NORM_EPS = 1e-6

kernel_name = "hybrid_s5_moba_gated_sandwich_adaln"


def rms_norm(x, g):
    xf = x.astype(jnp.float32)
    y = xf * lax.rsqrt(jnp.mean(xf * xf, axis=-1, keepdims=True) + NORM_EPS)
    return (y * g.astype(jnp.float32)).astype(x.dtype)


def alibi_slopes(n_heads):
    return jnp.asarray(2.0 ** (-8.0 * np.arange(1, n_heads + 1) / n_heads), jnp.float32)


def s5_ssm(u, a_re, a_im, log_dt, b_re, b_im, c_re, c_im, d_skip):
    bsz, s, _ = u.shape
    f32 = jnp.float32
    uf = u.astype(f32).reshape(bsz, s, SSM_GROUPS, SSM_GROUP)
    a_re = a_re.astype(f32); a_im = a_im.astype(f32)
    dt = jnp.exp(log_dt.astype(f32))[:, None]
    mag = jnp.exp(dt * a_re)
    ab_re = mag * jnp.cos(dt * a_im)
    ab_im = mag * jnp.sin(dt * a_im)
    den = a_re * a_re + a_im * a_im
    nr = ab_re - 1.0
    co_re = (nr * a_re + ab_im * a_im) / den
    co_im = (ab_im * a_re - nr * a_im) / den
    b_re = b_re.astype(f32); b_im = b_im.astype(f32)
    bb_re = co_re[..., None] * b_re - co_im[..., None] * b_im
    bb_im = co_re[..., None] * b_im + co_im[..., None] * b_re
    bu_re = jnp.einsum('bsgc,gpc->sbgp', uf, bb_re)
    bu_im = jnp.einsum('bsgc,gpc->sbgp', uf, bb_im)
    a_seq_re = jnp.broadcast_to(ab_re[None, None], (s, 1) + ab_re.shape)
    a_seq_im = jnp.broadcast_to(ab_im[None, None], (s, 1) + ab_im.shape)

    def combine(left, right):
        ar_l, ai_l, br_l, bi_l = left
        ar_r, ai_r, br_r, bi_r = right
        ar = ar_r * ar_l - ai_r * ai_l
        ai = ar_r * ai_l + ai_r * ar_l
        br = ar_r * br_l - ai_r * bi_l + br_r
        bi = ar_r * bi_l + ai_r * br_l + bi_r
        return (ar, ai, br, bi)

    _, _, x_re, x_im = lax.associative_scan(combine, (a_seq_re, a_seq_im, bu_re, bu_im), axis=0)
    y = (jnp.einsum('sbgp,gcp->bsgc', x_re, c_re.astype(f32))
         - jnp.einsum('sbgp,gcp->bsgc', x_im, c_im.astype(f32))
         + d_skip.astype(f32) * uf)
    return y.reshape(bsz, s, SSM_WIDTH).astype(u.dtype)


def moba_attention(q, k, v):
    out_dtype = q.dtype
    f32 = jnp.float32
    bsz, h, s, dh = q.shape
    n_blk = -(-s // MOBA_BLOCK)
    s_pad = n_blk * MOBA_BLOCK
    pad = ((0, 0), (0, 0), (0, s_pad - s), (0, 0))
    q = jnp.pad(q.astype(f32), pad)
    k = jnp.pad(k.astype(f32), pad)
    v = jnp.pad(v.astype(f32), pad)
    kb = k.reshape(bsz, h, n_blk, MOBA_BLOCK, dh)
    vb = v.reshape(bsz, h, n_blk, MOBA_BLOCK, dh)
    k_mean = jnp.mean(kb, axis=3)
    q_blk = jnp.arange(s_pad) // MOBA_BLOCK
    gate = jnp.einsum('bhtd,bhnd->bhtn', q, k_mean)
    fully_past = jnp.arange(n_blk)[None, :] < q_blk[:, None]
    gate = jnp.where(fully_past, gate, -jnp.inf)
    top_k = min(MOBA_TOPK, n_blk)
    _, sel = lax.top_k(gate, top_k)

    n_chunk = s_pad // Q_CHUNK

    def to_chunks(a):
        a = a.reshape((bsz, h, n_chunk, Q_CHUNK) + a.shape[3:])
        return jnp.moveaxis(a, 2, 0)

    slopes = alibi_slopes(h)[None, :, None, None]
    b_ix = jnp.arange(bsz)[:, None, None, None]
    h_ix = jnp.arange(h)[None, :, None, None]
    offs = jnp.arange(MOBA_BLOCK)
    scale = HEAD_DIM ** -0.5
    n_sel = top_k * MOBA_BLOCK

    def chunk_attn(args):
        qc, selc, ci = args
        t = ci * Q_CHUNK + jnp.arange(Q_CHUNK)
        own = t[0] // MOBA_BLOCK
        kg = kb[b_ix, h_ix, selc].reshape(bsz, h, Q_CHUNK, n_sel, dh)
        vg = vb[b_ix, h_ix, selc].reshape(bsz, h, Q_CHUNK, n_sel, dh)
        s_sel = (selc[..., None] * MOBA_BLOCK + offs).reshape(bsz, h, Q_CHUNK, n_sel)
        ok_sel = jnp.broadcast_to((selc < own)[..., None], selc.shape + (MOBA_BLOCK,)).reshape(bsz, h, Q_CHUNK, n_sel)
        l_sel = jnp.einsum('bhcd,bhckd->bhck', qc, kg) * scale - slopes * (t[:, None] - s_sel)
        l_sel = jnp.where(ok_sel, l_sel, -jnp.inf)
        ko = lax.dynamic_slice_in_dim(k, own * MOBA_BLOCK, MOBA_BLOCK, axis=2)
        vo = lax.dynamic_slice_in_dim(v, own * MOBA_BLOCK, MOBA_BLOCK, axis=2)
        s_own = own * MOBA_BLOCK + offs
        dist_own = t[:, None] - s_own[None, :]
        l_own = jnp.einsum('bhcd,bhkd->bhck', qc, ko) * scale - slopes * dist_own
        l_own = jnp.where(dist_own >= 0, l_own, -jnp.inf)
        p = jax.nn.softmax(jnp.concatenate([l_sel, l_own], axis=-1), axis=-1)
        o = (jnp.einsum('bhck,bhckd->bhcd', p[..., :n_sel], vg)
             + jnp.einsum('bhck,bhkd->bhcd', p[..., n_sel:], vo))
        return o

    out = lax.map(chunk_attn, (to_chunks(q), to_chunks(sel), jnp.arange(n_chunk)))
    out = jnp.moveaxis(out, 0, 2).reshape(bsz, h, s_pad, dh)[:, :, :s]
    return out.astype(out_dtype)


def parallel_mixer(hn, w_in, a_re, a_im, log_dt, b_re, b_im, c_re, c_im, d_skip,
                   w_glu_a, w_glu_b, w_attn_out, w_out):
    bsz, s, _ = hn.shape
    proj = hn @ w_in
    cuts = [SSM_WIDTH, SSM_WIDTH + ATTN_WIDTH, SSM_WIDTH + 2 * ATTN_WIDTH, SSM_WIDTH + 3 * ATTN_WIDTH]
    u, q, k, v, g = jnp.split(proj, cuts, axis=-1)
    z = jax.nn.gelu(s5_ssm(u, a_re, a_im, log_dt, b_re, b_im, c_re, c_im, d_skip))
    y_a = (z @ w_glu_a) * jax.nn.sigmoid(z @ w_glu_b)
    to_heads = lambda t: t.reshape(bsz, s, ATTN_HEADS, HEAD_DIM).transpose(0, 2, 1, 3)
    o = moba_attention(to_heads(q), to_heads(k), to_heads(v))
    y_b = o.transpose(0, 2, 1, 3).reshape(bsz, s, ATTN_WIDTH) @ w_attn_out
    g_a, g_b = jnp.split(jax.nn.sigmoid(g), N_BRANCH, axis=-1)
    return (g_a * y_a + g_b * y_b) @ w_out


def swiglu_ffn(hn, w_gate, w_up, w_down):
    return (jax.nn.silu(hn @ w_gate) * (hn @ w_up)) @ w_down


def setup_inputs(seed: int = 0) -> dict:
    key = jax.random.key(seed)
    ks = jax.random.split(key, 24)
    f32 = jnp.float32
    nrm = lambda k, shape, s: jax.random.normal(k, shape, f32) * s
    L, G, P, C = DEPTH, SSM_GROUPS, SSM_STATE, SSM_GROUP
    n_idx = jnp.arange(P, dtype=f32)
    return {
        "x": nrm(ks[0], (BATCH, SEQ, D_MODEL), 1.0),
        "c": nrm(ks[1], (BATCH, D_MODEL), 1.0),
        "w_ada": nrm(ks[2], (L, D_MODEL, N_MOD * D_MODEL), 0.2 * D_MODEL ** -0.5),
        "b_ada": nrm(ks[3], (L, N_MOD * D_MODEL), 0.01),
        "g_pre_mix": 1.0 + nrm(ks[4], (L, D_MODEL), 0.02),
        "g_post_mix": 1.0 + nrm(ks[5], (L, D_MODEL), 0.02),
        "w_in": nrm(ks[6], (L, D_MODEL, IN_WIDTH), D_MODEL ** -0.5),
        "ssm_a_re": -0.5 * jnp.exp(nrm(ks[7], (L, G, P), 0.02)),
        "ssm_a_im": math.pi * n_idx + nrm(ks[8], (L, G, P), 0.01),
        "ssm_log_dt": jax.random.uniform(ks[9], (L, G), f32, math.log(DT_MIN), math.log(DT_MAX)),
        "ssm_b_re": nrm(ks[10], (L, G, P, C), (2 * C) ** -0.5),
        "ssm_b_im": nrm(ks[11], (L, G, P, C), (2 * C) ** -0.5),
        "ssm_c_re": nrm(ks[12], (L, G, C, P), P ** -0.5),
        "ssm_c_im": nrm(ks[13], (L, G, C, P), P ** -0.5),
        "ssm_d": nrm(ks[14], (L, G, C), 1.0),
        "w_glu_a": nrm(ks[15], (L, SSM_WIDTH, D_MODEL), SSM_WIDTH ** -0.5),
        "w_glu_b": nrm(ks[16], (L, SSM_WIDTH, D_MODEL), SSM_WIDTH ** -0.5),
        "w_attn_out": nrm(ks[17], (L, ATTN_WIDTH, D_MODEL), ATTN_WIDTH ** -0.5),
        "w_out": nrm(ks[18], (L, D_MODEL, D_MODEL), D_MODEL ** -0.5),
        "g_pre_ffn": 1.0 + nrm(ks[19], (L, D_MODEL), 0.02),
        "g_post_ffn": 1.0 + nrm(ks[20], (L, D_MODEL), 0.02),
        "w_ff_gate": nrm(ks[21], (L, D_MODEL, D_FF), D_MODEL ** -0.5),
        "w_ff_up": nrm(ks[22], (L, D_MODEL, D_FF), D_MODEL ** -0.5),
        "w_ff_down": nrm(ks[23], (L, D_FF, D_MODEL), D_FF ** -0.5),
    }


def reference(x, c, w_ada, b_ada, g_pre_mix, g_post_mix, w_in, ssm_a_re, ssm_a_im, ssm_log_dt,
              ssm_b_re, ssm_b_im, ssm_c_re, ssm_c_im, ssm_d, w_glu_a, w_glu_b, w_attn_out, w_out,
              g_pre_ffn, g_post_ffn, w_ff_gate, w_ff_up, w_ff_down):
    c_act = jax.nn.silu(c)
    for l in range(DEPTH):
        mod = c_act @ w_ada[l] + b_ada[l]
        sh_m, sc_m, gt_m, sh_f, sc_f, gt_f = [m[:, None, :] for m in jnp.split(mod, N_MOD, axis=-1)]
        hn = rms_norm(x, g_pre_mix[l]) * (1.0 + sc_m) + sh_m
        y = parallel_mixer(hn, w_in[l], ssm_a_re[l], ssm_a_im[l], ssm_log_dt[l], ssm_b_re[l], ssm_b_im[l],
                           ssm_c_re[l], ssm_c_im[l], ssm_d[l], w_glu_a[l], w_glu_b[l], w_attn_out[l], w_out[l])
        x = x + gt_m * rms_norm(y, g_post_mix[l])
        hn = rms_norm(x, g_pre_ffn[l]) * (1.0 + sc_f) + sh_f
        y = swiglu_ffn(hn, w_ff_gate[l], w_ff_up[l], w_ff_down[l])
        x = x + gt_f * rms_norm(y, g_post_ffn[l])
    return x
```

```python
import numpy as np
from contextlib import ExitStack
import concourse.bass as bass
import concourse.mybir as mybir
from concourse.bass_utils import run_bass_kernel_spmd

F32 = mybir.dt.float32
BF16 = mybir.dt.bfloat16
AF = mybir.ActivationFunctionType
ALU = mybir.AluOpType
AX = mybir.AxisListType

D = 1024
TOK = 4096
TT = 1024
NT = TOK // TT
DFF = 2816
NF = DFF // 128
EPS = 1e-6
NEG = -30000.0


class DSem:
    def __init__(self, sem):
        self.sem = sem
        self.cnt = 0


class Buf:
    def __init__(self, name):
        self.name = name
        self.w = {}
        self.r = {}
        self.dsem = None


class Sched:
    def __init__(self, nc, es):
        self.nc = nc
        self.es = es
        self.E = dict(pe=nc.tensor, act=nc.scalar, dve=nc.vector, pool=nc.gpsimd, sp=nc.sync)
        self.sem = {k: es.enter_context(nc.semaphore("s_" + k)) for k in ("pe", "act", "dve", "pool")}
        self.cnt = {k: 0 for k in self.sem}
        self.seen = {k: {} for k in self.E}
        self.dsems = []
        self.free_dsems = []

    def _semof(self, key):
        return self.sem[key] if isinstance(key, str) else key.sem

    def _cntof(self, key):
        return self.cnt[key] if isinstance(key, str) else key.cnt

    def get_dsem(self, buf):
        if buf.dsem is None:
            if self.free_dsems:
                buf.dsem = self.free_dsems.pop()
            else:
                buf.dsem = DSem(self.es.enter_context(self.nc.semaphore("d%d" % len(self.dsems))))
                self.dsems.append(buf.dsem)
        return buf.dsem

    def release(self, bufs):
        for b in bufs:
            if b.dsem is not None:
                self.free_dsems.append(b.dsem)
                b.dsem = None

    def _waits(self, eng, r, w):
        need = {}
        for b in r:
            for k, v in b.w.items():
                if need.get(k, 0) < v:
                    need[k] = v
        for b in w:
            for dd in (b.w, b.r):
                for k, v in dd.items():
                    if need.get(k, 0) < v:
                        need[k] = v
        for k, v in need.items():
            if eng == "pe" and k == "pe":
                continue
            if self.seen[eng].get(k, 0) >= v:
                continue
            self.seen[eng][k] = v
            self.E[eng].wait_ge(self._semof(k), v)

    def op(self, eng, fns, r=(), w=()):
        if callable(fns):
            fns = [fns]
        self._waits(eng, r, w)
        ins = None
        for f in fns:
            ins = f(self.E[eng])
        self.cnt[eng] += 1
        ins.then_inc(self.sem[eng], 1)
        c = self.cnt[eng]
        for b in r:
            b.r[eng] = c
        for b in w:
            b.w[eng] = c
            b.r = {}

    def dma(self, eng, out, in_, sb, load, extra_r=(), extra_w=(), **kw):
        ds = self.get_dsem(sb)
        r = list(extra_r) + ([] if load else [sb])
        w = list(extra_w) + ([sb] if load else [])
        self._waits(eng, r, w)
        ins = self.E[eng].dma_start(out=out, in_=in_, **kw)
        ds.cnt += 16
        ins.then_inc(ds.sem, 16)
        for b in r:
            b.r[ds] = ds.cnt
        for b in w:
            b.w[ds] = ds.cnt
            b.r = {}

    def barrier(self):
        for eng in self.E:
            for k in self.sem:
                if k == eng:
                    continue
                v = self.cnt[k]
                if v > 0 and self.seen[eng].get(k, 0) < v:
                    self.seen[eng][k] = v
                    self.E[eng].wait_ge(self.sem[k], v)
            for ds in self.dsems:
                if ds.cnt > 0 and self.seen[eng].get(ds, 0) < ds.cnt:
                    self.seen[eng][ds] = ds.cnt
                    self.E[eng].wait_ge(ds.sem, ds.cnt)


def build_program(debug=False, upto=99):
    nc = bass.Bass("TRN2", target_bir_lowering=False)
    dram = {}

    def din(name, shape, dt=F32):
        dram[name] = nc.dram_tensor(name, list(shape), dt, kind="ExternalInput").ap()
        return dram[name]

    def dscr(name, shape, dt):
        return nc.dram_tensor(name, list(shape), dt, kind="Internal").ap()

    xo = din("xo", [TOK, D])
    xp = din("xp", [TOK, D])
    flag = din("flag", [128, 1])
    c_col = din("c_col", [128, 8])
    w_ada = din("w_ada", [D, 6 * D])
    b_ada = din("b_ada", [1, 6 * D])
    g_pre_mix_c = din("g_pre_mix_c", [128, 8])
    g_pre_ffn_c = din("g_pre_ffn_c", [128, 8])
    g_post_mix_r = din("g_post_mix_r", [1, D])
    g_post_ffn_r = din("g_post_ffn_r", [1, D])
    w_in = din("w_in", [D, 4096])
    s_are = din("s_are", [128, 32])
    s_aim = din("s_aim", [128, 32])
    s_ldt = din("s_ldt", [128, 32])
    s_bre = din("s_bre", [128, 32, 16])
    s_bim = din("s_bim", [128, 32, 16])
    s_cre = din("s_cre", [128, 32, 16])
    s_cim = din("s_cim", [128, 32, 16])
    s_dcol = din("s_dcol", [128, 32])
    w_glu_a = din("w_glu_a", [512, D])
    w_glu_b = din("w_glu_b", [512, D])
    w_attn_out = din("w_attn_out", [512, D])
    w_out = din("w_out", [D, D])
    w_ff_gate = din("w_ff_gate", [D, DFF])
    w_ff_up = din("w_ff_up", [D, DFF])
    w_ff_down = din("w_ff_down", [DFF, D])
    out = nc.dram_tensor("out", [TOK, D], F32, kind="ExternalOutput").ap()
    dbg = {}

    def dbg_out(name, shape, dt=F32):
        dbg[name] = nc.dram_tensor("dbg_" + name, list(shape), dt, kind="ExternalOutput").ap()
        return dbg[name]

    scr_hn = dscr("scr_hn", [NT, 128, 8, TT], BF16)
    scr_kt = dscr("scr_kt", [4, 128, 2 * TOK], BF16)
    scr_v = dscr("scr_v", [2 * NT, 8, 128, 512], BF16)
    scr_u = dscr("scr_u", [2 * NT, 128, 32, 8, 16], BF16)
    scr_q = dscr("scr_q", [4, 128, TOK], BF16)
    scr_g = dscr("scr_g", [16, 128, TOK], BF16)
    scr_z = dscr("scr_z", [4, 128, TOK], BF16)
    scr_o = dscr("scr_o", [4, 128, TOK], BF16)
    scr_W1t = dscr("scr_W1t", [128, 32, 128], BF16)
    scr_Ktoep = dscr("scr_Ktoep", [128, 32, 128], BF16)
    scr_Ctab = dscr("scr_Ctab", [128, 32, 128], BF16)
    scr_cosT = dscr("scr_cosT", [128, 32, 64], F32)
    scr_sinT = dscr("scr_sinT", [128, 32, 64], F32)
    scr_rho = dscr("scr_rho", [128, 32], F32)
    scr_Jt = dscr("scr_Jt", [128, 128], F32)
    scr_wga = dscr("scr_wga", [128, 4, D], BF16)
    scr_wgb = dscr("scr_wgb", [128, 4, D], BF16)
    scr_wao = dscr("scr_wao", [128, 4, D], BF16)
    scr_wo = dscr("scr_wo", [128, 8, D], BF16)
    scr_wg = dscr("scr_wg", [128, 8, DFF], BF16)
    scr_wu = dscr("scr_wu", [128, 8, DFF], BF16)
    scr_wd = dscr("scr_wd", [128, NF, D], BF16)

    with ExitStack() as es:
        S = Sched(nc, es)

        def sbt(stack, name, shape, dt):
            return stack.enter_context(nc.sbuf_tensor(name, list(shape), dt))

        def pst(stack, name, shape, dt):
            return stack.enter_context(nc.psum_tensor(name, list(shape), dt))

        class Ring:
            def __init__(self, tiles, name):
                self.t = [(t, Buf("%s%d" % (name, i))) for i, t in enumerate(tiles)]
                self.i = 0

            def next(self):
                x = self.t[self.i % len(self.t)]
                self.i += 1
                return x

        ident_f = sbt(es, "ident_f", [128, 128], F32)
        ident_b = sbt(es, "ident_b", [128, 128], BF16)
        ones_f = sbt(es, "ones_f", [128, 128], F32)
        gmod_m = sbt(es, "gmod_m", [128, 8], F32)
        sh_m = sbt(es, "sh_m", [128, 8], F32)
        gmod_f = sbt(es, "gmod_f", [128, 8], F32)
        sh_f = sbt(es, "sh_f", [128, 8], F32)
        bc_m = sbt(es, "bc_m", [128, D], F32)
        bc_f = sbt(es, "bc_f", [128, D], F32)
        flag_sb = sbt(es, "flag_sb", [128, 1], F32)
        kmean = sbt(es, "kmean", [128, 4, 32], BF16)
        B_const = Buf("const")
        B_mod = Buf("mod")
        B_kmean = Buf("kmean")

        S.op("pool", lambda e: e.memset(ident_f[:], 1.0), w=[B_const])
        S.op("pool", lambda e: e.affine_select(out=ident_f[:], in_=ident_f[:], pattern=[[-1, 128]],
                                                compare_op=ALU.is_equal, fill=0.0, base=0, channel_multiplier=1),
             w=[B_const])
        S.op("pool", lambda e: e.memset(ones_f[:], 1.0), w=[B_const])
        S.op("dve", lambda e: e.tensor_copy(out=ident_b[:], in_=ident_f[:]), r=[B_const], w=[B_const])
        S.dma("sp", flag_sb[:], flag, B_const, True)

        with ExitStack() as ps:
            cc = sbt(ps, "cc", [128, 8], F32)
            modrow = sbt(ps, "modrow", [1, 6 * D], F32)
            cact = sbt(ps, "cact", [128, 8], F32)
            wab = [sbt(ps, "wab%d" % i, [128, 8, 256], F32) for i in range(2)]
            gpm = sbt(ps, "gpm", [128, 8], F32)
            gpf = sbt(ps, "gpf", [128, 8], F32)
            grow = sbt(ps, "grow", [1, 2 * D], F32)
            rprod = sbt(ps, "rprod", [1, 2 * D], F32)
            pr = [pst(ps, "p0r%d" % i, [128, 512], F32) for i in range(2)]
            pc = pst(ps, "p0c", [128, 512], F32)
            B_cc, B_cact, B_brow, B_g = Buf("cc"), Buf("cact"), Buf("brow"), Buf("g")
            B_wab = [Buf("wab0"), Buf("wab1")]
            B_pr = [Buf("pr0"), Buf("pr1")]
            B_pc = Buf("pc")
            B_rp = Buf("rprod")
            S.dma("sp", cc[:], c_col, B_cc, True)
            S.dma("sp", modrow[:], b_ada, B_mod, True)
            S.dma("sp", gpm[:], g_pre_mix_c, B_g, True)
            S.dma("sp", gpf[:], g_pre_ffn_c, B_g, True)
            S.dma("sp", grow[:, 0:D], g_post_mix_r, B_g, True)
            S.dma("sp", grow[:, D:2 * D], g_post_ffn_r, B_g, True)
            S.op("act", lambda e: e.activation(out=cact[:], in_=cc[:], func=AF.Silu), r=[B_cc], w=[B_cact])
            wada_v = w_ada.rearrange("(k p) n -> p k n", p=128)
            W1t = sbt(ps, "W1t0", [128, 32, 128], BF16)
            Ktoep = sbt(ps, "Ktoep0", [128, 32, 128], BF16)
            Ctab = sbt(ps, "Ctab0", [128, 32, 128], BF16)
            cosT = sbt(ps, "cosT0", [128, 32, 64], F32)
            sinT = sbt(ps, "sinT0", [128, 32, 64], F32)
            rho = sbt(ps, "rho0", [128, 32], F32)
            Jt = sbt(ps, "Jt0", [128, 128], F32)
            B_tab = Buf("tab0")
            pss = Ring([pst(ps, "pss0_%d" % i, [128, 512], F32) for i in range(3)], "pss0")

            def T(eng, fn):
                S.op(eng, fn, r=[B_tab, B_const], w=[B_tab])

            def bc3(ap2, n):
                return ap2.unsqueeze(2).to_broadcast([ap2.shape[0], ap2.shape[1], n])

            def tab_gen():
                f32t = lambda nm, shp: sbt(ps, nm, shp, F32)
                are, aim, ldt = f32t("are", [128, 32]), f32t("aim", [128, 32]), f32t("ldt", [128, 32])
                bre, bim = f32t("bre", [128, 32, 16]), f32t("bim", [128, 32, 16])
                cre, cim = f32t("cre", [128, 32, 16]), f32t("cim", [128, 32, 16])
                dcol = f32t("dcol", [128, 32])
                for t_, src in ((are, s_are), (aim, s_aim), (ldt, s_ldt), (bre, s_bre), (bim, s_bim), (cre, s_cre),
                                (cim, s_cim), (dcol, s_dcol)):
                    S.dma("sp", t_[:], src, B_tab, True)
                dtt, xr, xi, mag = f32t("dtt", [128, 32]), f32t("xr", [128, 32]), f32t("xi", [128, 32]), f32t("mag", [128, 32])
                ys, sn, cs = f32t("ys", [128, 32]), f32t("sn", [128, 32]), f32t("cs", [128, 32])
                t1, t2, t3 = f32t("t1", [128, 32]), f32t("t2", [128, 32]), f32t("t3", [128, 32])
                cor, coi = f32t("cor", [128, 32]), f32t("coi", [128, 32])
                PWr, PWi = f32t("PWr", [128, 9, 32]), f32t("PWi", [128, 9, 32])
                NPr, NPi = f32t("NPr", [128, 8, 32]), f32t("NPi", [128, 8, 32])
                bbr, bbi = f32t("bbr", [128, 32, 16]), f32t("bbi", [128, 32, 16])
                u1, u2 = f32t("u1", [128, 32, 16]), f32t("u2", [128, 32, 16])
                BB = f32t("BB", [128, 32, 8, 16])
                WW = f32t("WW", [128, 32, 8, 16])
                CC = f32t("CC", [128, 32, 9, 16])
                maskLT = f32t("maskLT", [128, 8, 16])
                pi = float(np.pi)
                T("act", lambda e: e.activation(out=dtt[:], in_=ldt[:], func=AF.Exp))
                T("dve", lambda e: e.tensor_tensor(out=xr[:], in0=dtt[:], in1=are[:], op=ALU.mult))
                T("dve", lambda e: e.tensor_tensor(out=xi[:], in0=dtt[:], in1=aim[:], op=ALU.mult))
                T("act", lambda e: e.activation(out=mag[:], in_=xr[:], func=AF.Exp))
                T("act", lambda e: e.activation(out=rho[:], in_=xr[:], func=AF.Exp, scale=8.0))
                MAGIC = 12582912.0

                def sin_of(dst, src_ap, shift):
                    T("dve", lambda e: e.tensor_scalar(out=t1[:], in0=src_ap, scalar1=shift, scalar2=None, op0=ALU.add))
                    T("dve", lambda e: e.tensor_scalar(out=t2[:], in0=t1[:], scalar1=1.0 / (2 * pi), scalar2=MAGIC,
                                                       op0=ALU.mult, op1=ALU.add))
                    T("dve", lambda e: e.tensor_scalar(out=t2[:], in0=t2[:], scalar1=-MAGIC, scalar2=None, op0=ALU.add))
                    T("dve", lambda e: e.scalar_tensor_tensor(out=ys[:], in0=t2[:], scalar=-2 * pi, in1=t1[:],
                                                              op0=ALU.mult, op1=ALU.add))
                    T("dve", lambda e: e.tensor_scalar(out=ys[:], in0=ys[:], scalar1=-3.14159, scalar2=3.14159,
                                                       op0=ALU.max, op1=ALU.min))
                    T("act", lambda e: e.activation(out=dst, in_=ys[:], func=AF.Sin))

                sin_of(sn[:], xi[:], 0.0)
                sin_of(cs[:], xi[:], 0.5 * pi)
                yield
                T("dve", lambda e: e.memset(PWr[:, 0, :], 1.0))
                T("dve", lambda e: e.memset(PWi[:, 0, :], 0.0))
                T("dve", lambda e: e.memset(NPr[:, 0, :], 1.0))
                T("dve", lambda e: e.memset(NPi[:, 0, :], 0.0))
                T("dve", lambda e: e.tensor_tensor(out=PWr[:, 1, :], in0=mag[:], in1=cs[:], op=ALU.mult))
                T("dve", lambda e: e.tensor_tensor(out=PWi[:, 1, :], in0=mag[:], in1=sn[:], op=ALU.mult))
                abr, abi = PWr[:, 1, :], PWi[:, 1, :]
                T("dve", lambda e: e.tensor_tensor(out=t1[:], in0=are[:], in1=are[:], op=ALU.mult))
                T("dve", lambda e: e.tensor_tensor(out=t2[:], in0=aim[:], in1=aim[:], op=ALU.mult))
                T("dve", lambda e: e.tensor_tensor(out=t1[:], in0=t1[:], in1=t2[:], op=ALU.add))
                T("dve", lambda e: e.reciprocal(out=t1[:], in_=t1[:]))
                T("dve", lambda e: e.tensor_scalar(out=t2[:], in0=abr, scalar1=-1.0, scalar2=None, op0=ALU.add))
                T("dve", lambda e: e.tensor_tensor(out=cor[:], in0=t2[:], in1=are[:], op=ALU.mult))
                T("dve", lambda e: e.tensor_tensor(out=t3[:], in0=abi, in1=aim[:], op=ALU.mult))
                T("dve", lambda e: e.tensor_tensor(out=cor[:], in0=cor[:], in1=t3[:], op=ALU.add))
                T("dve", lambda e: e.tensor_tensor(out=cor[:], in0=cor[:], in1=t1[:], op=ALU.mult))
                T("dve", lambda e: e.tensor_tensor(out=coi[:], in0=abi, in1=are[:], op=ALU.mult))
                T("dve", lambda e: e.tensor_tensor(out=t3[:], in0=t2[:], in1=aim[:], op=ALU.mult))
                T("dve", lambda e: e.tensor_tensor(out=coi[:], in0=coi[:], in1=t3[:], op=ALU.subtract))
                T("dve", lambda e: e.tensor_tensor(out=coi[:], in0=coi[:], in1=t1[:], op=ALU.mult))
                T("dve", lambda e: e.tensor_tensor(out=bbr[:], in0=bre[:], in1=bc3(cor[:], 16), op=ALU.mult))
                T("dve", lambda e: e.tensor_tensor(out=u1[:], in0=bim[:], in1=bc3(coi[:], 16), op=ALU.mult))
                T("dve", lambda e: e.tensor_tensor(out=bbr[:], in0=bbr[:], in1=u1[:], op=ALU.subtract))
                T("dve", lambda e: e.tensor_tensor(out=bbi[:], in0=bim[:], in1=bc3(cor[:], 16), op=ALU.mult))
                T("dve", lambda e: e.tensor_tensor(out=u1[:], in0=bre[:], in1=bc3(coi[:], 16), op=ALU.mult))
                T("dve", lambda e: e.tensor_tensor(out=bbi[:], in0=bbi[:], in1=u1[:], op=ALU.add))
                T("dve", lambda e: e.tensor_tensor(out=t1[:], in0=abr, in1=abr, op=ALU.mult))
                T("dve", lambda e: e.tensor_tensor(out=t2[:], in0=abi, in1=abi, op=ALU.mult))
                T("dve", lambda e: e.tensor_tensor(out=t1[:], in0=t1[:], in1=t2[:], op=ALU.add))
                T("dve", lambda e: e.reciprocal(out=t1[:], in_=t1[:]))
                T("dve", lambda e: e.tensor_tensor(out=NPr[:, 1, :], in0=abr, in1=t1[:], op=ALU.mult))
                T("dve", lambda e: e.scalar_tensor_tensor(out=NPi[:, 1, :], in0=abi, scalar=-1.0, in1=t1[:], op0=ALU.mult, op1=ALU.mult))

                def cmul(orr, oii, ar_, ai_, br_, bi_, tA, tB):
                    T("dve", lambda e: e.tensor_tensor(out=tA, in0=ar_, in1=br_, op=ALU.mult))
                    T("dve", lambda e: e.tensor_tensor(out=tB, in0=ai_, in1=bi_, op=ALU.mult))
                    T("dve", lambda e: e.tensor_tensor(out=tA, in0=tA, in1=tB, op=ALU.subtract))
                    T("dve", lambda e: e.tensor_tensor(out=tB, in0=ar_, in1=bi_, op=ALU.mult))
                    T("dve", lambda e: e.tensor_tensor(out=oii, in0=ai_, in1=br_, op=ALU.mult))
                    T("dve", lambda e: e.tensor_tensor(out=oii, in0=oii, in1=tB, op=ALU.add))
                    T("dve", lambda e: e.tensor_copy(out=orr, in_=tA))

                for k in range(2, 9):
                    cmul(PWr[:, k, :], PWi[:, k, :], PWr[:, k - 1, :], PWi[:, k - 1, :], abr, abi, t1[:], t2[:])
                    yield
                for k in range(2, 8):
                    cmul(NPr[:, k, :], NPi[:, k, :], NPr[:, k - 1, :], NPi[:, k - 1, :], NPr[:, 1, :], NPi[:, 1, :], t1[:], t2[:])
                    yield
                lo, hi = slice(0, 64), slice(64, 128)
                for s_ in range(8):
                    for (dst, pr_, pi_) in ((BB, NPr[:, s_, :], NPi[:, s_, :]), (WW, PWr[:, 7 - s_, :], PWi[:, 7 - s_, :])):
                        T("dve", lambda e: e.tensor_tensor(out=u1[lo], in0=bbr[lo], in1=bc3(pr_[lo], 16), op=ALU.mult))
                        T("dve", lambda e: e.tensor_tensor(out=u2[lo], in0=bbi[lo], in1=bc3(pi_[lo], 16), op=ALU.mult))
                        T("dve", lambda e: e.tensor_tensor(out=dst[lo, :, s_, :], in0=u1[lo], in1=u2[lo], op=ALU.subtract))
                        T("dve", lambda e: e.tensor_tensor(out=u1[hi], in0=bbi[hi], in1=bc3(pr_[hi], 16), op=ALU.mult))
                        T("dve", lambda e: e.tensor_tensor(out=u2[hi], in0=bbr[hi], in1=bc3(pi_[hi], 16), op=ALU.mult))
                        T("dve", lambda e: e.tensor_tensor(out=dst[hi, :, s_, :], in0=u1[hi], in1=u2[hi], op=ALU.add))
                        yield
                for k in range(9):
                    pr_, pi_ = PWr[:, k, :], PWi[:, k, :]
                    T("dve", lambda e: e.tensor_tensor(out=u1[lo], in0=cre[lo], in1=bc3(pr_[lo], 16), op=ALU.mult))
                    T("dve", lambda e: e.tensor_tensor(out=u2[lo], in0=cim[lo], in1=bc3(pi_[lo], 16), op=ALU.mult))
                    T("dve", lambda e: e.tensor_tensor(out=CC[lo, :, k, :], in0=u1[lo], in1=u2[lo], op=ALU.subtract))
                    T("dve", lambda e: e.tensor_tensor(out=u1[hi], in0=cre[hi], in1=bc3(pi_[hi], 16), op=ALU.mult))
                    T("dve", lambda e: e.tensor_tensor(out=u2[hi], in0=cim[hi], in1=bc3(pr_[hi], 16), op=ALU.mult))
                    T("dve", lambda e: e.scalar_tensor_tensor(out=CC[hi, :, k, :], in0=u1[hi], scalar=-1.0, in1=u2[hi],
                                                              op0=ALU.mult, op1=ALU.subtract))
                    yield
                T("dve", lambda e: e.tensor_copy(out=Ctab[:].rearrange("p g (k c) -> p g k c", k=8), in_=CC[:, :, 1:9, :]))
                T("pool", lambda e: e.memset(maskLT[:], 1.0))
                T("pool", lambda e: e.affine_select(out=maskLT[:], in_=maskLT[:], pattern=[[16, 8], [0, 16]],
                                                    compare_op=ALU.is_ge, fill=0.0, base=15, channel_multiplier=-1))
                jt2 = f32t("jt2", [128, 128])
                T("pool", lambda e: e.memset(Jt[:], 1.0))
                T("pool", lambda e: e.affine_select(out=Jt[:], in_=Jt[:], pattern=[[1, 128]], compare_op=ALU.is_equal,
                                                    fill=0.0, base=-64, channel_multiplier=-1))
                T("pool", lambda e: e.memset(jt2[:], 1.0))
                T("pool", lambda e: e.affine_select(out=jt2[:], in_=jt2[:], pattern=[[1, 128]], compare_op=ALU.is_equal,
                                                    fill=0.0, base=64, channel_multiplier=-1))
                T("dve", lambda e: e.tensor_tensor(out=Jt[:], in0=Jt[:], in1=jt2[:], op=ALU.subtract))
                ktmp = f32t("ktmp", [128, 128])
                for g in range(32):
                    pk, B_pk = pss.next()
                    S.op("pe", lambda e: e.matmul(pk[:, 0:128], lhsT=BB[:, g, :, :].rearrange("p a b -> p (a b)"), rhs=CC[:, g, 0:8, :].rearrange("p a b -> p (a b)"), start=True, stop=True),
                         r=[B_tab], w=[B_pk])
                    S.op("dve", lambda e: e.tensor_tensor(out=ktmp[:], in0=pk[:, 0:128], in1=maskLT[:].rearrange("p a b -> p (a b)"),
                                                          op=ALU.mult), r=[B_pk, B_tab], w=[B_tab])
                    T("dve", lambda e: e.scalar_tensor_tensor(out=Ktoep[:, g, :], in0=ident_f[:], scalar=dcol[:, g:g + 1],
                                                              in1=ktmp[:], op0=ALU.mult, op1=ALU.add))
                    pw, B_pw = pss.next()
                    S.op("pe", lambda e: e.transpose(out=pw[:, 0:128], in_=WW[:, g, :, :].rearrange("p a b -> p (a b)"), identity=ident_f[:]),
                         r=[B_tab, B_const], w=[B_pw])
                    S.op("act", lambda e: e.activation(out=W1t[:, g, :], in_=pw[:, 0:128], func=AF.Copy), r=[B_pw], w=[B_tab])
                    yield
                T("dve", lambda e: e.reciprocal(out=t3[:], in_=rho[:]))
                T("dve", lambda e: e.tensor_tensor(out=cosT[:, :, 0], in0=PWr[:, 8, :], in1=t3[:], op=ALU.mult))
                T("dve", lambda e: e.tensor_tensor(out=sinT[:, :, 0], in0=PWi[:, 8, :], in1=t3[:], op=ALU.mult))
                e1, e2 = f32t("e1", [128, 32, 32]), f32t("e2", [128, 32, 32])
                m = 1
                while m < 64:
                    br_ = cosT[:, :, m - 1:m].to_broadcast([128, 32, m])
                    bi_ = sinT[:, :, m - 1:m].to_broadcast([128, 32, m])
                    cmul(cosT[:, :, m:2 * m], sinT[:, :, m:2 * m], cosT[:, :, 0:m], sinT[:, :, 0:m], br_, bi_,
                         e1[:, :, 0:m], e2[:, :, 0:m])
                    m *= 2
                    yield

            tgen = tab_gen()
            for nb in range(24):
                i = nb % 2
                S.dma("sp", wab[i][:], wada_v[:, :, nb * 256:(nb + 1) * 256], B_wab[i], True)
                S.op("pe", [(lambda e, k=k: e.matmul(pr[i][0:1, 0:256], lhsT=cact[:, k:k + 1], rhs=wab[i][:, k, :],
                                                     start=(k == 0), stop=(k == 7))) for k in range(8)],
                     r=[B_cact, B_wab[i]], w=[B_pr[i]])
                S.op("dve", lambda e: e.tensor_tensor(out=modrow[0:1, nb * 256:(nb + 1) * 256], in0=pr[i][0:1, 0:256],
                                                      in1=modrow[0:1, nb * 256:(nb + 1) * 256], op=ALU.add),
                     r=[B_pr[i]], w=[B_mod])
                for _ in range(4):
                    next(tgen, None)
            for _ in tgen:
                pass
            for t_, dst_ in ((W1t, scr_W1t), (Ktoep, scr_Ktoep), (Ctab, scr_Ctab), (cosT, scr_cosT), (sinT, scr_sinT),
                             (rho, scr_rho), (Jt, scr_Jt)):
                S.dma("pool", dst_, t_[:], B_tab, False)
            cols = [(0, 0), (1, 8), (3, 16), (4, 24)]
            S.op("pe", [(lambda e, j=j, o=o, k=k: e.matmul(pc[:, o + k:o + k + 1],
                                                           lhsT=modrow[0:1, j * D + k * 128:j * D + (k + 1) * 128],
                                                           rhs=ones_f[0:1, 0:1], start=True, stop=True))
                        for (j, o) in cols for k in range(8)], r=[B_mod, B_const], w=[B_pc])
            S.op("dve", lambda e: e.tensor_copy(out=sh_m[:], in_=pc[:, 0:8]), r=[B_pc], w=[B_mod])
            S.op("dve", lambda e: e.tensor_copy(out=sh_f[:], in_=pc[:, 16:24]), r=[B_pc], w=[B_mod])
            S.op("dve", lambda e: e.scalar_tensor_tensor(out=gmod_m[:], in0=pc[:, 8:16], scalar=1.0, in1=gpm[:],
                                                         op0=ALU.add, op1=ALU.mult), r=[B_pc, B_g], w=[B_mod])
            S.op("dve", lambda e: e.scalar_tensor_tensor(out=gmod_f[:], in0=pc[:, 24:32], scalar=1.0, in1=gpf[:],
                                                         op0=ALU.add, op1=ALU.mult), r=[B_pc, B_g], w=[B_mod])
            S.op("dve", lambda e: e.tensor_tensor(out=rprod[0:1, 0:D], in0=modrow[0:1, 2 * D:3 * D],
                                                  in1=grow[0:1, 0:D], op=ALU.mult), r=[B_mod, B_g], w=[B_rp])
            S.op("dve", lambda e: e.tensor_tensor(out=rprod[0:1, D:2 * D], in0=modrow[0:1, 5 * D:6 * D],
                                                  in1=grow[0:1, D:2 * D], op=ALU.mult), r=[B_mod, B_g], w=[B_rp])
            for j, dst in ((0, bc_m), (1, bc_f)):
                for hh in range(2):
                    S.op("pe", lambda e: e.matmul(pr[hh][:, :], lhsT=ones_f[0:1, :],
                                                  rhs=rprod[0:1, j * D + hh * 512:j * D + (hh + 1) * 512],
                                                  start=True, stop=True), r=[B_rp, B_const], w=[B_pr[hh]])
                    S.op("act", lambda e: e.activation(out=dst[:, hh * 512:(hh + 1) * 512], in_=pr[hh][:, :],
                                                       func=AF.Copy), r=[B_pr[hh]], w=[B_mod])
            if debug:
                d = dbg_out("modrow", [1, 6 * D])
                S.dma("sp", d, modrow[:], B_mod, False)
                d = dbg_out("bc_m", [128, D])
                S.dma("sp", d, bc_m[:], B_mod, False)
                d = dbg_out("gmod_m", [128, 8])
                S.dma("sp", d, gmod_m[:], B_mod, False)
            S.barrier()
            S.release([B_cc, B_g, B_tab, B_mod] + B_wab)


        def load_cast_weight(stack_tmp, dst, src_view, ncols, B_dst, stg, B_stg, engs=("dve", "act"), doff=0):
            nblk = (ncols + 255) // 256
            for cbk in range(nblk):
                c0 = cbk * 256
                c1 = min(ncols, c0 + 256)
                i = load_cast_weight.n % len(stg)
                load_cast_weight.n += 1
                kk = src_view.shape[1]
                S.dma("sp", stg[i][:, 0:kk, 0:c1 - c0], src_view[:, :, c0:c1], B_stg[i], True)
                eng = engs[cbk % len(engs)]
                if eng == "act":
                    S.op("act", lambda e: e.activation(out=dst[:, :, doff + c0:doff + c1], in_=stg[i][:, 0:kk, 0:c1 - c0], func=AF.Copy),
                         r=[B_stg[i]], w=[B_dst])
                else:
                    S.op(eng, lambda e: e.tensor_copy(out=dst[:, :, doff + c0:doff + c1], in_=stg[i][:, 0:kk, 0:c1 - c0]),
                         r=[B_stg[i]], w=[B_dst])
        load_cast_weight.n = 0

        if upto < 1:
            S.barrier()
            return nc, dbg

        with ExitStack() as ps:
            wukv = sbt(ps, "wukv", [128, 8, 1536], BF16)
            stg = [sbt(ps, "stg%d" % i, [128, 8, 256], F32) for i in range(2)]
            B_stg = [Buf("stg0"), Buf("stg1")]
            B_wukv = Buf("wukv")
            win_v = w_in.rearrange("(k p) n -> p k n", p=128)
            load_cast_weight(ps, wukv, win_v[:, :, 0:512], 512, B_wukv, stg, B_stg)
            load_cast_weight(ps, wukv, win_v[:, :, 1024:2048], 1024, B_wukv, stg, B_stg, doff=512)
            xt = [sbt(ps, "xt%d" % i, [128, 8, D], F32) for i in range(2)]
            B_xt = [Buf("xt0"), Buf("xt1")]
            junk = sbt(ps, "junk", [128, D], BF16)
            B_junk = Buf("junk")
            ssq = sbt(ps, "ssq", [128, 8], F32)
            rstd = sbt(ps, "rstd", [128, 8], F32)
            B_ssq, B_rstd = Buf("ssq"), Buf("rstd")
            xn = sbt(ps, "xn", [128, 8, D], BF16)
            B_xn = Buf("xn")
            hnT = sbt(ps, "hnT", [128, 8, TT], BF16)
            B_hnT = Buf("hnT")
            u_cm = sbt(ps, "u_cm", [128, 32, 8, 16], BF16)
            v_cm = sbt(ps, "v_cm", [128, 8, 512], BF16)
            kT = sbt(ps, "kT", [128, 4, TT], BF16)
            B_ucm, B_vcm, B_kT = Buf("ucm"), Buf("vcm"), Buf("kT")
            km_f = sbt(ps, "km_f", [128, 4, 4], F32)
            B_kmf = Buf("kmf")
            ptr = Ring([pst(ps, "ptr%d" % i, [128, TT], BF16) for i in range(2)], "ptr")
            pmm = Ring([pst(ps, "pmm%d" % i, [128, 512], F32) for i in range(4)], "pmm")
            xo_v = xo.rearrange("(t c s) d -> t c s d", c=128, s=8)
            xp_v = xp.rearrange("(t c s) d -> t c s d", c=128, s=8)
            evac_i = [0]

            def evac_copy(dst_ap, src_ap, r, w, scale=None):
                evac_i[0] += 1
                if evac_i[0] % 2 == 0:
                    if scale is None:
                        S.op("act", lambda e: e.activation(out=dst_ap, in_=src_ap, func=AF.Copy), r=r, w=w)
                    else:
                        S.op("act", lambda e: e.activation(out=dst_ap, in_=src_ap, func=AF.Copy, scale=scale), r=r, w=w)
                else:
                    if scale is None:
                        S.op("dve", lambda e: e.tensor_copy(out=dst_ap, in_=src_ap), r=r, w=w)
                    else:
                        S.op("dve", lambda e: e.tensor_scalar(out=dst_ap, in0=src_ap, scalar1=scale, scalar2=None,
                                                              op0=ALU.mult), r=r, w=w)

            def normA(xsrc_ap, xi):
                S.dma("sp", xt[xi][:], xsrc_ap, B_xt[xi], True)
                for s_ in range(8):
                    S.op("act", lambda e: e.activation(out=junk[:], in_=xt[xi][:, s_, :], func=AF.Square,
                                                       accum_out=ssq[:, s_:s_ + 1]), r=[B_xt[xi]], w=[B_junk, B_ssq])
                S.op("act", lambda e: e.activation(out=rstd[:], in_=ssq[:], func=AF.Sqrt, scale=1.0 / D, bias=EPS),
                     r=[B_ssq], w=[B_rstd])
                S.op("dve", lambda e: e.reciprocal(out=rstd[:], in_=rstd[:]), r=[B_rstd], w=[B_rstd])
                for s_ in range(8):
                    eng = "dve"
                    S.op(eng, lambda e: e.tensor_scalar(out=xn[:, s_, :], in0=xt[xi][:, s_, :],
                                                        scalar1=rstd[:, s_:s_ + 1], scalar2=None, op0=ALU.mult),
                         r=[B_xt[xi], B_rstd], w=[B_xn])

            def normT(gmod, shc):
                for k in range(8):
                    pt, B_pt = ptr.next()
                    S.op("pe", [(lambda e, s_=s_: e.transpose(out=pt[:, s_ * 128:(s_ + 1) * 128],
                                                              in_=xn[:, s_, k * 128:(k + 1) * 128], identity=ident_b[:]))
                                for s_ in range(8)], r=[B_xn, B_const], w=[B_pt])
                    if k % 2 == 0:
                        S.op("dve", lambda e: e.tensor_scalar(out=hnT[:, k, :], in0=pt[:, :], scalar1=gmod[:, k:k + 1],
                                                              scalar2=shc[:, k:k + 1], op0=ALU.mult, op1=ALU.add),
                             r=[B_pt, B_mod], w=[B_hnT])
                    else:
                        S.op("act", lambda e: e.activation(out=hnT[:, k, :], in_=pt[:, :], func=AF.Identity,
                                                           scale=gmod[:, k:k + 1], bias=shc[:, k:k + 1]),
                             r=[B_pt, B_mod], w=[B_hnT])

            for gt in range(2 * NT):
                own = gt >= NT
                ot = gt - NT
                if gt == 0:
                    normA(xp_v[0], 0)
                normT(gmod_m, sh_m)
                if gt + 1 < 2 * NT:
                    g2 = gt + 1
                    normA(xo_v[g2 - NT] if g2 >= NT else xp_v[g2], g2 % 2)
                if own:
                    S.dma("pool", scr_hn[ot], hnT[:], B_hnT, False)
                for s_ in range(8):
                    pu, B_pu = pmm.next()
                    S.op("pe", [(lambda e, k=k: e.matmul(pu[:, :], lhsT=hnT[:, k, s_ * 128:(s_ + 1) * 128],
                                                         rhs=wukv[:, k, 0:512], start=(k == 0), stop=(k == 7)))
                                for k in range(8)], r=[B_hnT, B_wukv], w=[B_pu])
                    evac_copy(u_cm[:, :, s_, :], pu[:, :].rearrange("p (g c) -> p g c", g=32), [B_pu], [B_ucm])
                    pv, B_pv = pmm.next()
                    S.op("pe", [(lambda e, k=k: e.matmul(pv[:, :], lhsT=hnT[:, k, s_ * 128:(s_ + 1) * 128],
                                                         rhs=wukv[:, k, 1024:1536], start=(k == 0), stop=(k == 7)))
                                for k in range(8)], r=[B_hnT, B_wukv], w=[B_pv])
                    evac_copy(v_cm[:, s_, :], pv[:, :], [B_pv], [B_vcm])
                S.dma("pool", scr_v[gt].rearrange("s c f -> c s f"), v_cm[:], B_vcm, False)
                S.dma("pool", scr_u[gt], u_cm[:], B_ucm, False)
                for cb in range(4):
                    for hf in range(2):
                        pk, B_pk = pmm.next()
                        S.op("pe", [(lambda e, k=k: e.matmul(pk[:, :], lhsT=wukv[:, k, 512 + cb * 128:512 + (cb + 1) * 128],
                                                             rhs=hnT[:, k, hf * 512:(hf + 1) * 512],
                                                             start=(k == 0), stop=(k == 7))) for k in range(8)],
                             r=[B_hnT, B_wukv], w=[B_pk])
                        evac_copy(kT[:, cb, hf * 512:(hf + 1) * 512], pk[:, :], [B_pk], [B_kT])
                S.dma("pool", scr_kt[:, :, gt * TT:(gt + 1) * TT].rearrange("b p n -> p b n"), kT[:], B_kT, False)
                S.op("dve", lambda e: e.tensor_reduce(out=km_f[:], in_=kT[:].rearrange("p b (s k c) -> p b k s c", s=8, k=4, c=32),
                                                      axis=AX.XY, op=ALU.add), r=[B_kT], w=[B_kmf])
                S.op("dve", lambda e: e.tensor_scalar(out=kmean[:, :, gt * 4:(gt + 1) * 4], in0=km_f[:], scalar1=1.0 / 256.0,
                                                      scalar2=None, op0=ALU.mult), r=[B_kmf], w=[B_kmean])
                if debug and gt == 0:
                    for nm, t_, B_, shp, dt_ in (("hnT", hnT, B_hnT, [128, 8, TT], BF16), ("u_cm", u_cm, B_ucm, [128, 32, 8, 16], BF16),
                                                 ("v_cm", v_cm, B_vcm, [128, 8, 512], BF16), ("kT", kT, B_kT, [128, 4, TT], BF16)):
                        S.dma("sp", dbg_out(nm, shp, dt_), t_[:], B_, False)
            if debug:
                S.dma("sp", dbg_out("kmean", [128, 4, 32], BF16), kmean[:], B_kmean, False)
            S.barrier()
            S.release([B_wukv] + B_stg + B_xt + [B_hnT, B_vcm, B_kT, B_ucm, B_kmean])


        if upto < 2:
            S.barrier()
            return nc, dbg

        with ExitStack() as ps:
            W1t = sbt(ps, "W1t", [128, 32, 128], BF16)
            Ktoep = sbt(ps, "Ktoep", [128, 32, 128], BF16)
            Ctab = sbt(ps, "Ctab", [128, 32, 128], BF16)
            cosT = sbt(ps, "cosT", [128, 32, 64], F32)
            sinT = sbt(ps, "sinT", [128, 32, 64], F32)
            rho = sbt(ps, "rho", [128, 32], F32)
            Jt = sbt(ps, "Jt", [128, 128], F32)
            B_tab = Buf("tab")
            pss = Ring([pst(ps, "pss%d" % i, [128, 512], F32) for i in range(6)], "pss")
            ptb = Ring([pst(ps, "ptb%d" % i, [128, 1024], BF16) for i in range(2)], "ptb")

            for t_, src_ in ((W1t, scr_W1t), (Ktoep, scr_Ktoep), (Ctab, scr_Ctab), (cosT, scr_cosT), (sinT, scr_sinT),
                             (rho, scr_rho), (Jt, scr_Jt)):
                S.dma("sp", t_[:], src_, B_tab, True)
            ucm = [sbt(ps, "ucm%d" % i, [128, 32, 128], BF16) for i in range(2)]
            B_ucm2 = [Buf("ucm0"), Buf("ucm1")]
            U = sbt(ps, "U", [128, 32, 128], BF16)
            B_U = [Buf("U%d" % i) for i in range(4)]
            SX = sbt(ps, "SX", [128, 32, 128], F32)
            B_SX = [Buf("SX%d" % i) for i in range(8)]
            Wh = sbt(ps, "Wh", [128, 32, 64], F32)
            B_Wh = [Buf("Wh%d" % i) for i in range(4)]
            Xprev = sbt(ps, "Xprev", [128, 32, 128], BF16)
            B_Xp = Buf("Xprev")
            carry = sbt(ps, "carry", [128, 32], F32)
            carry2 = sbt(ps, "carry2", [128, 32], F32)
            B_carry, B_carry2 = Buf("carry"), Buf("carry2")
            tmp2 = [sbt(ps, "tmp2_%d" % i, [128, 8, 64], F32) for i in range(2)]
            B_tmp2 = [Buf("tmp2_0"), Buf("tmp2_1")]
            zg = sbt(ps, "zg", [128, 32, 128], BF16)
            B_zg = [Buf("zg%d" % i) for i in range(8)]
            z_cm = sbt(ps, "z_cm", [128, 8, 512], BF16)
            B_zcm = Buf("z_cm")
            zT = sbt(ps, "zT", [128, 4, TT], BF16)
            B_zT = Buf("zT")
            S.op("dve", lambda e: e.memset(carry[:], 0.0), w=[B_carry])
            tcount = [0]
            Zh = [sbt(ps, "Zh%d" % i, [128, 32, 64], F32) for i in range(2)]
            B_Zh = [Buf("Zh0"), Buf("Zh1")]
            rhoT = sbt(ps, "rhoT", [128, 32, 64], F32)
            tmpc = sbt(ps, "tmpc", [128, 32], F32)
            B_tmpc = Buf("tmpc")
            S.op("dve", lambda e: e.tensor_copy(out=rhoT[:], in_=rho[:].unsqueeze(2).to_broadcast([128, 32, 64])), r=[B_tab], w=[B_tab])
            S.op("dve", lambda e: e.memset(rhoT[:, :, 0], 0.0), r=[B_tab], w=[B_tab])

            def scan_half(hf, init_buf_ap, B_init):
                S.op("dve", lambda e: e.tensor_tensor(out=tmpc[:], in0=rho[:], in1=init_buf_ap[:], op=ALU.mult),
                     r=[B_init, B_tab], w=[B_tmpc])
                S.op("dve", lambda e: e.tensor_tensor(out=Zh[hf][:, :, 0], in0=Zh[hf][:, :, 0], in1=tmpc[:], op=ALU.add),
                     r=[B_tmpc], w=[B_Zh[hf]])
                S.op("dve", lambda e: e.tensor_tensor_scan(out=Wh[:].rearrange("p g c -> p (g c)"),
                                                           data0=rhoT[:].rearrange("p g c -> p (g c)"),
                                                           data1=Zh[hf][:].rearrange("p g c -> p (g c)"), initial=0.0,
                                                           op0=ALU.mult, op1=ALU.add),
                     r=[B_Zh[hf], B_tab], w=B_Wh)

            def last_state(dst, B_dst):
                pj, B_pj = pss.next()
                S.op("pe", lambda e: e.matmul(pj[:, 0:32], lhsT=Jt[:], rhs=Wh[:, :, 63], start=True, stop=True),
                     r=B_Wh + [B_tab], w=[B_pj])
                S.op("dve", lambda e: e.tensor_tensor(out=tmpc[:], in0=pj[:, 0:32], in1=sinT[:, :, 63], op=ALU.mult),
                     r=[B_pj, B_tab], w=[B_tmpc])
                S.op("dve", lambda e: e.tensor_tensor(out=dst[:], in0=Wh[:, :, 63], in1=cosT[:, :, 63], op=ALU.mult),
                     r=B_Wh + [B_tab], w=[B_dst])
                S.op("dve", lambda e: e.tensor_tensor(out=dst[:], in0=dst[:], in1=tmpc[:], op=ALU.add), r=[B_tmpc], w=[B_dst])

            def rot_unrot(hf, init_buf_ap, B_init):
                c0 = hf * 64
                scan_half(hf, init_buf_ap, B_init)
                for q in range(4):
                    pj, B_pj = pss.next()
                    S.op("pe", lambda e: e.matmul(pj[:, :], lhsT=Jt[:], rhs=Wh[:, 8 * q:8 * q + 8, :].rearrange("p g c -> p (g c)"), start=True, stop=True),
                         r=[B_Wh[q], B_tab], w=[B_pj])
                    i2 = tcount[0] % 2
                    tcount[0] += 1
                    S.op("dve", lambda e: e.tensor_tensor(out=tmp2[i2][:], in0=pj[:, :].rearrange("p (g c) -> p g c", g=8),
                                                          in1=sinT[:, 8 * q:8 * q + 8, :], op=ALU.mult),
                         r=[B_pj, B_tab], w=[B_tmp2[i2]])
                    sxv = SX[:, 8 * q:8 * q + 8, c0:c0 + 64]
                    S.op("dve", lambda e: e.tensor_tensor(out=sxv, in0=Wh[:, 8 * q:8 * q + 8, :], in1=cosT[:, 8 * q:8 * q + 8, :],
                                                           op=ALU.mult), r=[B_Wh[q], B_tab], w=[B_SX[2 * q], B_SX[2 * q + 1]])
                    S.op("dve", lambda e: e.tensor_tensor(out=sxv, in0=sxv, in1=tmp2[i2][:], op=ALU.add),
                         r=[B_tmp2[i2]], w=[B_SX[2 * q], B_SX[2 * q + 1]])

            for gt in range(2 * NT):
                own = gt >= NT
                ot = gt - NT
                ui = gt % 2
                S.dma("sp", ucm[ui][:], scr_u[gt].rearrange("p g s c -> p g (s c)"), B_ucm2[ui], True)
                for q in range(4):
                    pt, B_pt = ptb.next()
                    S.op("pe", [(lambda e, j=j: e.transpose(out=pt[:, j * 128:(j + 1) * 128],
                                                            in_=ucm[ui][:, 8 * q + j, :],
                                                            identity=ident_b[:])) for j in range(8)],
                         r=[B_ucm2[ui], B_const], w=[B_pt])
                    evac_copy2 = "act" if q % 2 == 0 else "dve"
                    if evac_copy2 == "act":
                        S.op("act", lambda e: e.activation(out=U[:, 8 * q:8 * q + 8, :], in_=pt[:, :].rearrange("p (g c) -> p g c", g=8),
                                                           func=AF.Copy), r=[B_pt], w=[B_U[q]])
                    else:
                        S.op("dve", lambda e: e.tensor_copy(out=U[:, 8 * q:8 * q + 8, :], in_=pt[:, :].rearrange("p (g c) -> p g c", g=8)),
                             r=[B_pt], w=[B_U[q]])
                S.op("dve", lambda e: e.tensor_copy(out=Xprev[:, :, 0], in_=carry[:]), r=[B_carry], w=[B_Xp])
                for gb in range(8):
                    pa, B_pa = pss.next()
                    S.op("pe", [(lambda e, j=j: e.matmul(pa[:, j * 128:(j + 1) * 128], lhsT=W1t[:, 4 * gb + j, :],
                                                         rhs=U[:, 4 * gb + j, :], start=True, stop=True)) for j in range(4)],
                         r=[B_U[gb // 2], B_tab], w=[B_pa])
                    S.op("act", lambda e: e.activation(out=SX[:, 4 * gb:4 * gb + 4, :], in_=pa[:, :].rearrange("p (g c) -> p g c", g=4),
                                                       func=AF.Copy), r=[B_pa], w=[B_SX[gb]])
                    pj, B_pj = pss.next()
                    S.op("pe", lambda e: e.matmul(pj[:, :], lhsT=Jt[:], rhs=SX[:, 4 * gb:4 * gb + 4, :].rearrange("p g c -> p (g c)"), start=True, stop=True),
                         r=[B_SX[gb], B_tab], w=[B_pj])
                    for hf in range(2):
                        i2 = tcount[0] % 2
                        tcount[0] += 1
                        pjv = pj[:, :].rearrange("p (g c) -> p g c", g=4)[:, :, hf * 64:(hf + 1) * 64]
                        S.op("dve", lambda e: e.tensor_tensor(out=tmp2[i2][:, 0:4, :], in0=pjv, in1=sinT[:, 4 * gb:4 * gb + 4, :],
                                                              op=ALU.mult), r=[B_pj, B_tab], w=[B_tmp2[i2]])
                        sxv = SX[:, 4 * gb:4 * gb + 4, hf * 64:(hf + 1) * 64]
                        S.op("dve", lambda e: e.tensor_tensor(out=sxv, in0=sxv, in1=cosT[:, 4 * gb:4 * gb + 4, :], op=ALU.mult),
                             r=[B_tab], w=[B_SX[gb]])
                        S.op("dve", lambda e: e.tensor_tensor(out=Zh[hf][:, 4 * gb:4 * gb + 4, :], in0=sxv, in1=tmp2[i2][:, 0:4, :], op=ALU.subtract),
                             r=[B_tmp2[i2], B_SX[gb]], w=[B_Zh[hf]])
                if own:
                    rot_unrot(0, carry, B_carry)
                    S.op("dve", lambda e: e.tensor_copy(out=carry2[:], in_=SX[:, :, 63]), r=B_SX, w=[B_carry2])
                    rot_unrot(1, carry2, B_carry2)
                    S.op("dve", lambda e: e.tensor_copy(out=carry[:], in_=SX[:, :, 127]), r=B_SX, w=[B_carry])
                else:
                    scan_half(0, carry, B_carry)
                    last_state(carry2, B_carry2)
                    scan_half(1, carry2, B_carry2)
                    last_state(carry, B_carry)
                if gt == NT - 1:
                    S.op("dve", lambda e: e.tensor_scalar(out=carry[:], in0=carry[:], scalar1=flag_sb[:, 0:1], scalar2=None,
                                                          op0=ALU.mult), r=[B_carry, B_const], w=[B_carry])
                if not own:
                    continue
                S.op("act", lambda e: e.activation(out=Xprev[:, :, 1:128], in_=SX[:, :, 0:127], func=AF.Copy), r=B_SX, w=[B_Xp])
                for gb in range(8):
                    py, B_py = pss.next()
                    fns = []
                    for j in range(4):
                        g = 4 * gb + j
                        fns.append(lambda e, j=j, g=g: e.matmul(py[:, j * 128:(j + 1) * 128], lhsT=Ktoep[:, g, :], rhs=U[:, g, :],
                                                                start=True, stop=False))
                        fns.append(lambda e, j=j, g=g: e.matmul(py[:, j * 128:(j + 1) * 128], lhsT=Ctab[:, g, :], rhs=Xprev[:, g, :],
                                                                start=False, stop=True))
                    S.op("pe", fns, r=[B_U[gb // 2], B_Xp, B_tab], w=[B_py])
                    S.op("act", lambda e: e.activation(out=zg[:, 4 * gb:4 * gb + 4, :], in_=py[:, :].rearrange("p (g c) -> p g c", g=4),
                                                       func=AF.Gelu), r=[B_py], w=[B_zg[gb]])
                for q in range(4):
                    pt, B_pt = ptb.next()
                    S.op("pe", [(lambda e, j=j: e.transpose(out=pt[:, j * 128:(j + 1) * 128], in_=zg[:, 8 * q + j, :],
                                                            identity=ident_b[:])) for j in range(8)],
                         r=[B_zg[2 * q], B_zg[2 * q + 1], B_const], w=[B_pt])
                    S.op("dve", lambda e: e.tensor_copy(out=z_cm[:, :, q * 128:(q + 1) * 128].rearrange("p t (g c) -> p g t c", g=8),
                                                        in_=pt[:, :].rearrange("p (g t c) -> p g t c", g=8, t=8)),
                         r=[B_pt], w=[B_zcm])
                for blk in range(4):
                    pt, B_pt = ptb.next()
                    S.op("pe", [(lambda e, t_=t_: e.transpose(out=pt[:, t_ * 128:(t_ + 1) * 128],
                                                              in_=z_cm[:, t_, blk * 128:(blk + 1) * 128], identity=ident_b[:]))
                                for t_ in range(8)], r=[B_zcm, B_const], w=[B_pt])
                    if blk % 2 == 0:
                        S.op("act", lambda e: e.activation(out=zT[:, blk, :], in_=pt[:, :], func=AF.Copy), r=[B_pt], w=[B_zT])
                    else:
                        S.op("dve", lambda e: e.tensor_copy(out=zT[:, blk, :], in_=pt[:, :]), r=[B_pt], w=[B_zT])
                S.dma("pool", scr_z[:, :, ot * TT:(ot + 1) * TT].rearrange("b p n -> p b n"), zT[:], B_zT, False)
                if debug and ot == 0:
                    S.dma("sp", dbg_out("z_cm", [128, 8, 512], BF16), z_cm[:], B_zcm, False)
                    S.dma("sp", dbg_out("SX", [128, 32, 128], F32), SX[:], B_SX[0], False, extra_r=B_SX)
            S.barrier()
            S.release([B_tab, B_zT, B_zcm] + B_ucm2 + B_SX)


        if upto < 3:
            S.barrier()
            return nc, dbg

        ps12 = ExitStack()
        st12 = ExitStack()
        Kaug = [sbt(ps12, "Kaug%d" % i, [128, 2 * TOK], BF16) for i in range(2)]
        QAbase = sbt(ps12, "QAbase", [128, TOK], BF16)
        CB = sbt(ps12, "CB", [128, 8, 8, 128], BF16)
        VB = sbt(ps12, "VB", [128, NT, 32], F32)
        OWNM = sbt(ps12, "OWNM", [128, NT, 32], F32)
        B_st = Buf("p2static")
        wqg = sbt(st12, "wqg", [128, 8, 2560], BF16)
        B_wqg = Buf("wqg")
        with ExitStack() as st:
            stg = [sbt(st, "stgb%d" % i, [128, 8, 256], F32) for i in range(2)]
            B_stg = [Buf("stgb0"), Buf("stgb1")]
            load_cast_weight(st, wqg, win_v[:, :, 512:1024], 512, B_wqg, stg, B_stg)
            load_cast_weight(st, wqg, win_v[:, :, 2048:4096], 2048, B_wqg, stg, B_stg, doff=512)
            S.barrier()
            S.release(B_stg)
        def P2s(eng, fn):
            S.op(eng, fn, r=[B_st, B_const], w=[B_st])

        st = st12
        f32t = lambda nm, shp: sbt(st, nm, shp, F32)
        pidx = f32t("pidx", [128, 1])
        cA, cB_, cC = f32t("cA", [128, 1]), f32t("cB", [128, 1]), f32t("cC", [128, 1])
        qA, qB, qC = f32t("qA", [128, 1]), f32t("qB", [128, 1]), f32t("qC", [128, 1])
        p64 = f32t("p64", [128, 1])
        shi, slo, blk = f32t("shi", [128, TT]), f32t("slo", [128, TT]), f32t("blkt", [128, TT])
        ka = f32t("ka", [128, TT])
        cble, cblt = f32t("cble", [128, 128]), f32t("cblt", [128, 128])
        P2s("pool", lambda e: e.iota(pidx[:], pattern=[[0, 1]], base=0, channel_multiplier=1,
                                     allow_small_or_imprecise_dtypes=True))
        P2s("dve", lambda e: e.tensor_scalar(out=p64[:], in0=pidx[:], scalar1=-64.0, scalar2=None, op0=ALU.add))
        for (dst, val) in ((cA, 98.0), (cB_, 99.0), (qA, 96.0), (qB, 97.0)):
            P2s("dve", lambda e: e.tensor_scalar(out=dst[:], in0=pidx[:], scalar1=val, scalar2=None, op0=ALU.is_equal))
        P2s("dve", lambda e: e.tensor_tensor(out=cC[:], in0=qA[:], in1=qB[:], op=ALU.add))
        P2s("dve", lambda e: e.tensor_tensor(out=qC[:], in0=cA[:], in1=cB_[:], op=ALU.add))
        P2s("dve", lambda e: e.tensor_scalar(out=qA[:], in0=qA[:], scalar1=-1.0, scalar2=None, op0=ALU.mult))
        P2s("dve", lambda e: e.tensor_scalar(out=qB[:], in0=qB[:], scalar1=-1.0, scalar2=None, op0=ALU.mult))
        P2s("pool", lambda e: e.iota(slo[:], pattern=[[1, 8], [0, 16], [8, 8]], base=0, channel_multiplier=0,
                                     allow_small_or_imprecise_dtypes=True))
        for gt in range(2 * NT):
            P2s("pool", lambda e: e.iota(shi[:], pattern=[[0, 8], [64, 16], [0, 8]], base=gt * TT - TOK, channel_multiplier=0,
                                         allow_small_or_imprecise_dtypes=True))
            P2s("pool", lambda e: e.iota(blk[:], pattern=[[0, 8], [1, 4], [0, 32]], base=gt * 4, channel_multiplier=0,
                                         allow_small_or_imprecise_dtypes=True))
            P2s("dve", lambda e: e.tensor_scalar(out=ka[:], in0=blk[:], scalar1=p64[:, 0:1], scalar2=cC[:, 0:1],
                                                 op0=ALU.is_equal, op1=ALU.add))
            P2s("dve", lambda e: e.scalar_tensor_tensor(out=ka[:], in0=shi[:], scalar=cA[:, 0:1], in1=ka[:],
                                                        op0=ALU.mult, op1=ALU.add))
            P2s("dve", lambda e: e.scalar_tensor_tensor(out=ka[:], in0=slo[:], scalar=cB_[:, 0:1], in1=ka[:],
                                                        op0=ALU.mult, op1=ALU.add))
            for i in range(2):
                P2s("dve", lambda e: e.tensor_copy(out=Kaug[i][64:128, gt * TT:(gt + 1) * TT], in_=ka[64:128, :]))
            if gt >= NT:
                ot = gt - NT
                P2s("dve", lambda e: e.tensor_scalar(out=ka[:], in0=shi[:], scalar1=qA[:, 0:1], scalar2=qC[:, 0:1],
                                                     op0=ALU.mult, op1=ALU.add))
                P2s("dve", lambda e: e.scalar_tensor_tensor(out=QAbase[:, ot * TT:(ot + 1) * TT], in0=slo[:], scalar=qB[:, 0:1],
                                                            in1=ka[:], op0=ALU.mult, op1=ALU.add))
        P2s("pool", lambda e: e.memset(cble[:], 0.0))
        P2s("pool", lambda e: e.memset(cblt[:], 0.0))
        for i in range(4):
            sl = slice(32 * i, 32 * i + 32)
            P2s("pool", lambda e: e.affine_select(out=cble[sl, sl], in_=cble[sl, sl], pattern=[[1, 32]], compare_op=ALU.is_ge,
                                                  fill=NEG, base=0, channel_multiplier=-1))
            P2s("pool", lambda e: e.affine_select(out=cblt[sl, sl], in_=cblt[sl, sl], pattern=[[1, 32]], compare_op=ALU.is_ge,
                                                  fill=NEG, base=-1, channel_multiplier=-1))
        for sk in range(8):
            for sq in range(8):
                src = cble if sk <= sq else cblt
                P2s("dve", lambda e: e.tensor_copy(out=CB[:, sk, sq, :], in_=src[:]))
        P2s("pool", lambda e: e.memset(VB[:], 0.0))
        P2s("pool", lambda e: e.memset(OWNM[:], 1.0))
        P2s("dve", lambda e: e.tensor_scalar(out=VB[:, :, 0:16], in0=ones_f[:, 0:64].rearrange("p (a b) -> p a b", a=NT),
                                             scalar1=flag_sb[:, 0:1], scalar2=-1.0, op0=ALU.mult, op1=ALU.add))
        P2s("dve", lambda e: e.tensor_scalar(out=VB[:, :, 0:16], in0=VB[:, :, 0:16], scalar1=1e30, scalar2=None, op0=ALU.mult))
        for ot in range(NT):
            for i in range(4):
                j = 4 * ot + i
                sl = slice(32 * i, 32 * i + 32)
                P2s("pool", lambda e: e.affine_select(out=VB[sl, ot, 16:32], in_=VB[sl, ot, 16:32], pattern=[[-1, 16]],
                                                      compare_op=ALU.is_ge, fill=-1e30, base=j - 1, channel_multiplier=0))
                P2s("pool", lambda e: e.affine_select(out=OWNM[sl, ot, :], in_=OWNM[sl, ot, :], pattern=[[1, 32]],
                                                      compare_op=ALU.not_equal, fill=0.0, base=-(16 + j), channel_multiplier=0))

        with ExitStack() as ps:
            hnl = [sbt(ps, "hnl%d" % i, [128, 8, TT], BF16) for i in range(2)]
            B_hnl = [Buf("hnl0"), Buf("hnl1")]
            qT = sbt(ps, "qT", [128, 4, TT], BF16)
            gT = sbt(ps, "gT", [128, 16, TT], BF16)
            B_qT, B_gT = Buf("qT"), Buf("gT")
            pmm = Ring([pst(ps, "pmb%d" % i, [128, 512], F32) for i in range(6)], "pmb")
            for ot in range(NT):
                hi_ = ot % 2
                S.dma("sp", hnl[hi_][:], scr_hn[ot], B_hnl[hi_], True)
                for cb in range(20):
                    for hf in range(2):
                        pq, B_pq = pmm.next()
                        S.op("pe", [(lambda e, k=k: e.matmul(pq[:, :], lhsT=wqg[:, k, cb * 128:(cb + 1) * 128],
                                                             rhs=hnl[hi_][:, k, hf * 512:(hf + 1) * 512],
                                                             start=(k == 0), stop=(k == 7))) for k in range(8)],
                             r=[B_hnl[hi_], B_wqg], w=[B_pq])
                        if cb < 4:
                            S.op("act", lambda e: e.activation(out=qT[:, cb, hf * 512:(hf + 1) * 512], in_=pq[:, :],
                                                               func=AF.Copy, scale=0.125), r=[B_pq], w=[B_qT])
                        elif cb < 12:
                            S.op("act", lambda e: e.activation(out=gT[:, cb - 4, hf * 512:(hf + 1) * 512], in_=pq[:, :],
                                                               func=AF.Sigmoid), r=[B_pq], w=[B_gT])
                        else:
                            S.op("dve", lambda e: e.tensor_copy(out=gT[:, cb - 4, hf * 512:(hf + 1) * 512], in_=pq[:, :]),
                                 r=[B_pq], w=[B_gT])
                S.dma("pool", scr_q[:, :, ot * TT:(ot + 1) * TT].rearrange("b p n -> p b n"), qT[:], B_qT, False)
                S.dma("pool", scr_g[:, :, ot * TT:(ot + 1) * TT].rearrange("b p n -> p b n"), gT[:], B_gT, False)
            S.barrier()
            S.release([B_qT, B_gT, B_wqg] + B_hnl)
        st12.close()

        if upto < 4:
            S.barrier()
            return nc, dbg

        with ExitStack() as ps:
            Qaug = [sbt(ps, "Qaug%d" % i, [128, TOK], BF16) for i in range(2)]
            Vh = [sbt(ps, "Vh%d" % i, [128, 64, 128], BF16) for i in range(2)]
            B_K = [Buf("K0"), Buf("K1")]
            B_Q = [Buf("Q0"), Buf("Q1")]
            B_Qs = [Buf("Qs0"), Buf("Qs1")]
            B_V = [Buf("V0"), Buf("V1")]
            oT = sbt(ps, "oT", [128, TOK], BF16)
            B_oT = Buf("oT")
            kmh = sbt(ps, "kmh", [128, 32], F32)
            kmb = sbt(ps, "kmb", [128, 32], BF16)
            B_kmh = Buf("kmh")
            gm = sbt(ps, "gm", [128, 8, 32], F32)
            m8 = sbt(ps, "m8", [128, 8, 8], F32)
            thr = sbt(ps, "thr", [128, 8], F32)
            selb = sbt(ps, "selb", [128, 8, 32], BF16)
            B_gm, B_m8, B_thr, B_selb = Buf("gm"), Buf("m8"), Buf("thr"), Buf("selb")
            pTs = [sbt(ps, "pTs%d" % i, [128, 1024], BF16) for i in range(3)]
            B_pTs = [Buf("pTs%d" % i) for i in range(3)]
            rrow = sbt(ps, "rrow", [128, 512], F32)
            bcs = sbt(ps, "bcs", [128, 512], F32)
            B_rrow, B_bcs = Buf("rrow"), Buf("bcs")
            pS = Ring([pst(ps, "pS%d" % i, [128, 1024], F32) for i in range(2)], "pS")
            pAcc = Ring([pst(ps, "pAcc%d" % i, [128, 512], F32) for i in range(2)], "pAcc")
            pG = Ring([pst(ps, "pG%d" % i, [128, 512], F32) for i in range(1)], "pG")
            pX = Ring([pst(ps, "pX%d" % i, [128, 1024], BF16) for i in range(1)], "pX")

            for i in range(2):
                S.op("dve", lambda e: e.memset(Vh[i][:, :, 64:128], 0.0), w=[B_V[i]])
                S.op("dve", lambda e: e.memset(Vh[i][:, :, 64:65], 1.0), w=[B_V[i]])

            scr_v_r = scr_v.rearrange("t s c f -> c (t s) f")
            LAG = 1
            pend = []
            step = [0]
            seq = [0]

            def sched(due, fn):
                seq[0] += 1
                pend.append((due, seq[0], fn))
                pend.sort(key=lambda t_: (t_[0], t_[1]))

            def flush(upto_step):
                while pend and pend[0][0] <= upto_step:
                    pend.pop(0)[2]()

            def emit_loads(hd):
                cb, h2, hb = hd // 2, hd % 2, hd % 2
                slope = 2.0 ** (-(hd + 1))
                S.dma("sp", Kaug[hb][0:64, :], scr_kt[cb, h2 * 64:(h2 + 1) * 64, :], B_K[hb], True)
                S.dma("sp", Qaug[hb][0:64, :], scr_q[cb, h2 * 64:(h2 + 1) * 64, :], B_Q[hb], True)
                S.dma("sp", Vh[hb][:, :, 0:64], scr_v_r[:, :, hd * 64:(hd + 1) * 64], B_V[hb], True)

            def sel_items(hd):
                cb, h2, hb = hd // 2, hd % 2, hd % 2
                slope = 2.0 ** (-(hd + 1))
                items = []

                def st0():
                    S.op("dve", lambda e: e.tensor_scalar(out=Qaug[hb][96:128, :], in0=QAbase[96:128, :], scalar1=slope, scalar2=None,
                                                           op0=ALU.mult), r=[B_st], w=[B_Qs[hb]])
                    S.op("dve", lambda e: e.tensor_reduce(out=kmh[0:64, :].rearrange("p (t k) -> p t k", t=2 * NT),
                                                          in_=Kaug[hb][0:64, :].rearrange("p (t s k c) -> p t k s c", t=2 * NT, s=8, k=4, c=32),
                                                          axis=AX.XY, op=ALU.add), r=[B_K[hb]], w=[B_kmh])
                    S.op("dve", lambda e: e.tensor_scalar(out=kmb[0:64, :], in0=kmh[0:64, :], scalar1=1.0 / 256.0, scalar2=None, op0=ALU.mult),
                         r=[B_kmh], w=[B_kmh])
                items.append((2, st0))
                for ot in range(NT):
                    base = 15 + 30 * ot
                    holder = {}

                    def stA(ot=ot, holder=holder):
                        pg, B_pg = pG.next()
                        holder["pg"] = (pg, B_pg)
                        S.op("pe", [(lambda e, s_=s_: e.matmul(pg[:, s_ * 32:(s_ + 1) * 32],
                                                               lhsT=Qaug[hb][0:64, ot * TT + s_ * 128:ot * TT + (s_ + 1) * 128],
                                                               rhs=kmb[0:64, :], start=True, stop=True)) for s_ in range(8)],
                             r=[B_Q[hb], B_kmh], w=[B_pg])

                    def stB(ot=ot, holder=holder):
                        pg, B_pg = holder["pg"]
                        S.op("dve", lambda e: e.tensor_tensor(out=gm[:], in0=pg[:, 0:256].rearrange("p (s n) -> p s n", s=8),
                                                              in1=VB[:, ot:ot + 1, :].to_broadcast([128, 8, 32]), op=ALU.add),
                             r=[B_pg, B_st], w=[B_gm])
                        S.op("dve", [(lambda e, s_=s_: e.max(out=m8[:, s_, :], in_=gm[:, s_, :])) for s_ in range(8)], r=[B_gm], w=[B_m8])
                        S.op("dve", lambda e: e.tensor_scalar(out=thr[:], in0=m8[:, :, 2], scalar1=-1e29, scalar2=None, op0=ALU.max),
                             r=[B_m8], w=[B_thr])
                        S.op("dve", lambda e: e.tensor_tensor(out=gm[:], in0=gm[:], in1=thr[:].unsqueeze(2).to_broadcast([128, 8, 32]),
                                                              op=ALU.subtract), r=[B_thr], w=[B_gm])
                        S.op("dve", lambda e: e.tensor_scalar(out=gm[:], in0=gm[:], scalar1=0.0, scalar2=NEG, op0=ALU.is_lt, op1=ALU.mult),
                             r=[B_gm], w=[B_gm])
                        S.op("dve", lambda e: e.tensor_tensor(out=selb[:], in0=gm[:], in1=OWNM[:, ot:ot + 1, :].to_broadcast([128, 8, 32]),
                                                              op=ALU.mult), r=[B_gm, B_st], w=[B_selb])

                    def stC(ot=ot, holder=holder):
                        px, B_px = pX.next()
                        holder["px"] = (px, B_px)
                        S.op("pe", [(lambda e, s_=s_: e.transpose(out=px[64:96, s_ * 128:(s_ + 1) * 128], in_=selb[:, s_, :],
                                                                  identity=ident_b[:])) for s_ in range(8)],
                             r=[B_selb, B_const], w=[B_px])

                    def stD(ot=ot, holder=holder):
                        px, B_px = holder["px"]
                        S.op("dve", lambda e: e.tensor_copy(out=Qaug[hb][64:96, ot * TT:(ot + 1) * TT], in_=px[64:96, :]),
                             r=[B_px], w=[B_Qs[hb]])
                    items += [(base, stA), (base, stB), (base + 12, stC), (base + 16, stD)]
                return items

            cvA = [sbt(ps, "cvA%d" % i, [128, 8, 256], F32) for i in range(3)]
            cvB = [sbt(ps, "cvB%d" % i, [128, 8, 256], BF16) for i in range(3)]
            B_cvA = [Buf("cvA%d" % i) for i in range(3)]
            B_cvB = [Buf("cvB%d" % i) for i in range(3)]
            cv_jobs = []
            kp = lambda w_: w_.rearrange("(k p) n -> p k n", p=128)
            for src, dst, K_, N_ in ((kp(w_glu_a), scr_wga, 4, D), (kp(w_glu_b), scr_wgb, 4, D), (kp(w_attn_out), scr_wao, 4, D),
                                     (kp(w_out), scr_wo, 8, D), (kp(w_ff_gate), scr_wg, 8, DFF), (kp(w_ff_up), scr_wu, 8, DFF),
                                     (kp(w_ff_down), scr_wd, NF, D)):
                for k0 in range(0, K_, 8):
                    kk = min(8, K_ - k0)
                    for c0 in range(0, N_, 256):
                        cv_jobs.append((src, dst, k0, kk, c0, min(256, N_ - c0)))
            for ji, (src, dst, k0, kk, c0, w_) in enumerate(cv_jobs):
                i3 = ji % 3
                t_in = 30 + 18 * ji

                def cv_in(src=src, k0=k0, kk=kk, c0=c0, w_=w_, i3=i3):
                    S.dma("sp", cvA[i3][:, 0:kk, 0:w_], src[:, k0:k0 + kk, c0:c0 + w_], B_cvA[i3], True)

                def cv_cast(kk=kk, w_=w_, i3=i3):
                    S.op("dve", lambda e: e.tensor_copy(out=cvB[i3][:, 0:kk, 0:w_], in_=cvA[i3][:, 0:kk, 0:w_]),
                         r=[B_cvA[i3]], w=[B_cvB[i3]])

                def cv_out(dst=dst, k0=k0, kk=kk, c0=c0, w_=w_, i3=i3):
                    S.dma("pool", dst[:, k0:k0 + kk, c0:c0 + w_], cvB[i3][:, 0:kk, 0:w_], B_cvB[i3], False)
                sched(t_in, cv_in)
                sched(t_in + 15, cv_cast)
                sched(t_in + 17, cv_out)

            emit_loads(0)
            for (_, fn) in sel_items(0):
                fn()
            for hd in range(8):
                cb, h2 = hd // 2, hd % 2
                hb = hd % 2
                if hd + 1 < 8:
                    emit_loads(hd + 1)
                    for (rel, fn) in sel_items(hd + 1):
                        sched(step[0] + rel, fn)
                for ot in range(NT):
                    for qh in range(2):
                        q0 = ot * TT + qh * 512
                        acc, B_acc = pAcc.next()
                        kts = [(gt, sk) for gt in range(NT + ot + 1) for sk in range(8)]
                        npair = len(kts) // 2
                        for pi2 in range(npair):
                            pS_, B_pS = pS.next()
                            fns = []
                            for j2 in range(2):
                                gt, sk = kts[2 * pi2 + j2]
                                diag = (gt == NT + ot)
                                k0 = gt * TT + sk * 128
                                fns.append(lambda e, pS_=pS_, k0=k0, q0=q0, diag=diag, j2=j2: e.matmul(
                                    pS_[:, j2 * 512:(j2 + 1) * 512], lhsT=Kaug[hb][:, k0:k0 + 128], rhs=Qaug[hb][:, q0:q0 + 512],
                                    start=True, stop=not diag))
                                if diag:
                                    fns.append(lambda e, pS_=pS_, sk=sk, qh=qh, j2=j2: e.matmul(
                                        pS_[:, j2 * 512:(j2 + 1) * 512], lhsT=ident_b[:],
                                        rhs=CB[:, sk, 4 * qh:4 * qh + 4, :].rearrange("p a b -> p (a b)"), start=False, stop=True))
                            S.op("pe", fns, r=[B_K[hb], B_Q[hb], B_Qs[hb], B_st, B_const], w=[B_pS])
                            pi_ = step[0] % 3
                            S.op("act", lambda e, pS_=pS_, pi_=pi_: e.activation(out=pTs[pi_][:], in_=pS_[:, :], func=AF.Exp),
                                 r=[B_pS], w=[B_pTs[pi_]])

                            def pv(acc=acc, B_acc=B_acc, pi2=pi2, pi_=pi_, kts=kts, npair=npair, hb=hb):
                                fl = []
                                for j2 in range(2):
                                    gt, sk = kts[2 * pi2 + j2]
                                    fl.append(lambda e, gt=gt, sk=sk, j2=j2: e.matmul(
                                        acc[:, :], lhsT=Vh[hb][:, gt * 8 + sk, :], rhs=pTs[pi_][:, j2 * 512:(j2 + 1) * 512],
                                        start=(pi2 == 0 and j2 == 0), stop=(pi2 == npair - 1 and j2 == 1)))
                                S.op("pe", fl, r=[B_V[hb], B_pTs[pi_]], w=[B_acc])
                            sched(step[0] + LAG, pv)
                            flush(step[0])
                            step[0] += 1

                        def tail1(acc=acc, B_acc=B_acc):
                            S.op("dve", lambda e: e.reciprocal(out=rrow[64:65, :], in_=acc[64:65, :]), r=[B_acc], w=[B_rrow])

                        def tail2(acc=acc, B_acc=B_acc, q0=q0):
                            pb, B_pb = pG.next()
                            S.op("pe", lambda e: e.matmul(pb[0:64, :], lhsT=ones_f[64:65, 0:64], rhs=rrow[64:65, :], start=True, stop=True),
                                 r=[B_rrow, B_const], w=[B_pb])
                            S.op("dve", lambda e: e.tensor_copy(out=bcs[0:64, :], in_=pb[0:64, :]), r=[B_pb], w=[B_bcs])
                            S.op("dve", lambda e: e.tensor_tensor(out=oT[0:64, q0:q0 + 512], in0=acc[0:64, :], in1=bcs[0:64, :], op=ALU.mult),
                                 r=[B_acc, B_bcs], w=[B_oT])
                        sched(step[0] + LAG + 1, tail1)
                        sched(step[0] + LAG + 4, tail2)
                flush(step[0] + LAG + 4)
                S.dma("pool", scr_o[cb, h2 * 64:(h2 + 1) * 64, :], oT[0:64, :], B_oT, False)
            flush(10 ** 9)
            S.barrier()
            S.release(B_K + B_Q + B_V + [B_oT] + B_cvA + B_cvB)
        ps12.close()


        if upto < 5:
            S.barrier()
            return nc, dbg

        def post_norm_residual(py2, B_py2, xres, B_xres, s_loc, bc, ssq2, rs2, B_ssq2, B_rs2, junkf, B_junkf):
            for nh in range(2):
                S.op("act", lambda e: e.activation(out=junkf[:], in_=py2[nh][:, :], func=AF.Square, accum_out=ssq2[:, nh:nh + 1]),
                     r=[B_py2[nh]], w=[B_junkf, B_ssq2])
            S.op("dve", lambda e: e.tensor_tensor(out=rs2[:, 0:1], in0=ssq2[:, 0:1], in1=ssq2[:, 1:2], op=ALU.add), r=[B_ssq2], w=[B_rs2])
            S.op("act", lambda e: e.activation(out=rs2[:, 1:2], in_=rs2[:, 0:1], func=AF.Sqrt, scale=1.0 / D, bias=EPS), r=[B_rs2], w=[B_rs2])
            S.op("dve", lambda e: e.reciprocal(out=rs2[:, 2:3], in_=rs2[:, 1:2]), r=[B_rs2], w=[B_rs2])
            for nh in range(2):
                S.op("dve", lambda e: e.scalar_tensor_tensor(out=junkf[:, 0:512] if False else py2_sb[nh][:], in0=py2[nh][:, :], scalar=rs2[:, 2:3],
                                                             in1=bc[:, nh * 512:(nh + 1) * 512], op0=ALU.mult, op1=ALU.mult),
                     r=[B_py2[nh], B_rs2, B_mod], w=[B_py2sb[nh]])
                S.op("dve", lambda e: e.tensor_tensor(out=xres[:, s_loc, nh * 512:(nh + 1) * 512], in0=xres[:, s_loc, nh * 512:(nh + 1) * 512],
                                                       in1=py2_sb[nh][:], op=ALU.add), r=[B_py2sb[nh]], w=[B_xres])

        with ExitStack() as ps:
            Wga = sbt(ps, "Wga", [128, 4, D], BF16)
            Wgb = sbt(ps, "Wgb", [128, 4, D], BF16)
            Wao = sbt(ps, "Wao", [128, 4, D], BF16)
            Wo = sbt(ps, "Wo", [128, 8, D], BF16)
            B_w3 = Buf("w3")
            for dst, src in ((Wga, scr_wga), (Wgb, scr_wgb), (Wao, scr_wao), (Wo, scr_wo)):
                S.dma("sp", dst[:], src, B_w3, True)
            zT3 = sbt(ps, "zT3", [128, 4, TT], BF16)
            oT3 = sbt(ps, "oT3", [128, 4, TT], BF16)
            gT3 = sbt(ps, "gT3", [128, 16, TT], BF16)
            x3 = sbt(ps, "x3", [128, 8, D], F32)
            mT = sbt(ps, "mT", [128, 8, TT], BF16)
            B_zT3, B_oT3, B_gT3, B_x3, B_mT = Buf("zT3"), Buf("oT3"), Buf("gT3"), Buf("x3"), Buf("mT")
            sg = [sbt(ps, "sg%d" % i, [128, 512], F32) for i in range(2)]
            B_sg = [Buf("sg0"), Buf("sg1")]
            ta = [sbt(ps, "ta%d" % i, [128, 512], BF16) for i in range(2)]
            tb_ = [sbt(ps, "tb%d" % i, [128, 512], BF16) for i in range(2)]
            B_ta, B_tb = [Buf("ta0"), Buf("ta1")], [Buf("tb0"), Buf("tb1")]
            gbs = [sbt(ps, "gbs%d" % i, [128, 512], BF16) for i in range(2)]
            B_gbs = [Buf("gbs0"), Buf("gbs1")]
            py2_sb = [sbt(ps, "py2sb%d" % i, [128, 512], F32) for i in range(2)]
            B_py2sb = [Buf("py2sb0"), Buf("py2sb1")]
            junkf = sbt(ps, "junkf", [128, 512], BF16)
            B_junkf = Buf("junkf")
            ssq2 = sbt(ps, "ssq2", [128, 2], F32)
            rs2 = sbt(ps, "rs2", [128, 3], F32)
            B_ssq2, B_rs2 = Buf("ssq2"), Buf("rs2")
            p3 = Ring([pst(ps, "p3_%d" % i, [128, 512], F32) for i in range(8)], "p3")
            xo_v3 = xo.rearrange("(t c s) d -> t c s d", c=128, s=8)
            out_v3 = out.rearrange("(t c s) d -> t c s d", c=128, s=8)
            it = 0
            for ot in range(NT):
                S.dma("sp", zT3[:], scr_z[:, :, ot * TT:(ot + 1) * TT].rearrange("b p n -> p b n"), B_zT3, True)
                S.dma("sp", oT3[:], scr_o[:, :, ot * TT:(ot + 1) * TT].rearrange("b p n -> p b n"), B_oT3, True)
                S.dma("sp", gT3[:], scr_g[:, :, ot * TT:(ot + 1) * TT].rearrange("b p n -> p b n"), B_gT3, True)
                S.dma("sp", x3[:], xo_v3[ot], B_x3, True)
                for ncb in range(8):
                    for hf in range(2):
                        sl = slice(hf * 512, (hf + 1) * 512)
                        i2 = it % 2
                        it += 1
                        pa, B_pa = p3.next()
                        pb, B_pb = p3.next()
                        pc, B_pc = p3.next()
                        S.op("pe", [(lambda e, k=k: e.matmul(pa[:, :], lhsT=Wga[:, k, ncb * 128:(ncb + 1) * 128], rhs=zT3[:, k, sl],
                                                             start=(k == 0), stop=(k == 3))) for k in range(4)], r=[B_w3, B_zT3], w=[B_pa])
                        S.op("pe", [(lambda e, k=k: e.matmul(pb[:, :], lhsT=Wgb[:, k, ncb * 128:(ncb + 1) * 128], rhs=zT3[:, k, sl],
                                                             start=(k == 0), stop=(k == 3))) for k in range(4)], r=[B_w3, B_zT3], w=[B_pb])
                        S.op("pe", [(lambda e, k=k: e.matmul(pc[:, :], lhsT=Wao[:, k, ncb * 128:(ncb + 1) * 128], rhs=oT3[:, k, sl],
                                                             start=(k == 0), stop=(k == 3))) for k in range(4)], r=[B_w3, B_oT3], w=[B_pc])
                        S.op("act", lambda e: e.activation(out=sg[i2][:], in_=pb[:, :], func=AF.Sigmoid), r=[B_pb], w=[B_sg[i2]])
                        S.op("dve", lambda e: e.tensor_tensor(out=sg[i2][:], in0=pa[:, :], in1=sg[i2][:], op=ALU.mult), r=[B_pa, B_sg[i2]], w=[B_sg[i2]])
                        S.op("dve", lambda e: e.tensor_tensor(out=ta[i2][:], in0=sg[i2][:], in1=gT3[:, ncb, sl], op=ALU.mult),
                             r=[B_sg[i2], B_gT3], w=[B_ta[i2]])
                        S.op("act", lambda e: e.activation(out=gbs[i2][:], in_=gT3[:, 8 + ncb, sl], func=AF.Sigmoid), r=[B_gT3], w=[B_gbs[i2]])
                        S.op("dve", lambda e: e.tensor_tensor(out=tb_[i2][:], in0=pc[:, :], in1=gbs[i2][:], op=ALU.mult),
                             r=[B_pc, B_gbs[i2]], w=[B_tb[i2]])
                        S.op("dve", lambda e: e.tensor_tensor(out=mT[:, ncb, sl], in0=ta[i2][:], in1=tb_[i2][:], op=ALU.add),
                             r=[B_ta[i2], B_tb[i2]], w=[B_mT])
                for s_ in range(8):
                    py2, B_py2 = [], []
                    for nh in range(2):
                        p_, B_p = p3.next()
                        S.op("pe", [(lambda e, k=k: e.matmul(p_[:, :], lhsT=mT[:, k, s_ * 128:(s_ + 1) * 128], rhs=Wo[:, k, nh * 512:(nh + 1) * 512],
                                                             start=(k == 0), stop=(k == 7))) for k in range(8)], r=[B_mT, B_w3], w=[B_p])
                        py2.append(p_)
                        B_py2.append(B_p)
                    post_norm_residual(py2, B_py2, x3, B_x3, s_, bc_m, ssq2, rs2, B_ssq2, B_rs2, junkf, B_junkf)
                S.dma("pool", out_v3[ot], x3[:], B_x3, False)
            S.barrier()
            S.release([B_w3, B_zT3, B_oT3, B_gT3, B_x3])

        if upto < 6:
            S.barrier()
            return nc, dbg

        with ExitStack() as ps:
            Wg = sbt(ps, "Wg", [128, 8, DFF], BF16)
            Wu = sbt(ps, "Wu", [128, 8, DFF], BF16)
            Wd = sbt(ps, "Wd", [128, NF, D], BF16)
            B_w4 = Buf("w4")
            for dst, src in ((Wg, scr_wg), (Wu, scr_wu), (Wd, scr_wd)):
                S.dma("sp", dst[:], src, B_w4, True)
            x4 = sbt(ps, "x4", [128, 4, D], F32)
            xn4 = sbt(ps, "xn4", [128, 4, D], BF16)
            hn4 = sbt(ps, "hn4", [128, 8, 512], BF16)
            aT = sbt(ps, "aT", [128, NF, 512], BF16)
            B_x4, B_xn4, B_hn4, B_aT = Buf("x4"), Buf("xn4"), Buf("hn4"), Buf("aT")
            sl4 = [sbt(ps, "sl4_%d" % i, [128, 512], BF16) for i in range(2)]
            B_sl4 = [Buf("sl4_0"), Buf("sl4_1")]
            py2_sb = [sbt(ps, "py4sb%d" % i, [128, 512], F32) for i in range(2)]
            B_py2sb = [Buf("py4sb0"), Buf("py4sb1")]
            junk4 = sbt(ps, "junk4", [128, D], BF16)
            junkf = sbt(ps, "junkf4", [128, 512], BF16)
            B_junk4, B_junkf = Buf("junk4"), Buf("junkf4")
            ssq4 = sbt(ps, "ssq4", [128, 4], F32)
            rstd4 = sbt(ps, "rstd4", [128, 4], F32)
            ssq2 = sbt(ps, "ssq2b", [128, 2], F32)
            rs2 = sbt(ps, "rs2b", [128, 3], F32)
            B_ssq4, B_rstd4, B_ssq2, B_rs2 = Buf("ssq4"), Buf("rstd4"), Buf("ssq2b"), Buf("rs2b")
            ptr4 = Ring([pst(ps, "ptr4_%d" % i, [128, 512], BF16) for i in range(2)], "ptr4")
            p4 = Ring([pst(ps, "p4_%d" % i, [128, 512], F32) for i in range(6)], "p4")
            out_v4 = out.rearrange("(t c s) d -> t c s d", c=128, s=8)
            it = 0
            for ot in range(NT):
                for sh in range(2):
                    S.dma("sp", x4[:], out_v4[ot][:, 4 * sh:4 * sh + 4, :], B_x4, True)
                    for s_ in range(4):
                        S.op("act", lambda e: e.activation(out=junk4[:], in_=x4[:, s_, :], func=AF.Square, accum_out=ssq4[:, s_:s_ + 1]),
                             r=[B_x4], w=[B_junk4, B_ssq4])
                    S.op("act", lambda e: e.activation(out=rstd4[:], in_=ssq4[:], func=AF.Sqrt, scale=1.0 / D, bias=EPS), r=[B_ssq4], w=[B_rstd4])
                    S.op("dve", lambda e: e.reciprocal(out=rstd4[:], in_=rstd4[:]), r=[B_rstd4], w=[B_rstd4])
                    for s_ in range(4):
                        S.op("dve",
                             lambda e: e.tensor_scalar(out=xn4[:, s_, :], in0=x4[:, s_, :], scalar1=rstd4[:, s_:s_ + 1], scalar2=None, op0=ALU.mult),
                             r=[B_x4, B_rstd4], w=[B_xn4])
                    for k in range(8):
                        pt, B_pt = ptr4.next()
                        S.op("pe", [(lambda e, s_=s_: e.transpose(out=pt[:, s_ * 128:(s_ + 1) * 128], in_=xn4[:, s_, k * 128:(k + 1) * 128],
                                                                  identity=ident_b[:])) for s_ in range(4)], r=[B_xn4, B_const], w=[B_pt])
                        if k % 2 == 0:
                            S.op("dve", lambda e: e.tensor_scalar(out=hn4[:, k, :], in0=pt[:, :], scalar1=gmod_f[:, k:k + 1], scalar2=sh_f[:, k:k + 1],
                                                                  op0=ALU.mult, op1=ALU.add), r=[B_pt, B_mod], w=[B_hn4])
                        else:
                            S.op("act", lambda e: e.activation(out=hn4[:, k, :], in_=pt[:, :], func=AF.Identity, scale=gmod_f[:, k:k + 1],
                                                               bias=sh_f[:, k:k + 1]), r=[B_pt, B_mod], w=[B_hn4])
                    for f in range(NF):
                        i2 = it % 2
                        it += 1
                        pg_, B_pg = p4.next()
                        pu_, B_pu = p4.next()
                        S.op("pe", [(lambda e, k=k: e.matmul(pg_[:, :], lhsT=Wg[:, k, f * 128:(f + 1) * 128], rhs=hn4[:, k, :],
                                                             start=(k == 0), stop=(k == 7))) for k in range(8)], r=[B_w4, B_hn4], w=[B_pg])
                        S.op("pe", [(lambda e, k=k: e.matmul(pu_[:, :], lhsT=Wu[:, k, f * 128:(f + 1) * 128], rhs=hn4[:, k, :],
                                                             start=(k == 0), stop=(k == 7))) for k in range(8)], r=[B_w4, B_hn4], w=[B_pu])
                        S.op("act", lambda e: e.activation(out=sl4[i2][:], in_=pg_[:, :], func=AF.Silu), r=[B_pg], w=[B_sl4[i2]])
                        S.op("dve", lambda e: e.tensor_tensor(out=aT[:, f, :], in0=pu_[:, :], in1=sl4[i2][:], op=ALU.mult),
                             r=[B_pu, B_sl4[i2]], w=[B_aT])
                    for s_ in range(4):
                        py2, B_py2 = [], []
                        for nh in range(2):
                            p_, B_p = p4.next()
                            S.op("pe", [(lambda e, f=f: e.matmul(p_[:, :], lhsT=aT[:, f, s_ * 128:(s_ + 1) * 128], rhs=Wd[:, f, nh * 512:(nh + 1) * 512],
                                                                 start=(f == 0), stop=(f == NF - 1))) for f in range(NF)], r=[B_aT, B_w4], w=[B_p])
                            py2.append(p_)
                            B_py2.append(B_p)
                        post_norm_residual(py2, B_py2, x4, B_x4, s_, bc_f, ssq2, rs2, B_ssq2, B_rs2, junkf, B_junkf)
                    S.dma("pool", out_v4[ot][:, 4 * sh:4 * sh + 4, :], x4[:], B_x4, False)
            S.barrier()

        S.barrier()
    return nc, dbg


def _prep_inputs(inputs):
    f = lambda a: np.ascontiguousarray(np.asarray(a, dtype=np.float32))
    x = f(inputs["x"])
    c = f(inputs["c"])
    shared = {}
    shared["w_ada"] = f(inputs["w_ada"][0])
    shared["b_ada"] = f(inputs["b_ada"][0]).reshape(1, -1)
    shared["g_pre_mix_c"] = f(inputs["g_pre_mix"][0].reshape(8, 128).T)
    shared["g_pre_ffn_c"] = f(inputs["g_pre_ffn"][0].reshape(8, 128).T)
    shared["g_post_mix_r"] = f(inputs["g_post_mix"][0]).reshape(1, -1)
    shared["g_post_ffn_r"] = f(inputs["g_post_ffn"][0]).reshape(1, -1)
    shared["w_in"] = f(inputs["w_in"][0])
    st2 = lambda a: f(np.concatenate([a, a], axis=0))
    shared["s_are"] = st2(np.asarray(inputs["ssm_a_re"][0]).T)
    shared["s_aim"] = st2(np.asarray(inputs["ssm_a_im"][0]).T)
    shared["s_ldt"] = f(np.broadcast_to(np.asarray(inputs["ssm_log_dt"][0])[None, :], (128, 32)))
    shared["s_bre"] = st2(np.asarray(inputs["ssm_b_re"][0]).transpose(1, 0, 2))
    shared["s_bim"] = st2(np.asarray(inputs["ssm_b_im"][0]).transpose(1, 0, 2))
    shared["s_cre"] = st2(np.asarray(inputs["ssm_c_re"][0]).transpose(2, 0, 1))
    shared["s_cim"] = st2(np.asarray(inputs["ssm_c_im"][0]).transpose(2, 0, 1))
    shared["s_dcol"] = f(np.tile(np.asarray(inputs["ssm_d"][0]).T, (8, 1)))
    for k in ("w_glu_a", "w_glu_b", "w_attn_out", "w_out", "w_ff_gate", "w_ff_up", "w_ff_down"):
        shared[k] = f(inputs[k][0])
    in_maps = []
    for core in range(8):
        b, h = core // 2, core % 2
        m = dict(shared)
        m["xo"] = f(x[b, h * TOK:(h + 1) * TOK])
        m["xp"] = f(x[b, 0:TOK])
        m["flag"] = np.full((128, 1), float(h), np.float32)
        m["c_col"] = f(c[b].reshape(8, 128).T)
        in_maps.append(m)
    return in_maps


_CACHE = {}


def kernel(**inputs):
    in_maps = _prep_inputs(inputs)
    if "nc" not in _CACHE:
        _CACHE["nc"] = build_program()[0]
    nc = _CACHE["nc"]
    res = run_bass_kernel_spmd(nc, in_maps, core_ids=list(range(8)))
    outp = np.empty((4, 8192, D), np.float32)
    for core in range(8):
        b, h = core // 2, core % 2
        outp[b, h * TOK:(h + 1) * TOK] = res.results[core]["out"]
    return outp
```

```python
import numpy as np
from contextlib import ExitStack
import concourse.bass as bass
import concourse.mybir as mybir
from concourse.bass_utils import run_bass_kernel_spmd

F32 = mybir.dt.float32
BF16 = mybir.dt.bfloat16
AF = mybir.ActivationFunctionType
ALU = mybir.AluOpType
AX = mybir.AxisListType

D = 1024
TOK = 4096
TT = 1024
NT = TOK // TT
DFF = 2816
NF = DFF // 128
EPS = 1e-6
NEG = -30000.0


class DSem:
    def __init__(self, sem):
        self.sem = sem
        self.cnt = 0


class Buf:
    def __init__(self, name):
        self.name = name
        self.w = {}
        self.r = {}
        self.dsem = None


class Sched:
    def __init__(self, nc, es):
        self.nc = nc
        self.es = es
        self.E = dict(pe=nc.tensor, act=nc.scalar, dve=nc.vector, pool=nc.gpsimd, sp=nc.sync)
        self.sem = {k: es.enter_context(nc.semaphore("s_" + k)) for k in ("pe", "act", "dve", "pool")}
        self.cnt = {k: 0 for k in self.sem}
        self.seen = {k: {} for k in self.E}
        self.dsems = []
        self.free_dsems = []

    def _semof(self, key):
        return self.sem[key] if isinstance(key, str) else key.sem

    def _cntof(self, key):
        return self.cnt[key] if isinstance(key, str) else key.cnt

    def get_dsem(self, buf):
        if buf.dsem is None:
            if self.free_dsems:
                buf.dsem = self.free_dsems.pop()
            else:
                buf.dsem = DSem(self.es.enter_context(self.nc.semaphore("d%d" % len(self.dsems))))
                self.dsems.append(buf.dsem)
        return buf.dsem

    def release(self, bufs):
        for b in bufs:
            if b.dsem is not None:
                self.free_dsems.append(b.dsem)
                b.dsem = None

    def _waits(self, eng, r, w):
        need = {}
        for b in r:
            for k, v in b.w.items():
                if need.get(k, 0) < v:
                    need[k] = v
        for b in w:
            for dd in (b.w, b.r):
                for k, v in dd.items():
                    if need.get(k, 0) < v:
                        need[k] = v
        for k, v in need.items():
            if eng == "pe" and k == "pe":
                continue
            if self.seen[eng].get(k, 0) >= v:
                continue
            self.seen[eng][k] = v
            self.E[eng].wait_ge(self._semof(k), v)

    def op(self, eng, fns, r=(), w=()):
        if callable(fns):
            fns = [fns]
        self._waits(eng, r, w)
        ins = None
        for f in fns:
            ins = f(self.E[eng])
        self.cnt[eng] += 1
        ins.then_inc(self.sem[eng], 1)
        c = self.cnt[eng]
        for b in r:
            b.r[eng] = c
        for b in w:
            b.w[eng] = c
            b.r = {}

    def dma(self, eng, out, in_, sb, load, extra_r=(), extra_w=(), **kw):
        ds = self.get_dsem(sb)
        r = list(extra_r) + ([] if load else [sb])
        w = list(extra_w) + ([sb] if load else [])
        self._waits(eng, r, w)
        ins = self.E[eng].dma_start(out=out, in_=in_, **kw)
        ds.cnt += 16
        ins.then_inc(ds.sem, 16)
        for b in r:
            b.r[ds] = ds.cnt
        for b in w:
            b.w[ds] = ds.cnt
            b.r = {}

    def barrier(self):
        for eng in self.E:
            for k in self.sem:
                if k == eng:
                    continue
                v = self.cnt[k]
                if v > 0 and self.seen[eng].get(k, 0) < v:
                    self.seen[eng][k] = v
                    self.E[eng].wait_ge(self.sem[k], v)
            for ds in self.dsems:
                if ds.cnt > 0 and self.seen[eng].get(ds, 0) < ds.cnt:
                    self.seen[eng][ds] = ds.cnt
                    self.E[eng].wait_ge(ds.sem, ds.cnt)


def build_program(debug=False, upto=99):
    nc = bass.Bass("TRN2", target_bir_lowering=False)
    dram = {}

    def din(name, shape, dt=F32):
        dram[name] = nc.dram_tensor(name, list(shape), dt, kind="ExternalInput").ap()
        return dram[name]

    def dscr(name, shape, dt):
        return nc.dram_tensor(name, list(shape), dt, kind="Internal").ap()

    xo = din("xo", [TOK, D])
    xp = din("xp", [TOK, D])
    flag = din("flag", [128, 1])
    c_col = din("c_col", [128, 8])
    w_ada = din("w_ada", [D, 6 * D])
    b_ada = din("b_ada", [1, 6 * D])
    g_pre_mix_c = din("g_pre_mix_c", [128, 8])
    g_pre_ffn_c = din("g_pre_ffn_c", [128, 8])
    g_post_mix_r = din("g_post_mix_r", [1, D])
    g_post_ffn_r = din("g_post_ffn_r", [1, D])
    w_in = din("w_in", [D, 4096])
    s_are = din("s_are", [128, 32])
    s_aim = din("s_aim", [128, 32])
    s_ldt = din("s_ldt", [128, 32])
    s_bre = din("s_bre", [128, 32, 16])
    s_bim = din("s_bim", [128, 32, 16])
    s_cre = din("s_cre", [128, 32, 16])
    s_cim = din("s_cim", [128, 32, 16])
    s_dcol = din("s_dcol", [128, 32])
    w_glu_a = din("w_glu_a", [512, D])
    w_glu_b = din("w_glu_b", [512, D])
    w_attn_out = din("w_attn_out", [512, D])
    w_out = din("w_out", [D, D])
    w_ff_gate = din("w_ff_gate", [D, DFF])
    w_ff_up = din("w_ff_up", [D, DFF])
    w_ff_down = din("w_ff_down", [DFF, D])
    out = nc.dram_tensor("out", [TOK, D], F32, kind="ExternalOutput").ap()
    dbg = {}

    def dbg_out(name, shape, dt=F32):
        dbg[name] = nc.dram_tensor("dbg_" + name, list(shape), dt, kind="ExternalOutput").ap()
        return dbg[name]

    scr_hn = dscr("scr_hn", [NT, 128, 8, TT], BF16)
    scr_kt = dscr("scr_kt", [4, 128, 2 * TOK], BF16)
    scr_v = dscr("scr_v", [2 * NT, 8, 128, 512], BF16)
    scr_u = dscr("scr_u", [2 * NT, 128, 32, 8, 16], BF16)
    scr_q = dscr("scr_q", [4, 128, TOK], BF16)
    scr_g = dscr("scr_g", [16, 128, TOK], BF16)
    scr_z = dscr("scr_z", [4, 128, TOK], BF16)
    scr_o = dscr("scr_o", [4, 128, TOK], BF16)
    scr_W1t = dscr("scr_W1t", [128, 32, 128], BF16)
    scr_Ktoep = dscr("scr_Ktoep", [128, 32, 128], BF16)
    scr_Ctab = dscr("scr_Ctab", [128, 32, 128], BF16)
    scr_cosT = dscr("scr_cosT", [128, 32, 64], F32)
    scr_sinT = dscr("scr_sinT", [128, 32, 64], F32)
    scr_rho = dscr("scr_rho", [128, 32], F32)
    scr_Jt = dscr("scr_Jt", [128, 128], F32)
    scr_wga = dscr("scr_wga", [128, 4, D], BF16)
    scr_wgb = dscr("scr_wgb", [128, 4, D], BF16)
    scr_wao = dscr("scr_wao", [128, 4, D], BF16)
    scr_wo = dscr("scr_wo", [128, 8, D], BF16)
    scr_wg = dscr("scr_wg", [128, 8, DFF], BF16)
    scr_wu = dscr("scr_wu", [128, 8, DFF], BF16)
    scr_wd = dscr("scr_wd", [128, NF, D], BF16)

    with ExitStack() as es:
        S = Sched(nc, es)

        def sbt(stack, name, shape, dt):
            return stack.enter_context(nc.sbuf_tensor(name, list(shape), dt))

        def pst(stack, name, shape, dt):
            return stack.enter_context(nc.psum_tensor(name, list(shape), dt))

        class Ring:
            def __init__(self, tiles, name):
                self.t = [(t, Buf("%s%d" % (name, i))) for i, t in enumerate(tiles)]
                self.i = 0

            def next(self):
                x = self.t[self.i % len(self.t)]
                self.i += 1
                return x

        ident_f = sbt(es, "ident_f", [128, 128], F32)
        ident_b = sbt(es, "ident_b", [128, 128], BF16)
        ones_f = sbt(es, "ones_f", [128, 128], F32)
        gmod_m = sbt(es, "gmod_m", [128, 8], F32)
        sh_m = sbt(es, "sh_m", [128, 8], F32)
        gmod_f = sbt(es, "gmod_f", [128, 8], F32)
        sh_f = sbt(es, "sh_f", [128, 8], F32)
        bc_m = sbt(es, "bc_m", [128, D], F32)
        bc_f = sbt(es, "bc_f", [128, D], F32)
        flag_sb = sbt(es, "flag_sb", [128, 1], F32)
        kmean = sbt(es, "kmean", [128, 4, 32], BF16)
        B_const = Buf("const")
        B_mod = Buf("mod")
        B_kmean = Buf("kmean")

        S.op("pool", lambda e: e.memset(ident_f[:], 1.0), w=[B_const])
        S.op("pool", lambda e: e.affine_select(out=ident_f[:], in_=ident_f[:], pattern=[[-1, 128]],
                                                compare_op=ALU.is_equal, fill=0.0, base=0, channel_multiplier=1),
             w=[B_const])
        S.op("pool", lambda e: e.memset(ones_f[:], 1.0), w=[B_const])
        S.op("dve", lambda e: e.tensor_copy(out=ident_b[:], in_=ident_f[:]), r=[B_const], w=[B_const])
        S.dma("sp", flag_sb[:], flag, B_const, True)

        with ExitStack() as ps:
            cc = sbt(ps, "cc", [128, 8], F32)
            modrow = sbt(ps, "modrow", [1, 6 * D], F32)
            cact = sbt(ps, "cact", [128, 8], F32)
            wab = [sbt(ps, "wab%d" % i, [128, 8, 256], F32) for i in range(2)]
            gpm = sbt(ps, "gpm", [128, 8], F32)
            gpf = sbt(ps, "gpf", [128, 8], F32)
            grow = sbt(ps, "grow", [1, 2 * D], F32)
            rprod = sbt(ps, "rprod", [1, 2 * D], F32)
            pr = [pst(ps, "p0r%d" % i, [128, 512], F32) for i in range(2)]
            pc = pst(ps, "p0c", [128, 512], F32)
            B_cc, B_cact, B_brow, B_g = Buf("cc"), Buf("cact"), Buf("brow"), Buf("g")
            B_wab = [Buf("wab0"), Buf("wab1")]
            B_pr = [Buf("pr0"), Buf("pr1")]
            B_pc = Buf("pc")
            B_rp = Buf("rprod")
            S.dma("sp", cc[:], c_col, B_cc, True)
            S.dma("sp", modrow[:], b_ada, B_mod, True)
            S.dma("sp", gpm[:], g_pre_mix_c, B_g, True)
            S.dma("sp", gpf[:], g_pre_ffn_c, B_g, True)
            S.dma("sp", grow[:, 0:D], g_post_mix_r, B_g, True)
            S.dma("sp", grow[:, D:2 * D], g_post_ffn_r, B_g, True)
            S.op("act", lambda e: e.activation(out=cact[:], in_=cc[:], func=AF.Silu), r=[B_cc], w=[B_cact])
            wada_v = w_ada.rearrange("(k p) n -> p k n", p=128)
            W1t = sbt(ps, "W1t0", [128, 32, 128], BF16)
            Ktoep = sbt(ps, "Ktoep0", [128, 32, 128], BF16)
            Ctab = sbt(ps, "Ctab0", [128, 32, 128], BF16)
            cosT = sbt(ps, "cosT0", [128, 32, 64], F32)
            sinT = sbt(ps, "sinT0", [128, 32, 64], F32)
            rho = sbt(ps, "rho0", [128, 32], F32)
            Jt = sbt(ps, "Jt0", [128, 128], F32)
            B_tab = Buf("tab0")
            pss = Ring([pst(ps, "pss0_%d" % i, [128, 512], F32) for i in range(3)], "pss0")

            def T(eng, fn):
                S.op(eng, fn, r=[B_tab, B_const], w=[B_tab])

            def bc3(ap2, n):
                return ap2.unsqueeze(2).to_broadcast([ap2.shape[0], ap2.shape[1], n])

            def tab_gen():
                f32t = lambda nm, shp: sbt(ps, nm, shp, F32)
                are, aim, ldt = f32t("are", [128, 32]), f32t("aim", [128, 32]), f32t("ldt", [128, 32])
                bre, bim = f32t("bre", [128, 32, 16]), f32t("bim", [128, 32, 16])
                cre, cim = f32t("cre", [128, 32, 16]), f32t("cim", [128, 32, 16])
                dcol = f32t("dcol", [128, 32])
                for t_, src in ((are, s_are), (aim, s_aim), (ldt, s_ldt), (bre, s_bre), (bim, s_bim), (cre, s_cre),
                                (cim, s_cim), (dcol, s_dcol)):
                    S.dma("sp", t_[:], src, B_tab, True)
                dtt, xr, xi, mag = f32t("dtt", [128, 32]), f32t("xr", [128, 32]), f32t("xi", [128, 32]), f32t("mag", [128, 32])
                ys, sn, cs = f32t("ys", [128, 32]), f32t("sn", [128, 32]), f32t("cs", [128, 32])
                t1, t2, t3 = f32t("t1", [128, 32]), f32t("t2", [128, 32]), f32t("t3", [128, 32])
                cor, coi = f32t("cor", [128, 32]), f32t("coi", [128, 32])
                PWr, PWi = f32t("PWr", [128, 9, 32]), f32t("PWi", [128, 9, 32])
                NPr, NPi = f32t("NPr", [128, 8, 32]), f32t("NPi", [128, 8, 32])
                bbr, bbi = f32t("bbr", [128, 32, 16]), f32t("bbi", [128, 32, 16])
                u1, u2 = f32t("u1", [128, 32, 16]), f32t("u2", [128, 32, 16])
                BB = f32t("BB", [128, 32, 8, 16])
                WW = f32t("WW", [128, 32, 8, 16])
                CC = f32t("CC", [128, 32, 9, 16])
                maskLT = f32t("maskLT", [128, 8, 16])
                pi = float(np.pi)
                T("act", lambda e: e.activation(out=dtt[:], in_=ldt[:], func=AF.Exp))
                T("dve", lambda e: e.tensor_tensor(out=xr[:], in0=dtt[:], in1=are[:], op=ALU.mult))
                T("dve", lambda e: e.tensor_tensor(out=xi[:], in0=dtt[:], in1=aim[:], op=ALU.mult))
                T("act", lambda e: e.activation(out=mag[:], in_=xr[:], func=AF.Exp))
                T("act", lambda e: e.activation(out=rho[:], in_=xr[:], func=AF.Exp, scale=8.0))
                MAGIC = 12582912.0

                def sin_of(dst, src_ap, shift):
                    T("dve", lambda e: e.tensor_scalar(out=t1[:], in0=src_ap, scalar1=shift, scalar2=None, op0=ALU.add))
                    T("dve", lambda e: e.tensor_scalar(out=t2[:], in0=t1[:], scalar1=1.0 / (2 * pi), scalar2=MAGIC,
                                                       op0=ALU.mult, op1=ALU.add))
                    T("dve", lambda e: e.tensor_scalar(out=t2[:], in0=t2[:], scalar1=-MAGIC, scalar2=None, op0=ALU.add))
                    T("dve", lambda e: e.scalar_tensor_tensor(out=ys[:], in0=t2[:], scalar=-2 * pi, in1=t1[:],
                                                              op0=ALU.mult, op1=ALU.add))
                    T("dve", lambda e: e.tensor_scalar(out=ys[:], in0=ys[:], scalar1=-3.14159, scalar2=3.14159,
                                                       op0=ALU.max, op1=ALU.min))
                    T("act", lambda e: e.activation(out=dst, in_=ys[:], func=AF.Sin))

                sin_of(sn[:], xi[:], 0.0)
                sin_of(cs[:], xi[:], 0.5 * pi)
                yield
                T("dve", lambda e: e.memset(PWr[:, 0, :], 1.0))
                T("dve", lambda e: e.memset(PWi[:, 0, :], 0.0))
                T("dve", lambda e: e.memset(NPr[:, 0, :], 1.0))
                T("dve", lambda e: e.memset(NPi[:, 0, :], 0.0))
                T("dve", lambda e: e.tensor_tensor(out=PWr[:, 1, :], in0=mag[:], in1=cs[:], op=ALU.mult))
                T("dve", lambda e: e.tensor_tensor(out=PWi[:, 1, :], in0=mag[:], in1=sn[:], op=ALU.mult))
                abr, abi = PWr[:, 1, :], PWi[:, 1, :]
                T("dve", lambda e: e.tensor_tensor(out=t1[:], in0=are[:], in1=are[:], op=ALU.mult))
                T("dve", lambda e: e.tensor_tensor(out=t2[:], in0=aim[:], in1=aim[:], op=ALU.mult))
                T("dve", lambda e: e.tensor_tensor(out=t1[:], in0=t1[:], in1=t2[:], op=ALU.add))
                T("dve", lambda e: e.reciprocal(out=t1[:], in_=t1[:]))
                T("dve", lambda e: e.tensor_scalar(out=t2[:], in0=abr, scalar1=-1.0, scalar2=None, op0=ALU.add))
                T("dve", lambda e: e.tensor_tensor(out=cor[:], in0=t2[:], in1=are[:], op=ALU.mult))
                T("dve", lambda e: e.tensor_tensor(out=t3[:], in0=abi, in1=aim[:], op=ALU.mult))
                T("dve", lambda e: e.tensor_tensor(out=cor[:], in0=cor[:], in1=t3[:], op=ALU.add))
                T("dve", lambda e: e.tensor_tensor(out=cor[:], in0=cor[:], in1=t1[:], op=ALU.mult))
                T("dve", lambda e: e.tensor_tensor(out=coi[:], in0=abi, in1=are[:], op=ALU.mult))
                T("dve", lambda e: e.tensor_tensor(out=t3[:], in0=t2[:], in1=aim[:], op=ALU.mult))
                T("dve", lambda e: e.tensor_tensor(out=coi[:], in0=coi[:], in1=t3[:], op=ALU.subtract))
                T("dve", lambda e: e.tensor_tensor(out=coi[:], in0=coi[:], in1=t1[:], op=ALU.mult))
                T("dve", lambda e: e.tensor_tensor(out=bbr[:], in0=bre[:], in1=bc3(cor[:], 16), op=ALU.mult))
                T("dve", lambda e: e.tensor_tensor(out=u1[:], in0=bim[:], in1=bc3(coi[:], 16), op=ALU.mult))
                T("dve", lambda e: e.tensor_tensor(out=bbr[:], in0=bbr[:], in1=u1[:], op=ALU.subtract))
                T("dve", lambda e: e.tensor_tensor(out=bbi[:], in0=bim[:], in1=bc3(cor[:], 16), op=ALU.mult))
                T("dve", lambda e: e.tensor_tensor(out=u1[:], in0=bre[:], in1=bc3(coi[:], 16), op=ALU.mult))
                T("dve", lambda e: e.tensor_tensor(out=bbi[:], in0=bbi[:], in1=u1[:], op=ALU.add))
                T("dve", lambda e: e.tensor_tensor(out=t1[:], in0=abr, in1=abr, op=ALU.mult))
                T("dve", lambda e: e.tensor_tensor(out=t2[:], in0=abi, in1=abi, op=ALU.mult))
                T("dve", lambda e: e.tensor_tensor(out=t1[:], in0=t1[:], in1=t2[:], op=ALU.add))
                T("dve", lambda e: e.reciprocal(out=t1[:], in_=t1[:]))
                T("dve", lambda e: e.tensor_tensor(out=NPr[:, 1, :], in0=abr, in1=t1[:], op=ALU.mult))
                T("dve", lambda e: e.scalar_tensor_tensor(out=NPi[:, 1, :], in0=abi, scalar=-1.0, in1=t1[:], op0=ALU.mult, op1=ALU.mult))

                def cmul(orr, oii, ar_, ai_, br_, bi_, tA, tB):
                    T("dve", lambda e: e.tensor_tensor(out=tA, in0=ar_, in1=br_, op=ALU.mult))
                    T("dve", lambda e: e.tensor_tensor(out=tB, in0=ai_, in1=bi_, op=ALU.mult))
                    T("dve", lambda e: e.tensor_tensor(out=tA, in0=tA, in1=tB, op=ALU.subtract))
                    T("dve", lambda e: e.tensor_tensor(out=tB, in0=ar_, in1=bi_, op=ALU.mult))
                    T("dve", lambda e: e.tensor_tensor(out=oii, in0=ai_, in1=br_, op=ALU.mult))
                    T("dve", lambda e: e.tensor_tensor(out=oii, in0=oii, in1=tB, op=ALU.add))
                    T("dve", lambda e: e.tensor_copy(out=orr, in_=tA))

                for k in range(2, 9):
                    cmul(PWr[:, k, :], PWi[:, k, :], PWr[:, k - 1, :], PWi[:, k - 1, :], abr, abi, t1[:], t2[:])
                    yield
                for k in range(2, 8):
                    cmul(NPr[:, k, :], NPi[:, k, :], NPr[:, k - 1, :], NPi[:, k - 1, :], NPr[:, 1, :], NPi[:, 1, :], t1[:], t2[:])
                    yield
                lo, hi = slice(0, 64), slice(64, 128)
                for s_ in range(8):
                    for (dst, pr_, pi_) in ((BB, NPr[:, s_, :], NPi[:, s_, :]), (WW, PWr[:, 7 - s_, :], PWi[:, 7 - s_, :])):
                        T("dve", lambda e: e.tensor_tensor(out=u1[lo], in0=bbr[lo], in1=bc3(pr_[lo], 16), op=ALU.mult))
                        T("dve", lambda e: e.tensor_tensor(out=u2[lo], in0=bbi[lo], in1=bc3(pi_[lo], 16), op=ALU.mult))
                        T("dve", lambda e: e.tensor_tensor(out=dst[lo, :, s_, :], in0=u1[lo], in1=u2[lo], op=ALU.subtract))
                        T("dve", lambda e: e.tensor_tensor(out=u1[hi], in0=bbi[hi], in1=bc3(pr_[hi], 16), op=ALU.mult))
                        T("dve", lambda e: e.tensor_tensor(out=u2[hi], in0=bbr[hi], in1=bc3(pi_[hi], 16), op=ALU.mult))
                        T("dve", lambda e: e.tensor_tensor(out=dst[hi, :, s_, :], in0=u1[hi], in1=u2[hi], op=ALU.add))
                        yield
                for k in range(9):
                    pr_, pi_ = PWr[:, k, :], PWi[:, k, :]
                    T("dve", lambda e: e.tensor_tensor(out=u1[lo], in0=cre[lo], in1=bc3(pr_[lo], 16), op=ALU.mult))
                    T("dve", lambda e: e.tensor_tensor(out=u2[lo], in0=cim[lo], in1=bc3(pi_[lo], 16), op=ALU.mult))
                    T("dve", lambda e: e.tensor_tensor(out=CC[lo, :, k, :], in0=u1[lo], in1=u2[lo], op=ALU.subtract))
                    T("dve", lambda e: e.tensor_tensor(out=u1[hi], in0=cre[hi], in1=bc3(pi_[hi], 16), op=ALU.mult))
                    T("dve", lambda e: e.tensor_tensor(out=u2[hi], in0=cim[hi], in1=bc3(pr_[hi], 16), op=ALU.mult))
                    T("dve", lambda e: e.scalar_tensor_tensor(out=CC[hi, :, k, :], in0=u1[hi], scalar=-1.0, in1=u2[hi],
                                                              op0=ALU.mult, op1=ALU.subtract))
                    yield
                T("dve", lambda e: e.tensor_copy(out=Ctab[:].rearrange("p g (k c) -> p g k c", k=8), in_=CC[:, :, 1:9, :]))
                T("pool", lambda e: e.memset(maskLT[:], 1.0))
                T("pool", lambda e: e.affine_select(out=maskLT[:], in_=maskLT[:], pattern=[[16, 8], [0, 16]],
                                                    compare_op=ALU.is_ge, fill=0.0, base=15, channel_multiplier=-1))
                jt2 = f32t("jt2", [128, 128])
                T("pool", lambda e: e.memset(Jt[:], 1.0))
                T("pool", lambda e: e.affine_select(out=Jt[:], in_=Jt[:], pattern=[[1, 128]], compare_op=ALU.is_equal,
                                                    fill=0.0, base=-64, channel_multiplier=-1))
                T("pool", lambda e: e.memset(jt2[:], 1.0))
                T("pool", lambda e: e.affine_select(out=jt2[:], in_=jt2[:], pattern=[[1, 128]], compare_op=ALU.is_equal,
                                                    fill=0.0, base=64, channel_multiplier=-1))
                T("dve", lambda e: e.tensor_tensor(out=Jt[:], in0=Jt[:], in1=jt2[:], op=ALU.subtract))
                ktmp = f32t("ktmp", [128, 128])
                for g in range(32):
                    pk, B_pk = pss.next()
                    S.op("pe", lambda e: e.matmul(pk[:, 0:128], lhsT=BB[:, g, :, :].rearrange("p a b -> p (a b)"), rhs=CC[:, g, 0:8, :].rearrange("p a b -> p (a b)"), start=True, stop=True),
                         r=[B_tab], w=[B_pk])
                    S.op("dve", lambda e: e.tensor_tensor(out=ktmp[:], in0=pk[:, 0:128], in1=maskLT[:].rearrange("p a b -> p (a b)"),
                                                          op=ALU.mult), r=[B_pk, B_tab], w=[B_tab])
                    T("dve", lambda e: e.scalar_tensor_tensor(out=Ktoep[:, g, :], in0=ident_f[:], scalar=dcol[:, g:g + 1],
                                                              in1=ktmp[:], op0=ALU.mult, op1=ALU.add))
                    pw, B_pw = pss.next()
                    S.op("pe", lambda e: e.transpose(out=pw[:, 0:128], in_=WW[:, g, :, :].rearrange("p a b -> p (a b)"), identity=ident_f[:]),
                         r=[B_tab, B_const], w=[B_pw])
                    S.op("act", lambda e: e.activation(out=W1t[:, g, :], in_=pw[:, 0:128], func=AF.Copy), r=[B_pw], w=[B_tab])
                    yield
                T("dve", lambda e: e.reciprocal(out=t3[:], in_=rho[:]))
                T("dve", lambda e: e.tensor_tensor(out=cosT[:, :, 0], in0=PWr[:, 8, :], in1=t3[:], op=ALU.mult))
                T("dve", lambda e: e.tensor_tensor(out=sinT[:, :, 0], in0=PWi[:, 8, :], in1=t3[:], op=ALU.mult))
                e1, e2 = f32t("e1", [128, 32, 32]), f32t("e2", [128, 32, 32])
                m = 1
                while m < 64:
                    br_ = cosT[:, :, m - 1:m].to_broadcast([128, 32, m])
                    bi_ = sinT[:, :, m - 1:m].to_broadcast([128, 32, m])
                    cmul(cosT[:, :, m:2 * m], sinT[:, :, m:2 * m], cosT[:, :, 0:m], sinT[:, :, 0:m], br_, bi_,
                         e1[:, :, 0:m], e2[:, :, 0:m])
                    m *= 2
                    yield

            tgen = tab_gen()
            for nb in range(24):
                i = nb % 2
                S.dma("sp", wab[i][:], wada_v[:, :, nb * 256:(nb + 1) * 256], B_wab[i], True)
                S.op("pe", [(lambda e, k=k: e.matmul(pr[i][0:1, 0:256], lhsT=cact[:, k:k + 1], rhs=wab[i][:, k, :],
                                                     start=(k == 0), stop=(k == 7))) for k in range(8)],
                     r=[B_cact, B_wab[i]], w=[B_pr[i]])
                S.op("dve", lambda e: e.tensor_tensor(out=modrow[0:1, nb * 256:(nb + 1) * 256], in0=pr[i][0:1, 0:256],
                                                      in1=modrow[0:1, nb * 256:(nb + 1) * 256], op=ALU.add),
                     r=[B_pr[i]], w=[B_mod])
                for _ in range(4):
                    next(tgen, None)
            for _ in tgen:
                pass
            for t_, dst_ in ((W1t, scr_W1t), (Ktoep, scr_Ktoep), (Ctab, scr_Ctab), (cosT, scr_cosT), (sinT, scr_sinT),
                             (rho, scr_rho), (Jt, scr_Jt)):
                S.dma("pool", dst_, t_[:], B_tab, False)
            cols = [(0, 0), (1, 8), (3, 16), (4, 24)]
            S.op("pe", [(lambda e, j=j, o=o, k=k: e.matmul(pc[:, o + k:o + k + 1],
                                                           lhsT=modrow[0:1, j * D + k * 128:j * D + (k + 1) * 128],
                                                           rhs=ones_f[0:1, 0:1], start=True, stop=True))
                        for (j, o) in cols for k in range(8)], r=[B_mod, B_const], w=[B_pc])
            S.op("dve", lambda e: e.tensor_copy(out=sh_m[:], in_=pc[:, 0:8]), r=[B_pc], w=[B_mod])
            S.op("dve", lambda e: e.tensor_copy(out=sh_f[:], in_=pc[:, 16:24]), r=[B_pc], w=[B_mod])
            S.op("dve", lambda e: e.scalar_tensor_tensor(out=gmod_m[:], in0=pc[:, 8:16], scalar=1.0, in1=gpm[:],
                                                         op0=ALU.add, op1=ALU.mult), r=[B_pc, B_g], w=[B_mod])
            S.op("dve", lambda e: e.scalar_tensor_tensor(out=gmod_f[:], in0=pc[:, 24:32], scalar=1.0, in1=gpf[:],
                                                         op0=ALU.add, op1=ALU.mult), r=[B_pc, B_g], w=[B_mod])
            S.op("dve", lambda e: e.tensor_tensor(out=rprod[0:1, 0:D], in0=modrow[0:1, 2 * D:3 * D],
                                                  in1=grow[0:1, 0:D], op=ALU.mult), r=[B_mod, B_g], w=[B_rp])
            S.op("dve", lambda e: e.tensor_tensor(out=rprod[0:1, D:2 * D], in0=modrow[0:1, 5 * D:6 * D],
                                                  in1=grow[0:1, D:2 * D], op=ALU.mult), r=[B_mod, B_g], w=[B_rp])
            for j, dst in ((0, bc_m), (1, bc_f)):
                for hh in range(2):
                    S.op("pe", lambda e: e.matmul(pr[hh][:, :], lhsT=ones_f[0:1, :],
                                                  rhs=rprod[0:1, j * D + hh * 512:j * D + (hh + 1) * 512],
                                                  start=True, stop=True), r=[B_rp, B_const], w=[B_pr[hh]])
                    S.op("act", lambda e: e.activation(out=dst[:, hh * 512:(hh + 1) * 512], in_=pr[hh][:, :],
                                                       func=AF.Copy), r=[B_pr[hh]], w=[B_mod])
            if debug:
                d = dbg_out("modrow", [1, 6 * D])
                S.dma("sp", d, modrow[:], B_mod, False)
                d = dbg_out("bc_m", [128, D])
                S.dma("sp", d, bc_m[:], B_mod, False)
                d = dbg_out("gmod_m", [128, 8])
                S.dma("sp", d, gmod_m[:], B_mod, False)
            S.barrier()
            S.release([B_cc, B_g, B_tab, B_mod] + B_wab)


        def load_cast_weight(stack_tmp, dst, src_view, ncols, B_dst, stg, B_stg, engs=("dve", "act"), doff=0):
            nblk = (ncols + 255) // 256
            for cbk in range(nblk):
                c0 = cbk * 256
                c1 = min(ncols, c0 + 256)
                i = load_cast_weight.n % len(stg)
                load_cast_weight.n += 1
                kk = src_view.shape[1]
                S.dma("sp", stg[i][:, 0:kk, 0:c1 - c0], src_view[:, :, c0:c1], B_stg[i], True)
                eng = engs[cbk % len(engs)]
                if eng == "act":
                    S.op("act", lambda e: e.activation(out=dst[:, :, doff + c0:doff + c1], in_=stg[i][:, 0:kk, 0:c1 - c0], func=AF.Copy),
                         r=[B_stg[i]], w=[B_dst])
                else:
                    S.op(eng, lambda e: e.tensor_copy(out=dst[:, :, doff + c0:doff + c1], in_=stg[i][:, 0:kk, 0:c1 - c0]),
                         r=[B_stg[i]], w=[B_dst])
        load_cast_weight.n = 0

        if upto < 1:
            S.barrier()
            return nc, dbg

        with ExitStack() as ps:
            wukv = sbt(ps, "wukv", [128, 8, 1536], BF16)
            stg = [sbt(ps, "stg%d" % i, [128, 8, 256], F32) for i in range(2)]
            B_stg = [Buf("stg0"), Buf("stg1")]
            B_wukv = Buf("wukv")
            win_v = w_in.rearrange("(k p) n -> p k n", p=128)
            load_cast_weight(ps, wukv, win_v[:, :, 0:512], 512, B_wukv, stg, B_stg)
            load_cast_weight(ps, wukv, win_v[:, :, 1024:2048], 1024, B_wukv, stg, B_stg, doff=512)
            xt = [sbt(ps, "xt%d" % i, [128, 8, D], F32) for i in range(2)]
            B_xt = [Buf("xt0"), Buf("xt1")]
            junk = sbt(ps, "junk", [128, D], BF16)
            B_junk = Buf("junk")
            ssq = sbt(ps, "ssq", [128, 8], F32)
            rstd = sbt(ps, "rstd", [128, 8], F32)
            B_ssq, B_rstd = Buf("ssq"), Buf("rstd")
            xn = sbt(ps, "xn", [128, 8, D], BF16)
            B_xn = Buf("xn")
            hnT = sbt(ps, "hnT", [128, 8, TT], BF16)
            B_hnT = Buf("hnT")
            u_cm = sbt(ps, "u_cm", [128, 32, 8, 16], BF16)
            v_cm = sbt(ps, "v_cm", [128, 8, 512], BF16)
            kT = sbt(ps, "kT", [128, 4, TT], BF16)
            B_ucm, B_vcm, B_kT = Buf("ucm"), Buf("vcm"), Buf("kT")
            km_f = sbt(ps, "km_f", [128, 4, 4], F32)
            B_kmf = Buf("kmf")
            ptr = Ring([pst(ps, "ptr%d" % i, [128, TT], BF16) for i in range(2)], "ptr")
            pmm = Ring([pst(ps, "pmm%d" % i, [128, 512], F32) for i in range(4)], "pmm")
            xo_v = xo.rearrange("(t c s) d -> t c s d", c=128, s=8)
            xp_v = xp.rearrange("(t c s) d -> t c s d", c=128, s=8)
            evac_i = [0]

            def evac_copy(dst_ap, src_ap, r, w, scale=None):
                evac_i[0] += 1
                if evac_i[0] % 2 == 0:
                    if scale is None:
                        S.op("act", lambda e: e.activation(out=dst_ap, in_=src_ap, func=AF.Copy), r=r, w=w)
                    else:
                        S.op("act", lambda e: e.activation(out=dst_ap, in_=src_ap, func=AF.Copy, scale=scale), r=r, w=w)
                else:
                    if scale is None:
                        S.op("dve", lambda e: e.tensor_copy(out=dst_ap, in_=src_ap), r=r, w=w)
                    else:
                        S.op("dve", lambda e: e.tensor_scalar(out=dst_ap, in0=src_ap, scalar1=scale, scalar2=None,
                                                              op0=ALU.mult), r=r, w=w)

            def normA(xsrc_ap, xi):
                S.dma("sp", xt[xi][:], xsrc_ap, B_xt[xi], True)
                for s_ in range(8):
                    S.op("act", lambda e: e.activation(out=junk[:], in_=xt[xi][:, s_, :], func=AF.Square,
                                                       accum_out=ssq[:, s_:s_ + 1]), r=[B_xt[xi]], w=[B_junk, B_ssq])
                S.op("act", lambda e: e.activation(out=rstd[:], in_=ssq[:], func=AF.Sqrt, scale=1.0 / D, bias=EPS),
                     r=[B_ssq], w=[B_rstd])
                S.op("dve", lambda e: e.reciprocal(out=rstd[:], in_=rstd[:]), r=[B_rstd], w=[B_rstd])
                for s_ in range(8):
                    eng = "dve"
                    S.op(eng, lambda e: e.tensor_scalar(out=xn[:, s_, :], in0=xt[xi][:, s_, :],
                                                        scalar1=rstd[:, s_:s_ + 1], scalar2=None, op0=ALU.mult),
                         r=[B_xt[xi], B_rstd], w=[B_xn])

            def normT(gmod, shc):
                for k in range(8):
                    pt, B_pt = ptr.next()
                    S.op("pe", [(lambda e, s_=s_: e.transpose(out=pt[:, s_ * 128:(s_ + 1) * 128],
                                                              in_=xn[:, s_, k * 128:(k + 1) * 128], identity=ident_b[:]))
                                for s_ in range(8)], r=[B_xn, B_const], w=[B_pt])
                    if k % 2 == 0:
                        S.op("dve", lambda e: e.tensor_scalar(out=hnT[:, k, :], in0=pt[:, :], scalar1=gmod[:, k:k + 1],
                                                              scalar2=shc[:, k:k + 1], op0=ALU.mult, op1=ALU.add),
                             r=[B_pt, B_mod], w=[B_hnT])
                    else:
                        S.op("act", lambda e: e.activation(out=hnT[:, k, :], in_=pt[:, :], func=AF.Identity,
                                                           scale=gmod[:, k:k + 1], bias=shc[:, k:k + 1]),
                             r=[B_pt, B_mod], w=[B_hnT])

            for gt in range(2 * NT):
                own = gt >= NT
                ot = gt - NT
                if gt == 0:
                    normA(xp_v[0], 0)
                normT(gmod_m, sh_m)
                if gt + 1 < 2 * NT:
                    g2 = gt + 1
                    normA(xo_v[g2 - NT] if g2 >= NT else xp_v[g2], g2 % 2)
                if own:
                    S.dma("pool", scr_hn[ot], hnT[:], B_hnT, False)
                for s_ in range(8):
                    pu, B_pu = pmm.next()
                    S.op("pe", [(lambda e, k=k: e.matmul(pu[:, :], lhsT=hnT[:, k, s_ * 128:(s_ + 1) * 128],
                                                         rhs=wukv[:, k, 0:512], start=(k == 0), stop=(k == 7)))
                                for k in range(8)], r=[B_hnT, B_wukv], w=[B_pu])
                    evac_copy(u_cm[:, :, s_, :], pu[:, :].rearrange("p (g c) -> p g c", g=32), [B_pu], [B_ucm])
                    pv, B_pv = pmm.next()
                    S.op("pe", [(lambda e, k=k: e.matmul(pv[:, :], lhsT=hnT[:, k, s_ * 128:(s_ + 1) * 128],
                                                         rhs=wukv[:, k, 1024:1536], start=(k == 0), stop=(k == 7)))
                                for k in range(8)], r=[B_hnT, B_wukv], w=[B_pv])
                    evac_copy(v_cm[:, s_, :], pv[:, :], [B_pv], [B_vcm])
                S.dma("pool", scr_v[gt].rearrange("s c f -> c s f"), v_cm[:], B_vcm, False)
                S.dma("pool", scr_u[gt], u_cm[:], B_ucm, False)
                for cb in range(4):
                    for hf in range(2):
                        pk, B_pk = pmm.next()
                        S.op("pe", [(lambda e, k=k: e.matmul(pk[:, :], lhsT=wukv[:, k, 512 + cb * 128:512 + (cb + 1) * 128],
                                                             rhs=hnT[:, k, hf * 512:(hf + 1) * 512],
                                                             start=(k == 0), stop=(k == 7))) for k in range(8)],
                             r=[B_hnT, B_wukv], w=[B_pk])
                        evac_copy(kT[:, cb, hf * 512:(hf + 1) * 512], pk[:, :], [B_pk], [B_kT])
                S.dma("pool", scr_kt[:, :, gt * TT:(gt + 1) * TT].rearrange("b p n -> p b n"), kT[:], B_kT, False)
                S.op("dve", lambda e: e.tensor_reduce(out=km_f[:], in_=kT[:].rearrange("p b (s k c) -> p b k s c", s=8, k=4, c=32),
                                                      axis=AX.XY, op=ALU.add), r=[B_kT], w=[B_kmf])
                S.op("dve", lambda e: e.tensor_scalar(out=kmean[:, :, gt * 4:(gt + 1) * 4], in0=km_f[:], scalar1=1.0 / 256.0,
                                                      scalar2=None, op0=ALU.mult), r=[B_kmf], w=[B_kmean])
                if debug and gt == 0:
                    for nm, t_, B_, shp, dt_ in (("hnT", hnT, B_hnT, [128, 8, TT], BF16), ("u_cm", u_cm, B_ucm, [128, 32, 8, 16], BF16),
                                                 ("v_cm", v_cm, B_vcm, [128, 8, 512], BF16), ("kT", kT, B_kT, [128, 4, TT], BF16)):
                        S.dma("sp", dbg_out(nm, shp, dt_), t_[:], B_, False)
            if debug:
                S.dma("sp", dbg_out("kmean", [128, 4, 32], BF16), kmean[:], B_kmean, False)
            S.barrier()
            S.release([B_wukv] + B_stg + B_xt + [B_hnT, B_vcm, B_kT, B_ucm, B_kmean])


        if upto < 2:
            S.barrier()
            return nc, dbg

        with ExitStack() as ps:
            W1t = sbt(ps, "W1t", [128, 32, 128], BF16)
            Ktoep = sbt(ps, "Ktoep", [128, 32, 128], BF16)
            Ctab = sbt(ps, "Ctab", [128, 32, 128], BF16)
            cosT = sbt(ps, "cosT", [128, 32, 64], F32)
            sinT = sbt(ps, "sinT", [128, 32, 64], F32)
            rho = sbt(ps, "rho", [128, 32], F32)
            Jt = sbt(ps, "Jt", [128, 128], F32)
            B_tab = Buf("tab")
            pss = Ring([pst(ps, "pss%d" % i, [128, 512], F32) for i in range(6)], "pss")
            ptb = Ring([pst(ps, "ptb%d" % i, [128, 1024], BF16) for i in range(2)], "ptb")

            for t_, src_ in ((W1t, scr_W1t), (Ktoep, scr_Ktoep), (Ctab, scr_Ctab), (cosT, scr_cosT), (sinT, scr_sinT),
                             (rho, scr_rho), (Jt, scr_Jt)):
                S.dma("sp", t_[:], src_, B_tab, True)
            ucm = [sbt(ps, "ucm%d" % i, [128, 32, 128], BF16) for i in range(2)]
            B_ucm2 = [Buf("ucm0"), Buf("ucm1")]
            U = sbt(ps, "U", [128, 32, 128], BF16)
            B_U = [Buf("U%d" % i) for i in range(4)]
            SX = sbt(ps, "SX", [128, 32, 128], F32)
            B_SX = [Buf("SX%d" % i) for i in range(8)]
            Wh = sbt(ps, "Wh", [128, 32, 64], F32)
            B_Wh = [Buf("Wh%d" % i) for i in range(4)]
            Xprev = sbt(ps, "Xprev", [128, 32, 128], BF16)
            B_Xp = Buf("Xprev")
            carry = sbt(ps, "carry", [128, 32], F32)
            carry2 = sbt(ps, "carry2", [128, 32], F32)
            B_carry, B_carry2 = Buf("carry"), Buf("carry2")
            tmp2 = [sbt(ps, "tmp2_%d" % i, [128, 8, 64], F32) for i in range(2)]
            B_tmp2 = [Buf("tmp2_0"), Buf("tmp2_1")]
            zg = sbt(ps, "zg", [128, 32, 128], BF16)
            B_zg = [Buf("zg%d" % i) for i in range(8)]
            z_cm = sbt(ps, "z_cm", [128, 8, 512], BF16)
            B_zcm = Buf("z_cm")
            zT = sbt(ps, "zT", [128, 4, TT], BF16)
            B_zT = Buf("zT")
            S.op("dve", lambda e: e.memset(carry[:], 0.0), w=[B_carry])
            tcount = [0]
            Zh = [sbt(ps, "Zh%d" % i, [128, 32, 64], F32) for i in range(2)]
            B_Zh = [Buf("Zh0"), Buf("Zh1")]
            rhoT = sbt(ps, "rhoT", [128, 32, 64], F32)
            tmpc = sbt(ps, "tmpc", [128, 32], F32)
            B_tmpc = Buf("tmpc")
            S.op("dve", lambda e: e.tensor_copy(out=rhoT[:], in_=rho[:].unsqueeze(2).to_broadcast([128, 32, 64])), r=[B_tab], w=[B_tab])
            S.op("dve", lambda e: e.memset(rhoT[:, :, 0], 0.0), r=[B_tab], w=[B_tab])

            def scan_half(hf, init_buf_ap, B_init):
                S.op("dve", lambda e: e.tensor_tensor(out=tmpc[:], in0=rho[:], in1=init_buf_ap[:], op=ALU.mult),
                     r=[B_init, B_tab], w=[B_tmpc])
                S.op("dve", lambda e: e.tensor_tensor(out=Zh[hf][:, :, 0], in0=Zh[hf][:, :, 0], in1=tmpc[:], op=ALU.add),
                     r=[B_tmpc], w=[B_Zh[hf]])
                S.op("dve", lambda e: e.tensor_tensor_scan(out=Wh[:].rearrange("p g c -> p (g c)"),
                                                           data0=rhoT[:].rearrange("p g c -> p (g c)"),
                                                           data1=Zh[hf][:].rearrange("p g c -> p (g c)"), initial=0.0,
                                                           op0=ALU.mult, op1=ALU.add),
                     r=[B_Zh[hf], B_tab], w=B_Wh)

            def last_state(dst, B_dst):
                pj, B_pj = pss.next()
                S.op("pe", lambda e: e.matmul(pj[:, 0:32], lhsT=Jt[:], rhs=Wh[:, :, 63], start=True, stop=True),
                     r=B_Wh + [B_tab], w=[B_pj])
                S.op("dve", lambda e: e.tensor_tensor(out=tmpc[:], in0=pj[:, 0:32], in1=sinT[:, :, 63], op=ALU.mult),
                     r=[B_pj, B_tab], w=[B_tmpc])
                S.op("dve", lambda e: e.tensor_tensor(out=dst[:], in0=Wh[:, :, 63], in1=cosT[:, :, 63], op=ALU.mult),
                     r=B_Wh + [B_tab], w=[B_dst])
                S.op("dve", lambda e: e.tensor_tensor(out=dst[:], in0=dst[:], in1=tmpc[:], op=ALU.add), r=[B_tmpc], w=[B_dst])

            def rot_unrot(hf, init_buf_ap, B_init):
                c0 = hf * 64
                scan_half(hf, init_buf_ap, B_init)
                for q in range(4):
                    pj, B_pj = pss.next()
                    S.op("pe", lambda e: e.matmul(pj[:, :], lhsT=Jt[:], rhs=Wh[:, 8 * q:8 * q + 8, :].rearrange("p g c -> p (g c)"), start=True, stop=True),
                         r=[B_Wh[q], B_tab], w=[B_pj])
                    i2 = tcount[0] % 2
                    tcount[0] += 1
                    S.op("dve", lambda e: e.tensor_tensor(out=tmp2[i2][:], in0=pj[:, :].rearrange("p (g c) -> p g c", g=8),
                                                          in1=sinT[:, 8 * q:8 * q + 8, :], op=ALU.mult),
                         r=[B_pj, B_tab], w=[B_tmp2[i2]])
                    sxv = SX[:, 8 * q:8 * q + 8, c0:c0 + 64]
                    S.op("dve", lambda e: e.tensor_tensor(out=sxv, in0=Wh[:, 8 * q:8 * q + 8, :], in1=cosT[:, 8 * q:8 * q + 8, :],
                                                           op=ALU.mult), r=[B_Wh[q], B_tab], w=[B_SX[2 * q], B_SX[2 * q + 1]])
                    S.op("dve", lambda e: e.tensor_tensor(out=sxv, in0=sxv, in1=tmp2[i2][:], op=ALU.add),
                         r=[B_tmp2[i2]], w=[B_SX[2 * q], B_SX[2 * q + 1]])

            for gt in range(2 * NT):
                own = gt >= NT
                ot = gt - NT
                ui = gt % 2
                S.dma("sp", ucm[ui][:], scr_u[gt].rearrange("p g s c -> p g (s c)"), B_ucm2[ui], True)
                for q in range(4):
                    pt, B_pt = ptb.next()
                    S.op("pe", [(lambda e, j=j: e.transpose(out=pt[:, j * 128:(j + 1) * 128],
                                                            in_=ucm[ui][:, 8 * q + j, :],
                                                            identity=ident_b[:])) for j in range(8)],
                         r=[B_ucm2[ui], B_const], w=[B_pt])
                    evac_copy2 = "act" if q % 2 == 0 else "dve"
                    if evac_copy2 == "act":
                        S.op("act", lambda e: e.activation(out=U[:, 8 * q:8 * q + 8, :], in_=pt[:, :].rearrange("p (g c) -> p g c", g=8),
                                                           func=AF.Copy), r=[B_pt], w=[B_U[q]])
                    else:
                        S.op("dve", lambda e: e.tensor_copy(out=U[:, 8 * q:8 * q + 8, :], in_=pt[:, :].rearrange("p (g c) -> p g c", g=8)),
                             r=[B_pt], w=[B_U[q]])
                S.op("dve", lambda e: e.tensor_copy(out=Xprev[:, :, 0], in_=carry[:]), r=[B_carry], w=[B_Xp])
                for gb in range(8):
                    pa, B_pa = pss.next()
                    S.op("pe", [(lambda e, j=j: e.matmul(pa[:, j * 128:(j + 1) * 128], lhsT=W1t[:, 4 * gb + j, :],
                                                         rhs=U[:, 4 * gb + j, :], start=True, stop=True)) for j in range(4)],
                         r=[B_U[gb // 2], B_tab], w=[B_pa])
                    S.op("act", lambda e: e.activation(out=SX[:, 4 * gb:4 * gb + 4, :], in_=pa[:, :].rearrange("p (g c) -> p g c", g=4),
                                                       func=AF.Copy), r=[B_pa], w=[B_SX[gb]])
                    pj, B_pj = pss.next()
                    S.op("pe", lambda e: e.matmul(pj[:, :], lhsT=Jt[:], rhs=SX[:, 4 * gb:4 * gb + 4, :].rearrange("p g c -> p (g c)"), start=True, stop=True),
                         r=[B_SX[gb], B_tab], w=[B_pj])
                    for hf in range(2):
                        i2 = tcount[0] % 2
                        tcount[0] += 1
                        pjv = pj[:, :].rearrange("p (g c) -> p g c", g=4)[:, :, hf * 64:(hf + 1) * 64]
                        S.op("dve", lambda e: e.tensor_tensor(out=tmp2[i2][:, 0:4, :], in0=pjv, in1=sinT[:, 4 * gb:4 * gb + 4, :],
                                                              op=ALU.mult), r=[B_pj, B_tab], w=[B_tmp2[i2]])
                        sxv = SX[:, 4 * gb:4 * gb + 4, hf * 64:(hf + 1) * 64]
                        S.op("dve", lambda e: e.tensor_tensor(out=sxv, in0=sxv, in1=cosT[:, 4 * gb:4 * gb + 4, :], op=ALU.mult),
                             r=[B_tab], w=[B_SX[gb]])
                        S.op("dve", lambda e: e.tensor_tensor(out=Zh[hf][:, 4 * gb:4 * gb + 4, :], in0=sxv, in1=tmp2[i2][:, 0:4, :], op=ALU.subtract),
                             r=[B_tmp2[i2], B_SX[gb]], w=[B_Zh[hf]])
                if own:
                    rot_unrot(0, carry, B_carry)
                    S.op("dve", lambda e: e.tensor_copy(out=carry2[:], in_=SX[:, :, 63]), r=B_SX, w=[B_carry2])
                    rot_unrot(1, carry2, B_carry2)
                    S.op("dve", lambda e: e.tensor_copy(out=carry[:], in_=SX[:, :, 127]), r=B_SX, w=[B_carry])
                else:
                    scan_half(0, carry, B_carry)
                    last_state(carry2, B_carry2)
                    scan_half(1, carry2, B_carry2)
                    last_state(carry, B_carry)
                if gt == NT - 1:
                    S.op("dve", lambda e: e.tensor_scalar(out=carry[:], in0=carry[:], scalar1=flag_sb[:, 0:1], scalar2=None,
                                                          op0=ALU.mult), r=[B_carry, B_const], w=[B_carry])
                if not own:
                    continue
                S.op("act", lambda e: e.activation(out=Xprev[:, :, 1:128], in_=SX[:, :, 0:127], func=AF.Copy), r=B_SX, w=[B_Xp])
                for gb in range(8):
                    py, B_py = pss.next()
                    fns = []
                    for j in range(4):
                        g = 4 * gb + j
                        fns.append(lambda e, j=j, g=g: e.matmul(py[:, j * 128:(j + 1) * 128], lhsT=Ktoep[:, g, :], rhs=U[:, g, :],
                                                                start=True, stop=False))
                        fns.append(lambda e, j=j, g=g: e.matmul(py[:, j * 128:(j + 1) * 128], lhsT=Ctab[:, g, :], rhs=Xprev[:, g, :],
                                                                start=False, stop=True))
                    S.op("pe", fns, r=[B_U[gb // 2], B_Xp, B_tab], w=[B_py])
                    S.op("act", lambda e: e.activation(out=zg[:, 4 * gb:4 * gb + 4, :], in_=py[:, :].rearrange("p (g c) -> p g c", g=4),
                                                       func=AF.Gelu), r=[B_py], w=[B_zg[gb]])
                for q in range(4):
                    pt, B_pt = ptb.next()
                    S.op("pe", [(lambda e, j=j: e.transpose(out=pt[:, j * 128:(j + 1) * 128], in_=zg[:, 8 * q + j, :],
                                                            identity=ident_b[:])) for j in range(8)],
                         r=[B_zg[2 * q], B_zg[2 * q + 1], B_const], w=[B_pt])
                    S.op("dve", lambda e: e.tensor_copy(out=z_cm[:, :, q * 128:(q + 1) * 128].rearrange("p t (g c) -> p g t c", g=8),
                                                        in_=pt[:, :].rearrange("p (g t c) -> p g t c", g=8, t=8)),
                         r=[B_pt], w=[B_zcm])
                for blk in range(4):
                    pt, B_pt = ptb.next()
                    S.op("pe", [(lambda e, t_=t_: e.transpose(out=pt[:, t_ * 128:(t_ + 1) * 128],
                                                              in_=z_cm[:, t_, blk * 128:(blk + 1) * 128], identity=ident_b[:]))
                                for t_ in range(8)], r=[B_zcm, B_const], w=[B_pt])
                    if blk % 2 == 0:
                        S.op("act", lambda e: e.activation(out=zT[:, blk, :], in_=pt[:, :], func=AF.Copy), r=[B_pt], w=[B_zT])
                    else:
                        S.op("dve", lambda e: e.tensor_copy(out=zT[:, blk, :], in_=pt[:, :]), r=[B_pt], w=[B_zT])
                S.dma("pool", scr_z[:, :, ot * TT:(ot + 1) * TT].rearrange("b p n -> p b n"), zT[:], B_zT, False)
                if debug and ot == 0:
                    S.dma("sp", dbg_out("z_cm", [128, 8, 512], BF16), z_cm[:], B_zcm, False)
                    S.dma("sp", dbg_out("SX", [128, 32, 128], F32), SX[:], B_SX[0], False, extra_r=B_SX)
            S.barrier()
            S.release([B_tab, B_zT, B_zcm] + B_ucm2 + B_SX)


        if upto < 3:
            S.barrier()
            return nc, dbg

        ps12 = ExitStack()
        st12 = ExitStack()
        Kaug = [sbt(ps12, "Kaug%d" % i, [128, 2 * TOK], BF16) for i in range(2)]
        QAbase = sbt(ps12, "QAbase", [128, TOK], BF16)
        CB = sbt(ps12, "CB", [128, 8, 8, 128], BF16)
        VB = sbt(ps12, "VB", [128, NT, 32], F32)
        OWNM = sbt(ps12, "OWNM", [128, NT, 32], F32)
        B_st = Buf("p2static")
        wqg = sbt(st12, "wqg", [128, 8, 2560], BF16)
        B_wqg = Buf("wqg")
        with ExitStack() as st:
            stg = [sbt(st, "stgb%d" % i, [128, 8, 256], F32) for i in range(2)]
            B_stg = [Buf("stgb0"), Buf("stgb1")]
            load_cast_weight(st, wqg, win_v[:, :, 512:1024], 512, B_wqg, stg, B_stg)
            load_cast_weight(st, wqg, win_v[:, :, 2048:4096], 2048, B_wqg, stg, B_stg, doff=512)
            S.barrier()
            S.release(B_stg)
        def P2s(eng, fn):
            S.op(eng, fn, r=[B_st, B_const], w=[B_st])

        st = st12
        f32t = lambda nm, shp: sbt(st, nm, shp, F32)
        pidx = f32t("pidx", [128, 1])
        cA, cB_, cC = f32t("cA", [128, 1]), f32t("cB", [128, 1]), f32t("cC", [128, 1])
        qA, qB, qC = f32t("qA", [128, 1]), f32t("qB", [128, 1]), f32t("qC", [128, 1])
        p64 = f32t("p64", [128, 1])
        shi, slo, blk = f32t("shi", [128, TT]), f32t("slo", [128, TT]), f32t("blkt", [128, TT])
        ka = f32t("ka", [128, TT])
        cble, cblt = f32t("cble", [128, 128]), f32t("cblt", [128, 128])
        P2s("pool", lambda e: e.iota(pidx[:], pattern=[[0, 1]], base=0, channel_multiplier=1,
                                     allow_small_or_imprecise_dtypes=True))
        P2s("dve", lambda e: e.tensor_scalar(out=p64[:], in0=pidx[:], scalar1=-64.0, scalar2=None, op0=ALU.add))
        for (dst, val) in ((cA, 98.0), (cB_, 99.0), (qA, 96.0), (qB, 97.0)):
            P2s("dve", lambda e: e.tensor_scalar(out=dst[:], in0=pidx[:], scalar1=val, scalar2=None, op0=ALU.is_equal))
        P2s("dve", lambda e: e.tensor_tensor(out=cC[:], in0=qA[:], in1=qB[:], op=ALU.add))
        P2s("dve", lambda e: e.tensor_tensor(out=qC[:], in0=cA[:], in1=cB_[:], op=ALU.add))
        P2s("dve", lambda e: e.tensor_scalar(out=qA[:], in0=qA[:], scalar1=-1.0, scalar2=None, op0=ALU.mult))
        P2s("dve", lambda e: e.tensor_scalar(out=qB[:], in0=qB[:], scalar1=-1.0, scalar2=None, op0=ALU.mult))
        P2s("pool", lambda e: e.iota(slo[:], pattern=[[1, 8], [0, 16], [8, 8]], base=0, channel_multiplier=0,
                                     allow_small_or_imprecise_dtypes=True))
        for gt in range(2 * NT):
            P2s("pool", lambda e: e.iota(shi[:], pattern=[[0, 8], [64, 16], [0, 8]], base=gt * TT - TOK, channel_multiplier=0,
                                         allow_small_or_imprecise_dtypes=True))
            P2s("pool", lambda e: e.iota(blk[:], pattern=[[0, 8], [1, 4], [0, 32]], base=gt * 4, channel_multiplier=0,
                                         allow_small_or_imprecise_dtypes=True))
            P2s("dve", lambda e: e.tensor_scalar(out=ka[:], in0=blk[:], scalar1=p64[:, 0:1], scalar2=cC[:, 0:1],
                                                 op0=ALU.is_equal, op1=ALU.add))
            P2s("dve", lambda e: e.scalar_tensor_tensor(out=ka[:], in0=shi[:], scalar=cA[:, 0:1], in1=ka[:],
                                                        op0=ALU.mult, op1=ALU.add))
            P2s("dve", lambda e: e.scalar_tensor_tensor(out=ka[:], in0=slo[:], scalar=cB_[:, 0:1], in1=ka[:],
                                                        op0=ALU.mult, op1=ALU.add))
            for i in range(2):
                P2s("dve", lambda e: e.tensor_copy(out=Kaug[i][64:128, gt * TT:(gt + 1) * TT], in_=ka[64:128, :]))
            if gt >= NT:
                ot = gt - NT
                P2s("dve", lambda e: e.tensor_scalar(out=ka[:], in0=shi[:], scalar1=qA[:, 0:1], scalar2=qC[:, 0:1],
                                                     op0=ALU.mult, op1=ALU.add))
                P2s("dve", lambda e: e.scalar_tensor_tensor(out=QAbase[:, ot * TT:(ot + 1) * TT], in0=slo[:], scalar=qB[:, 0:1],
                                                            in1=ka[:], op0=ALU.mult, op1=ALU.add))
        P2s("pool", lambda e: e.memset(cble[:], 0.0))
        P2s("pool", lambda e: e.memset(cblt[:], 0.0))
        for i in range(4):
            sl = slice(32 * i, 32 * i + 32)
            P2s("pool", lambda e: e.affine_select(out=cble[sl, sl], in_=cble[sl, sl], pattern=[[1, 32]], compare_op=ALU.is_ge,
                                                  fill=NEG, base=0, channel_multiplier=-1))
            P2s("pool", lambda e: e.affine_select(out=cblt[sl, sl], in_=cblt[sl, sl], pattern=[[1, 32]], compare_op=ALU.is_ge,
                                                  fill=NEG, base=-1, channel_multiplier=-1))
        for sk in range(8):
            for sq in range(8):
                src = cble if sk <= sq else cblt
                P2s("dve", lambda e: e.tensor_copy(out=CB[:, sk, sq, :], in_=src[:]))
        P2s("pool", lambda e: e.memset(VB[:], 0.0))
        P2s("pool", lambda e: e.memset(OWNM[:], 1.0))
        P2s("dve", lambda e: e.tensor_scalar(out=VB[:, :, 0:16], in0=ones_f[:, 0:64].rearrange("p (a b) -> p a b", a=NT),
                                             scalar1=flag_sb[:, 0:1], scalar2=-1.0, op0=ALU.mult, op1=ALU.add))
        P2s("dve", lambda e: e.tensor_scalar(out=VB[:, :, 0:16], in0=VB[:, :, 0:16], scalar1=1e30, scalar2=None, op0=ALU.mult))
        for ot in range(NT):
            for i in range(4):
                j = 4 * ot + i
                sl = slice(32 * i, 32 * i + 32)
                P2s("pool", lambda e: e.affine_select(out=VB[sl, ot, 16:32], in_=VB[sl, ot, 16:32], pattern=[[-1, 16]],
                                                      compare_op=ALU.is_ge, fill=-1e30, base=j - 1, channel_multiplier=0))
                P2s("pool", lambda e: e.affine_select(out=OWNM[sl, ot, :], in_=OWNM[sl, ot, :], pattern=[[1, 32]],
                                                      compare_op=ALU.not_equal, fill=0.0, base=-(16 + j), channel_multiplier=0))

        with ExitStack() as ps:
            hnl = [sbt(ps, "hnl%d" % i, [128, 8, TT], BF16) for i in range(2)]
            B_hnl = [Buf("hnl0"), Buf("hnl1")]
            qT = sbt(ps, "qT", [128, 4, TT], BF16)
            gT = sbt(ps, "gT", [128, 16, TT], BF16)
            B_qT, B_gT = Buf("qT"), Buf("gT")
            pmm = Ring([pst(ps, "pmb%d" % i, [128, 512], F32) for i in range(6)], "pmb")
            for ot in range(NT):
                hi_ = ot % 2
                S.dma("sp", hnl[hi_][:], scr_hn[ot], B_hnl[hi_], True)
                for cb in range(20):
                    for hf in range(2):
                        pq, B_pq = pmm.next()
                        S.op("pe", [(lambda e, k=k: e.matmul(pq[:, :], lhsT=wqg[:, k, cb * 128:(cb + 1) * 128],
                                                             rhs=hnl[hi_][:, k, hf * 512:(hf + 1) * 512],
                                                             start=(k == 0), stop=(k == 7))) for k in range(8)],
                             r=[B_hnl[hi_], B_wqg], w=[B_pq])
                        if cb < 4:
                            S.op("act", lambda e: e.activation(out=qT[:, cb, hf * 512:(hf + 1) * 512], in_=pq[:, :],
                                                               func=AF.Copy, scale=0.125), r=[B_pq], w=[B_qT])
                        elif cb < 12:
                            S.op("act", lambda e: e.activation(out=gT[:, cb - 4, hf * 512:(hf + 1) * 512], in_=pq[:, :],
                                                               func=AF.Sigmoid), r=[B_pq], w=[B_gT])
                        else:
                            S.op("dve", lambda e: e.tensor_copy(out=gT[:, cb - 4, hf * 512:(hf + 1) * 512], in_=pq[:, :]),
                                 r=[B_pq], w=[B_gT])
                S.dma("pool", scr_q[:, :, ot * TT:(ot + 1) * TT].rearrange("b p n -> p b n"), qT[:], B_qT, False)
                S.dma("pool", scr_g[:, :, ot * TT:(ot + 1) * TT].rearrange("b p n -> p b n"), gT[:], B_gT, False)
            S.barrier()
            S.release([B_qT, B_gT, B_wqg] + B_hnl)
        st12.close()

        if upto < 4:
            S.barrier()
            return nc, dbg

        with ExitStack() as ps:
            Qaug = [sbt(ps, "Qaug%d" % i, [128, TOK], BF16) for i in range(2)]
            Vh = [sbt(ps, "Vh%d" % i, [128, 64, 128], BF16) for i in range(2)]
            B_K = [Buf("K0"), Buf("K1")]
            B_Q = [Buf("Q0"), Buf("Q1")]
            B_Qs = [Buf("Qs0"), Buf("Qs1")]
            B_V = [Buf("V0"), Buf("V1")]
            oT = sbt(ps, "oT", [128, TOK], BF16)
            B_oT = Buf("oT")
            kmh = sbt(ps, "kmh", [128, 32], F32)
            kmb = sbt(ps, "kmb", [128, 32], BF16)
            B_kmh = Buf("kmh")
            gm = sbt(ps, "gm", [128, 8, 32], F32)
            m8 = sbt(ps, "m8", [128, 8, 8], F32)
            thr = sbt(ps, "thr", [128, 8], F32)
            selb = sbt(ps, "selb", [128, 8, 32], BF16)
            B_gm, B_m8, B_thr, B_selb = Buf("gm"), Buf("m8"), Buf("thr"), Buf("selb")
            pTs = [sbt(ps, "pTs%d" % i, [128, 512], BF16) for i in range(4)]
            B_pTs = [Buf("pTs%d" % i) for i in range(4)]
            rrow = sbt(ps, "rrow", [128, 512], F32)
            bcs = sbt(ps, "bcs", [128, 512], F32)
            B_rrow, B_bcs = Buf("rrow"), Buf("bcs")
            pS = Ring([pst(ps, "pS%d" % i, [128, 512], F32) for i in range(3)], "pS")
            pBc = Ring([pst(ps, "pBc%d" % i, [128, 512], F32) for i in range(1)], "pBc")
            pAcc = Ring([pst(ps, "pAcc%d" % i, [128, 512], F32) for i in range(2)], "pAcc")
            pG = Ring([pst(ps, "pG%d" % i, [128, 512], F32) for i in range(1)], "pG")
            pX = Ring([pst(ps, "pX%d" % i, [128, 1024], BF16) for i in range(1)], "pX")

            for i in range(2):
                S.op("dve", lambda e: e.memset(Vh[i][:, :, 64:128], 0.0), w=[B_V[i]])
                S.op("dve", lambda e: e.memset(Vh[i][:, :, 64:65], 1.0), w=[B_V[i]])

            scr_v_r = scr_v.rearrange("t s c f -> c (t s) f")
            LAG = 2
            pend = []
            step = [0]
            seq = [0]

            def sched(due, fn):
                seq[0] += 1
                pend.append((due, seq[0], fn))
                pend.sort(key=lambda t_: (t_[0], t_[1]))

            def flush(upto_step):
                while pend and pend[0][0] <= upto_step:
                    pend.pop(0)[2]()

            def emit_loads(hd):
                cb, h2, hb = hd // 2, hd % 2, hd % 2
                slope = 2.0 ** (-(hd + 1))
                S.dma("sp", Kaug[hb][0:64, :], scr_kt[cb, h2 * 64:(h2 + 1) * 64, :], B_K[hb], True)
                S.dma("sp", Qaug[hb][0:64, :], scr_q[cb, h2 * 64:(h2 + 1) * 64, :], B_Q[hb], True)
                S.dma("sp", Vh[hb][:, :, 0:64], scr_v_r[:, :, hd * 64:(hd + 1) * 64], B_V[hb], True)

            def sel_items(hd):
                cb, h2, hb = hd // 2, hd % 2, hd % 2
                slope = 2.0 ** (-(hd + 1))
                items = []

                def st0():
                    S.op("dve", lambda e: e.tensor_scalar(out=Qaug[hb][96:128, :], in0=QAbase[96:128, :], scalar1=slope, scalar2=None,
                                                           op0=ALU.mult), r=[B_st], w=[B_Qs[hb]])
                    S.op("dve", lambda e: e.tensor_reduce(out=kmh[0:64, :].rearrange("p (t k) -> p t k", t=2 * NT),
                                                          in_=Kaug[hb][0:64, :].rearrange("p (t s k c) -> p t k s c", t=2 * NT, s=8, k=4, c=32),
                                                          axis=AX.XY, op=ALU.add), r=[B_K[hb]], w=[B_kmh])
                    S.op("dve", lambda e: e.tensor_scalar(out=kmb[0:64, :], in0=kmh[0:64, :], scalar1=1.0 / 256.0, scalar2=None, op0=ALU.mult),
                         r=[B_kmh], w=[B_kmh])
                items.append((4, st0))
                for ot in range(NT):
                    base = 30 + 60 * ot
                    holder = {}

                    def stA(ot=ot, holder=holder):
                        pg, B_pg = pG.next()
                        holder["pg"] = (pg, B_pg)
                        S.op("pe", [(lambda e, s_=s_: e.matmul(pg[:, s_ * 32:(s_ + 1) * 32],
                                                               lhsT=Qaug[hb][0:64, ot * TT + s_ * 128:ot * TT + (s_ + 1) * 128],
                                                               rhs=kmb[0:64, :], start=True, stop=True)) for s_ in range(8)],
                             r=[B_Q[hb], B_kmh], w=[B_pg])

                    def stB(ot=ot, holder=holder):
                        pg, B_pg = holder["pg"]
                        S.op("dve", lambda e: e.tensor_tensor(out=gm[:], in0=pg[:, 0:256].rearrange("p (s n) -> p s n", s=8),
                                                              in1=VB[:, ot:ot + 1, :].to_broadcast([128, 8, 32]), op=ALU.add),
                             r=[B_pg, B_st], w=[B_gm])
                        S.op("dve", [(lambda e, s_=s_: e.max(out=m8[:, s_, :], in_=gm[:, s_, :])) for s_ in range(8)], r=[B_gm], w=[B_m8])
                        S.op("dve", lambda e: e.tensor_scalar(out=thr[:], in0=m8[:, :, 2], scalar1=-1e29, scalar2=None, op0=ALU.max),
                             r=[B_m8], w=[B_thr])
                        S.op("dve", lambda e: e.tensor_tensor(out=gm[:], in0=gm[:], in1=thr[:].unsqueeze(2).to_broadcast([128, 8, 32]),
                                                              op=ALU.subtract), r=[B_thr], w=[B_gm])
                        S.op("dve", lambda e: e.tensor_scalar(out=gm[:], in0=gm[:], scalar1=0.0, scalar2=NEG, op0=ALU.is_lt, op1=ALU.mult),
                             r=[B_gm], w=[B_gm])
                        S.op("dve", lambda e: e.tensor_tensor(out=selb[:], in0=gm[:], in1=OWNM[:, ot:ot + 1, :].to_broadcast([128, 8, 32]),
                                                              op=ALU.mult), r=[B_gm, B_st], w=[B_selb])

                    def stC(ot=ot, holder=holder):
                        px, B_px = pX.next()
                        holder["px"] = (px, B_px)
                        S.op("pe", [(lambda e, s_=s_: e.transpose(out=px[64:96, s_ * 128:(s_ + 1) * 128], in_=selb[:, s_, :],
                                                                  identity=ident_b[:])) for s_ in range(8)],
                             r=[B_selb, B_const], w=[B_px])

                    def stD(ot=ot, holder=holder):
                        px, B_px = holder["px"]
                        S.op("dve", lambda e: e.tensor_copy(out=Qaug[hb][64:96, ot * TT:(ot + 1) * TT], in_=px[64:96, :]),
                             r=[B_px], w=[B_Qs[hb]])
                    items += [(base, stA), (base + 8, stB), (base + 24, stC), (base + 32, stD)]
                return items

            cvA = [sbt(ps, "cvA%d" % i, [128, 8, 256], F32) for i in range(3)]
            cvB = [sbt(ps, "cvB%d" % i, [128, 8, 256], BF16) for i in range(3)]
            B_cvA = [Buf("cvA%d" % i) for i in range(3)]
            B_cvB = [Buf("cvB%d" % i) for i in range(3)]
            cv_jobs = []
            kp = lambda w_: w_.rearrange("(k p) n -> p k n", p=128)
            for src, dst, K_, N_ in ((kp(w_glu_a), scr_wga, 4, D), (kp(w_glu_b), scr_wgb, 4, D), (kp(w_attn_out), scr_wao, 4, D),
                                     (kp(w_out), scr_wo, 8, D), (kp(w_ff_gate), scr_wg, 8, DFF), (kp(w_ff_up), scr_wu, 8, DFF),
                                     (kp(w_ff_down), scr_wd, NF, D)):
                for k0 in range(0, K_, 8):
                    kk = min(8, K_ - k0)
                    for c0 in range(0, N_, 256):
                        cv_jobs.append((src, dst, k0, kk, c0, min(256, N_ - c0)))
            for ji, (src, dst, k0, kk, c0, w_) in enumerate(cv_jobs):
                i3 = ji % 3
                t_in = 60 + 36 * ji

                def cv_in(src=src, k0=k0, kk=kk, c0=c0, w_=w_, i3=i3):
                    S.dma("sp", cvA[i3][:, 0:kk, 0:w_], src[:, k0:k0 + kk, c0:c0 + w_], B_cvA[i3], True)

                def cv_cast(kk=kk, w_=w_, i3=i3):
                    S.op("dve", lambda e: e.tensor_copy(out=cvB[i3][:, 0:kk, 0:w_], in_=cvA[i3][:, 0:kk, 0:w_]),
                         r=[B_cvA[i3]], w=[B_cvB[i3]])

                def cv_out(dst=dst, k0=k0, kk=kk, c0=c0, w_=w_, i3=i3):
                    S.dma("pool", dst[:, k0:k0 + kk, c0:c0 + w_], cvB[i3][:, 0:kk, 0:w_], B_cvB[i3], False)
                sched(t_in, cv_in)
                sched(t_in + 30, cv_cast)
                sched(t_in + 34, cv_out)

            emit_loads(0)
            for (_, fn) in sel_items(0):
                fn()
            for hd in range(8):
                cb, h2 = hd // 2, hd % 2
                hb = hd % 2
                if hd + 1 < 8:
                    emit_loads(hd + 1)
                    for (rel, fn) in sel_items(hd + 1):
                        sched(step[0] + rel, fn)
                for ot in range(NT):
                    for qh in range(2):
                        q0 = ot * TT + qh * 512
                        acc, B_acc = pAcc.next()
                        kts = [(gt, sk) for gt in range(NT + ot + 1) for sk in range(8)]
                        for ki, (gt, sk) in enumerate(kts):
                            pS_, B_pS = pS.next()
                            diag = (gt == NT + ot)
                            k0 = gt * TT + sk * 128
                            fns = [lambda e, pS_=pS_, k0=k0, q0=q0, diag=diag: e.matmul(pS_[:, :], lhsT=Kaug[hb][:, k0:k0 + 128],
                                                                                        rhs=Qaug[hb][:, q0:q0 + 512], start=True, stop=not diag)]
                            if diag:
                                fns.append(lambda e, pS_=pS_, sk=sk, qh=qh: e.matmul(pS_[:, :], lhsT=ident_b[:],
                                                                                      rhs=CB[:, sk, 4 * qh:4 * qh + 4, :].rearrange("p a b -> p (a b)"),
                                                                                      start=False, stop=True))
                            S.op("pe", fns, r=[B_K[hb], B_Q[hb], B_Qs[hb], B_st, B_const], w=[B_pS])
                            pi_ = step[0] % 4
                            S.op("act", lambda e, pS_=pS_, pi_=pi_: e.activation(out=pTs[pi_][:], in_=pS_[:, :], func=AF.Exp),
                                 r=[B_pS], w=[B_pTs[pi_]])

                            def pv(acc=acc, B_acc=B_acc, gt=gt, sk=sk, pi_=pi_, first=(ki == 0), last=(ki == len(kts) - 1), hb=hb):
                                S.op("pe", lambda e: e.matmul(acc[:, :], lhsT=Vh[hb][:, gt * 8 + sk, :], rhs=pTs[pi_][:],
                                                              start=first, stop=last), r=[B_V[hb], B_pTs[pi_]], w=[B_acc])
                            sched(step[0] + LAG, pv)
                            flush(step[0])
                            step[0] += 1

                        def tail1(acc=acc, B_acc=B_acc):
                            S.op("dve", lambda e: e.reciprocal(out=rrow[64:65, :], in_=acc[64:65, :]), r=[B_acc], w=[B_rrow])

                        def tail2(acc=acc, B_acc=B_acc, q0=q0):
                            pb, B_pb = pBc.next()
                            S.op("pe", lambda e: e.matmul(pb[0:64, :], lhsT=ones_f[64:65, 0:64], rhs=rrow[64:65, :], start=True, stop=True),
                                 r=[B_rrow, B_const], w=[B_pb])
                            S.op("dve", lambda e: e.tensor_copy(out=bcs[0:64, :], in_=pb[0:64, :]), r=[B_pb], w=[B_bcs])
                            S.op("dve", lambda e: e.tensor_tensor(out=oT[0:64, q0:q0 + 512], in0=acc[0:64, :], in1=bcs[0:64, :], op=ALU.mult),
                                 r=[B_acc, B_bcs], w=[B_oT])
                        sched(step[0] + LAG + 1, tail1)
                        sched(step[0] + LAG + 8, tail2)
                flush(step[0] + LAG + 8)
                S.dma("pool", scr_o[cb, h2 * 64:(h2 + 1) * 64, :], oT[0:64, :], B_oT, False)
            flush(10 ** 9)
            S.barrier()
            S.release(B_K + B_Q + B_V + [B_oT] + B_cvA + B_cvB)
        ps12.close()


        if upto < 5:
            S.barrier()
            return nc, dbg

        def post_norm_residual(py2, B_py2, xres, B_xres, s_loc, bc, ssq2, rs2, B_ssq2, B_rs2, junkf, B_junkf):
            for nh in range(2):
                S.op("act", lambda e: e.activation(out=junkf[:], in_=py2[nh][:, :], func=AF.Square, accum_out=ssq2[:, nh:nh + 1]),
                     r=[B_py2[nh]], w=[B_junkf, B_ssq2])
            S.op("dve", lambda e: e.tensor_tensor(out=rs2[:, 0:1], in0=ssq2[:, 0:1], in1=ssq2[:, 1:2], op=ALU.add), r=[B_ssq2], w=[B_rs2])
            S.op("act", lambda e: e.activation(out=rs2[:, 1:2], in_=rs2[:, 0:1], func=AF.Sqrt, scale=1.0 / D, bias=EPS), r=[B_rs2], w=[B_rs2])
            S.op("dve", lambda e: e.reciprocal(out=rs2[:, 2:3], in_=rs2[:, 1:2]), r=[B_rs2], w=[B_rs2])
            for nh in range(2):
                S.op("dve", lambda e: e.scalar_tensor_tensor(out=junkf[:, 0:512] if False else py2_sb[nh][:], in0=py2[nh][:, :], scalar=rs2[:, 2:3],
                                                             in1=bc[:, nh * 512:(nh + 1) * 512], op0=ALU.mult, op1=ALU.mult),
                     r=[B_py2[nh], B_rs2, B_mod], w=[B_py2sb[nh]])
                S.op("dve", lambda e: e.tensor_tensor(out=xres[:, s_loc, nh * 512:(nh + 1) * 512], in0=xres[:, s_loc, nh * 512:(nh + 1) * 512],
                                                       in1=py2_sb[nh][:], op=ALU.add), r=[B_py2sb[nh]], w=[B_xres])

        with ExitStack() as ps:
            Wga = sbt(ps, "Wga", [128, 4, D], BF16)
            Wgb = sbt(ps, "Wgb", [128, 4, D], BF16)
            Wao = sbt(ps, "Wao", [128, 4, D], BF16)
            Wo = sbt(ps, "Wo", [128, 8, D], BF16)
            B_w3 = Buf("w3")
            B_w3o = Buf("w3o")
            for dst, src in ((Wga, scr_wga), (Wgb, scr_wgb), (Wao, scr_wao)):
                S.dma("sp", dst[:], src, B_w3, True)
            zT3 = sbt(ps, "zT3", [128, 4, TT], BF16)
            oT3 = sbt(ps, "oT3", [128, 4, TT], BF16)
            gT3 = sbt(ps, "gT3", [128, 16, TT], BF16)
            x3 = sbt(ps, "x3", [128, 8, D], F32)
            mT = sbt(ps, "mT", [128, 8, TT], BF16)
            B_zT3, B_oT3, B_gT3, B_x3, B_mT = Buf("zT3"), Buf("oT3"), Buf("gT3"), Buf("x3"), Buf("mT")
            sg = [sbt(ps, "sg%d" % i, [128, 512], F32) for i in range(2)]
            B_sg = [Buf("sg0"), Buf("sg1")]
            ta = [sbt(ps, "ta%d" % i, [128, 512], BF16) for i in range(2)]
            tb_ = [sbt(ps, "tb%d" % i, [128, 512], BF16) for i in range(2)]
            B_ta, B_tb = [Buf("ta0"), Buf("ta1")], [Buf("tb0"), Buf("tb1")]
            gbs = [sbt(ps, "gbs%d" % i, [128, 512], BF16) for i in range(2)]
            B_gbs = [Buf("gbs0"), Buf("gbs1")]
            py2_sb = [sbt(ps, "py2sb%d" % i, [128, 512], F32) for i in range(2)]
            B_py2sb = [Buf("py2sb0"), Buf("py2sb1")]
            junkf = sbt(ps, "junkf", [128, 512], BF16)
            B_junkf = Buf("junkf")
            ssq2 = sbt(ps, "ssq2", [128, 2], F32)
            rs2 = sbt(ps, "rs2", [128, 3], F32)
            B_ssq2, B_rs2 = Buf("ssq2"), Buf("rs2")
            p3 = Ring([pst(ps, "p3_%d" % i, [128, 512], F32) for i in range(8)], "p3")
            xo_v3 = xo.rearrange("(t c s) d -> t c s d", c=128, s=8)
            out_v3 = out.rearrange("(t c s) d -> t c s d", c=128, s=8)
            it = 0
            for ot in range(NT):
                S.dma("sp", zT3[:], scr_z[:, :, ot * TT:(ot + 1) * TT].rearrange("b p n -> p b n"), B_zT3, True)
                S.dma("sp", oT3[:], scr_o[:, :, ot * TT:(ot + 1) * TT].rearrange("b p n -> p b n"), B_oT3, True)
                S.dma("sp", gT3[:], scr_g[:, :, ot * TT:(ot + 1) * TT].rearrange("b p n -> p b n"), B_gT3, True)
                S.dma("sp", x3[:], xo_v3[ot], B_x3, True)
                if ot == 0:
                    S.dma("sp", Wo[:], scr_wo, B_w3o, True)
                for ncb in range(8):
                    for hf in range(2):
                        sl = slice(hf * 512, (hf + 1) * 512)
                        i2 = it % 2
                        it += 1
                        pa, B_pa = p3.next()
                        pb, B_pb = p3.next()
                        pc, B_pc = p3.next()
                        S.op("pe", [(lambda e, k=k: e.matmul(pa[:, :], lhsT=Wga[:, k, ncb * 128:(ncb + 1) * 128], rhs=zT3[:, k, sl],
                                                             start=(k == 0), stop=(k == 3))) for k in range(4)], r=[B_w3, B_zT3], w=[B_pa])
                        S.op("pe", [(lambda e, k=k: e.matmul(pb[:, :], lhsT=Wgb[:, k, ncb * 128:(ncb + 1) * 128], rhs=zT3[:, k, sl],
                                                             start=(k == 0), stop=(k == 3))) for k in range(4)], r=[B_w3, B_zT3], w=[B_pb])
                        S.op("pe", [(lambda e, k=k: e.matmul(pc[:, :], lhsT=Wao[:, k, ncb * 128:(ncb + 1) * 128], rhs=oT3[:, k, sl],
                                                             start=(k == 0), stop=(k == 3))) for k in range(4)], r=[B_w3, B_oT3], w=[B_pc])
                        S.op("act", lambda e: e.activation(out=sg[i2][:], in_=pb[:, :], func=AF.Sigmoid), r=[B_pb], w=[B_sg[i2]])
                        S.op("dve", lambda e: e.tensor_tensor(out=sg[i2][:], in0=pa[:, :], in1=sg[i2][:], op=ALU.mult), r=[B_pa, B_sg[i2]], w=[B_sg[i2]])
                        S.op("dve", lambda e: e.tensor_tensor(out=ta[i2][:], in0=sg[i2][:], in1=gT3[:, ncb, sl], op=ALU.mult),
                             r=[B_sg[i2], B_gT3], w=[B_ta[i2]])
                        S.op("act", lambda e: e.activation(out=gbs[i2][:], in_=gT3[:, 8 + ncb, sl], func=AF.Sigmoid), r=[B_gT3], w=[B_gbs[i2]])
                        S.op("dve", lambda e: e.tensor_tensor(out=tb_[i2][:], in0=pc[:, :], in1=gbs[i2][:], op=ALU.mult),
                             r=[B_pc, B_gbs[i2]], w=[B_tb[i2]])
                        S.op("dve", lambda e: e.tensor_tensor(out=mT[:, ncb, sl], in0=ta[i2][:], in1=tb_[i2][:], op=ALU.add),
                             r=[B_ta[i2], B_tb[i2]], w=[B_mT])
                for s_ in range(8):
                    py2, B_py2 = [], []
                    for nh in range(2):
                        p_, B_p = p3.next()
                        S.op("pe", [(lambda e, k=k: e.matmul(p_[:, :], lhsT=mT[:, k, s_ * 128:(s_ + 1) * 128], rhs=Wo[:, k, nh * 512:(nh + 1) * 512],
                                                             start=(k == 0), stop=(k == 7))) for k in range(8)], r=[B_mT, B_w3o], w=[B_p])
                        py2.append(p_)
                        B_py2.append(B_p)
                    post_norm_residual(py2, B_py2, x3, B_x3, s_, bc_m, ssq2, rs2, B_ssq2, B_rs2, junkf, B_junkf)
                S.dma("pool", out_v3[ot], x3[:], B_x3, False)
            S.barrier()
            S.release([B_w3, B_w3o, B_zT3, B_oT3, B_gT3, B_x3])

        if upto < 6:
            S.barrier()
            return nc, dbg

        with ExitStack() as ps:
            Wg = sbt(ps, "Wg", [128, 8, DFF], BF16)
            Wu = sbt(ps, "Wu", [128, 8, DFF], BF16)
            Wd = sbt(ps, "Wd", [128, NF, D], BF16)
            B_w4 = Buf("w4")
            FCH = [(0, 4), (4, 10), (10, 16), (16, NF)]
            B_wgu = [Buf("wgu%d" % i) for i in range(len(FCH))]
            B_wd = Buf("wd")
            f2c = {}
            for ci, (f0, f1) in enumerate(FCH):
                for f in range(f0, f1):
                    f2c[f] = ci
                S.dma("sp", Wg[:, :, f0 * 128:f1 * 128], scr_wg[:, :, f0 * 128:f1 * 128], B_wgu[ci], True)
                S.dma("sp", Wu[:, :, f0 * 128:f1 * 128], scr_wu[:, :, f0 * 128:f1 * 128], B_wgu[ci], True)
            S.dma("sp", Wd[:], scr_wd, B_wd, True)
            x4 = sbt(ps, "x4", [128, 4, D], F32)
            xn4 = sbt(ps, "xn4", [128, 4, D], BF16)
            hn4 = sbt(ps, "hn4", [128, 8, 512], BF16)
            aT = sbt(ps, "aT", [128, NF, 512], BF16)
            B_x4, B_xn4, B_hn4, B_aT = Buf("x4"), Buf("xn4"), Buf("hn4"), Buf("aT")
            sl4 = [sbt(ps, "sl4_%d" % i, [128, 512], BF16) for i in range(2)]
            B_sl4 = [Buf("sl4_0"), Buf("sl4_1")]
            py2_sb = [sbt(ps, "py4sb%d" % i, [128, 512], F32) for i in range(2)]
            B_py2sb = [Buf("py4sb0"), Buf("py4sb1")]
            junk4 = sbt(ps, "junk4", [128, D], BF16)
            junkf = sbt(ps, "junkf4", [128, 512], BF16)
            B_junk4, B_junkf = Buf("junk4"), Buf("junkf4")
            ssq4 = sbt(ps, "ssq4", [128, 4], F32)
            rstd4 = sbt(ps, "rstd4", [128, 4], F32)
            ssq2 = sbt(ps, "ssq2b", [128, 2], F32)
            rs2 = sbt(ps, "rs2b", [128, 3], F32)
            B_ssq4, B_rstd4, B_ssq2, B_rs2 = Buf("ssq4"), Buf("rstd4"), Buf("ssq2b"), Buf("rs2b")
            ptr4 = Ring([pst(ps, "ptr4_%d" % i, [128, 512], BF16) for i in range(2)], "ptr4")
            p4 = Ring([pst(ps, "p4_%d" % i, [128, 512], F32) for i in range(6)], "p4")
            out_v4 = out.rearrange("(t c s) d -> t c s d", c=128, s=8)
            it = 0
            for ot in range(NT):
                for sh in range(2):
                    S.dma("sp", x4[:], out_v4[ot][:, 4 * sh:4 * sh + 4, :], B_x4, True)
                    for s_ in range(4):
                        S.op("act", lambda e: e.activation(out=junk4[:], in_=x4[:, s_, :], func=AF.Square, accum_out=ssq4[:, s_:s_ + 1]),
                             r=[B_x4], w=[B_junk4, B_ssq4])
                    S.op("act", lambda e: e.activation(out=rstd4[:], in_=ssq4[:], func=AF.Sqrt, scale=1.0 / D, bias=EPS), r=[B_ssq4], w=[B_rstd4])
                    S.op("dve", lambda e: e.reciprocal(out=rstd4[:], in_=rstd4[:]), r=[B_rstd4], w=[B_rstd4])
                    for s_ in range(4):
                        S.op("dve",
                             lambda e: e.tensor_scalar(out=xn4[:, s_, :], in0=x4[:, s_, :], scalar1=rstd4[:, s_:s_ + 1], scalar2=None, op0=ALU.mult),
                             r=[B_x4, B_rstd4], w=[B_xn4])
                    for k in range(8):
                        pt, B_pt = ptr4.next()
                        S.op("pe", [(lambda e, s_=s_: e.transpose(out=pt[:, s_ * 128:(s_ + 1) * 128], in_=xn4[:, s_, k * 128:(k + 1) * 128],
                                                                  identity=ident_b[:])) for s_ in range(4)], r=[B_xn4, B_const], w=[B_pt])
                        if k % 2 == 0:
                            S.op("dve", lambda e: e.tensor_scalar(out=hn4[:, k, :], in0=pt[:, :], scalar1=gmod_f[:, k:k + 1], scalar2=sh_f[:, k:k + 1],
                                                                  op0=ALU.mult, op1=ALU.add), r=[B_pt, B_mod], w=[B_hn4])
                        else:
                            S.op("act", lambda e: e.activation(out=hn4[:, k, :], in_=pt[:, :], func=AF.Identity, scale=gmod_f[:, k:k + 1],
                                                               bias=sh_f[:, k:k + 1]), r=[B_pt, B_mod], w=[B_hn4])
                    for f in range(NF):
                        i2 = it % 2
                        it += 1
                        pg_, B_pg = p4.next()
                        pu_, B_pu = p4.next()
                        S.op("pe", [(lambda e, k=k: e.matmul(pg_[:, :], lhsT=Wg[:, k, f * 128:(f + 1) * 128], rhs=hn4[:, k, :],
                                                             start=(k == 0), stop=(k == 7))) for k in range(8)], r=[B_wgu[f2c[f]], B_hn4], w=[B_pg])
                        S.op("pe", [(lambda e, k=k: e.matmul(pu_[:, :], lhsT=Wu[:, k, f * 128:(f + 1) * 128], rhs=hn4[:, k, :],
                                                             start=(k == 0), stop=(k == 7))) for k in range(8)], r=[B_wgu[f2c[f]], B_hn4], w=[B_pu])
                        S.op("act", lambda e: e.activation(out=sl4[i2][:], in_=pg_[:, :], func=AF.Silu), r=[B_pg], w=[B_sl4[i2]])
                        S.op("dve", lambda e: e.tensor_tensor(out=aT[:, f, :], in0=pu_[:, :], in1=sl4[i2][:], op=ALU.mult),
                             r=[B_pu, B_sl4[i2]], w=[B_aT])
                    for s_ in range(4):
                        py2, B_py2 = [], []
                        for nh in range(2):
                            p_, B_p = p4.next()
                            S.op("pe", [(lambda e, f=f: e.matmul(p_[:, :], lhsT=aT[:, f, s_ * 128:(s_ + 1) * 128], rhs=Wd[:, f, nh * 512:(nh + 1) * 512],
                                                                 start=(f == 0), stop=(f == NF - 1))) for f in range(NF)], r=[B_aT, B_wd], w=[B_p])
                            py2.append(p_)
                            B_py2.append(B_p)
                        post_norm_residual(py2, B_py2, x4, B_x4, s_, bc_f, ssq2, rs2, B_ssq2, B_rs2, junkf, B_junkf)
                    S.dma("pool", out_v4[ot][:, 4 * sh:4 * sh + 4, :], x4[:], B_x4, False)
            S.barrier()

        S.barrier()
    return nc, dbg


def _prep_inputs(inputs):
    f = lambda a: np.ascontiguousarray(np.asarray(a, dtype=np.float32))
    x = f(inputs["x"])
    c = f(inputs["c"])
    shared = {}
    shared["w_ada"] = f(inputs["w_ada"][0])
    shared["b_ada"] = f(inputs["b_ada"][0]).reshape(1, -1)
    shared["g_pre_mix_c"] = f(inputs["g_pre_mix"][0].reshape(8, 128).T)
    shared["g_pre_ffn_c"] = f(inputs["g_pre_ffn"][0].reshape(8, 128).T)
    shared["g_post_mix_r"] = f(inputs["g_post_mix"][0]).reshape(1, -1)
    shared["g_post_ffn_r"] = f(inputs["g_post_ffn"][0]).reshape(1, -1)
    shared["w_in"] = f(inputs["w_in"][0])
    st2 = lambda a: f(np.concatenate([a, a], axis=0))
    shared["s_are"] = st2(np.asarray(inputs["ssm_a_re"][0]).T)
    shared["s_aim"] = st2(np.asarray(inputs["ssm_a_im"][0]).T)
    shared["s_ldt"] = f(np.broadcast_to(np.asarray(inputs["ssm_log_dt"][0])[None, :], (128, 32)))
    shared["s_bre"] = st2(np.asarray(inputs["ssm_b_re"][0]).transpose(1, 0, 2))
    shared["s_bim"] = st2(np.asarray(inputs["ssm_b_im"][0]).transpose(1, 0, 2))
    shared["s_cre"] = st2(np.asarray(inputs["ssm_c_re"][0]).transpose(2, 0, 1))
    shared["s_cim"] = st2(np.asarray(inputs["ssm_c_im"][0]).transpose(2, 0, 1))
    shared["s_dcol"] = f(np.tile(np.asarray(inputs["ssm_d"][0]).T, (8, 1)))
    for k in ("w_glu_a", "w_glu_b", "w_attn_out", "w_out", "w_ff_gate", "w_ff_up", "w_ff_down"):
        shared[k] = f(inputs[k][0])
    in_maps = []
    for core in range(8):
        b, h = core // 2, core % 2
        m = dict(shared)
        m["xo"] = f(x[b, h * TOK:(h + 1) * TOK])
        m["xp"] = f(x[b, 0:TOK])
        m["flag"] = np.full((128, 1), float(h), np.float32)
        m["c_col"] = f(c[b].reshape(8, 128).T)
        in_maps.append(m)
    return in_maps


_CACHE = {}


def kernel(**inputs):
    in_maps = _prep_inputs(inputs)
    if "nc" not in _CACHE:
        _CACHE["nc"] = build_program()[0]
    nc = _CACHE["nc"]
    res = run_bass_kernel_spmd(nc, in_maps, core_ids=list(range(8)))
    outp = np.empty((4, 8192, D), np.float32)
    for core in range(8):
        b, h = core // 2, core % 2
        outp[b, h * TOK:(h + 1) * TOK] = res.results[core]["out"]
    return outp
```

```python
import numpy as np
from contextlib import ExitStack
import concourse.bass as bass
import concourse.mybir as mybir
from concourse.bass_utils import run_bass_kernel_spmd

F32 = mybir.dt.float32
BF16 = mybir.dt.bfloat16
AF = mybir.ActivationFunctionType
ALU = mybir.AluOpType
AX = mybir.AxisListType

D = 1024
TOK = 4096
TT = 1024
NT = TOK // TT
DFF = 2816
NF = DFF // 128
EPS = 1e-6
NEG = -30000.0


class DSem:
    def __init__(self, sem):
        self.sem = sem
        self.cnt = 0


class Buf:
    def __init__(self, name):
        self.name = name
        self.w = {}
        self.r = {}
        self.dsem = None


class Sched:
    def __init__(self, nc, es):
        self.nc = nc
        self.es = es
        self.E = dict(pe=nc.tensor, act=nc.scalar, dve=nc.vector, pool=nc.gpsimd, sp=nc.sync)
        self.sem = {k: es.enter_context(nc.semaphore("s_" + k)) for k in ("pe", "act", "dve", "pool")}
        self.cnt = {k: 0 for k in self.sem}
        self.seen = {k: {} for k in self.E}
        self.dsems = []
        self.free_dsems = []

    def _semof(self, key):
        return self.sem[key] if isinstance(key, str) else key.sem

    def _cntof(self, key):
        return self.cnt[key] if isinstance(key, str) else key.cnt

    def get_dsem(self, buf):
        if buf.dsem is None:
            if self.free_dsems:
                buf.dsem = self.free_dsems.pop()
            else:
                buf.dsem = DSem(self.es.enter_context(self.nc.semaphore("d%d" % len(self.dsems))))
                self.dsems.append(buf.dsem)
        return buf.dsem

    def release(self, bufs):
        for b in bufs:
            if b.dsem is not None:
                self.free_dsems.append(b.dsem)
                b.dsem = None

    def _waits(self, eng, r, w):
        need = {}
        for b in r:
            for k, v in b.w.items():
                if need.get(k, 0) < v:
                    need[k] = v
        for b in w:
            for dd in (b.w, b.r):
                for k, v in dd.items():
                    if need.get(k, 0) < v:
                        need[k] = v
        for k, v in need.items():
            if eng == "pe" and k == "pe":
                continue
            if self.seen[eng].get(k, 0) >= v:
                continue
            self.seen[eng][k] = v
            self.E[eng].wait_ge(self._semof(k), v)

    def op(self, eng, fns, r=(), w=()):
        if callable(fns):
            fns = [fns]
        self._waits(eng, r, w)
        ins = None
        for f in fns:
            ins = f(self.E[eng])
        self.cnt[eng] += 1
        ins.then_inc(self.sem[eng], 1)
        c = self.cnt[eng]
        for b in r:
            b.r[eng] = c
        for b in w:
            b.w[eng] = c
            b.r = {}

    def dma(self, eng, out, in_, sb, load, extra_r=(), extra_w=(), **kw):
        ds = self.get_dsem(sb)
        r = list(extra_r) + ([] if load else [sb])
        w = list(extra_w) + ([sb] if load else [])
        self._waits(eng, r, w)
        ins = self.E[eng].dma_start(out=out, in_=in_, **kw)
        ds.cnt += 16
        ins.then_inc(ds.sem, 16)
        for b in r:
            b.r[ds] = ds.cnt
        for b in w:
            b.w[ds] = ds.cnt
            b.r = {}

    def barrier(self):
        for eng in self.E:
            for k in self.sem:
                if k == eng:
                    continue
                v = self.cnt[k]
                if v > 0 and self.seen[eng].get(k, 0) < v:
                    self.seen[eng][k] = v
                    self.E[eng].wait_ge(self.sem[k], v)
            for ds in self.dsems:
                if ds.cnt > 0 and self.seen[eng].get(ds, 0) < ds.cnt:
                    self.seen[eng][ds] = ds.cnt
                    self.E[eng].wait_ge(ds.sem, ds.cnt)


def build_program(debug=False, upto=99):
    nc = bass.Bass("TRN2", target_bir_lowering=False)
    dram = {}

    def din(name, shape, dt=F32):
        dram[name] = nc.dram_tensor(name, list(shape), dt, kind="ExternalInput").ap()
        return dram[name]

    def dscr(name, shape, dt):
        return nc.dram_tensor(name, list(shape), dt, kind="Internal").ap()

    xo = din("xo", [TOK, D])
    xp = din("xp", [TOK, D])
    flag = din("flag", [128, 1])
    c_col = din("c_col", [128, 8])
    w_ada = din("w_ada", [D, 6 * D])
    b_ada = din("b_ada", [1, 6 * D])
    g_pre_mix_c = din("g_pre_mix_c", [128, 8])
    g_pre_ffn_c = din("g_pre_ffn_c", [128, 8])
    g_post_mix_r = din("g_post_mix_r", [1, D])
    g_post_ffn_r = din("g_post_ffn_r", [1, D])
    w_in = din("w_in", [D, 4096])
    s_are = din("s_are", [128, 32])
    s_aim = din("s_aim", [128, 32])
    s_ldt = din("s_ldt", [128, 32])
    s_bre = din("s_bre", [128, 32, 16])
    s_bim = din("s_bim", [128, 32, 16])
    s_cre = din("s_cre", [128, 32, 16])
    s_cim = din("s_cim", [128, 32, 16])
    s_dcol = din("s_dcol", [128, 32])
    w_glu_a = din("w_glu_a", [512, D])
    w_glu_b = din("w_glu_b", [512, D])
    w_attn_out = din("w_attn_out", [512, D])
    w_out = din("w_out", [D, D])
    w_ff_gate = din("w_ff_gate", [D, DFF])
    w_ff_up = din("w_ff_up", [D, DFF])
    w_ff_down = din("w_ff_down", [DFF, D])
    out = nc.dram_tensor("out", [TOK, D], F32, kind="ExternalOutput").ap()
    dbg = {}

    def dbg_out(name, shape, dt=F32):
        dbg[name] = nc.dram_tensor("dbg_" + name, list(shape), dt, kind="ExternalOutput").ap()
        return dbg[name]

    scr_hn = dscr("scr_hn", [NT, 128, 8, TT], BF16)
    scr_kt = dscr("scr_kt", [4, 128, 2 * TOK], BF16)
    scr_v = dscr("scr_v", [2 * NT, 8, 128, 512], BF16)
    scr_u = dscr("scr_u", [2 * NT, 128, 32, 8, 16], BF16)
    scr_q = dscr("scr_q", [4, 128, TOK], BF16)
    scr_g = dscr("scr_g", [16, 128, TOK], BF16)
    scr_z = dscr("scr_z", [4, 128, TOK], BF16)
    scr_o = dscr("scr_o", [4, 128, TOK], BF16)
    scr_W1t = dscr("scr_W1t", [128, 32, 128], BF16)
    scr_Ktoep = dscr("scr_Ktoep", [128, 32, 128], BF16)
    scr_Ctab = dscr("scr_Ctab", [128, 32, 128], BF16)
    scr_cosT = dscr("scr_cosT", [128, 32, 64], F32)
    scr_sinT = dscr("scr_sinT", [128, 32, 64], F32)
    scr_rho = dscr("scr_rho", [128, 32], F32)
    scr_Jt = dscr("scr_Jt", [128, 128], F32)
    scr_wga = dscr("scr_wga", [128, 4, D], BF16)
    scr_wgb = dscr("scr_wgb", [128, 4, D], BF16)
    scr_wao = dscr("scr_wao", [128, 4, D], BF16)
    scr_wo = dscr("scr_wo", [128, 8, D], BF16)
    scr_wg = dscr("scr_wg", [128, 8, DFF], BF16)
    scr_wu = dscr("scr_wu", [128, 8, DFF], BF16)
    scr_wd = dscr("scr_wd", [128, NF, D], BF16)

    with ExitStack() as es:
        S = Sched(nc, es)

        def sbt(stack, name, shape, dt):
            return stack.enter_context(nc.sbuf_tensor(name, list(shape), dt))

        def pst(stack, name, shape, dt):
            return stack.enter_context(nc.psum_tensor(name, list(shape), dt))

        class Ring:
            def __init__(self, tiles, name):
                self.t = [(t, Buf("%s%d" % (name, i))) for i, t in enumerate(tiles)]
                self.i = 0

            def next(self):
                x = self.t[self.i % len(self.t)]
                self.i += 1
                return x

        ident_f = sbt(es, "ident_f", [128, 128], F32)
        ident_b = sbt(es, "ident_b", [128, 128], BF16)
        ones_f = sbt(es, "ones_f", [128, 128], F32)
        gmod_m = sbt(es, "gmod_m", [128, 8], F32)
        sh_m = sbt(es, "sh_m", [128, 8], F32)
        gmod_f = sbt(es, "gmod_f", [128, 8], F32)
        sh_f = sbt(es, "sh_f", [128, 8], F32)
        bc_m = sbt(es, "bc_m", [128, D], F32)
        bc_f = sbt(es, "bc_f", [128, D], F32)
        flag_sb = sbt(es, "flag_sb", [128, 1], F32)
        kmean = sbt(es, "kmean", [128, 4, 32], BF16)
        B_const = Buf("const")
        B_mod = Buf("mod")
        B_kmean = Buf("kmean")

        S.op("pool", lambda e: e.memset(ident_f[:], 1.0), w=[B_const])
        S.op("pool", lambda e: e.affine_select(out=ident_f[:], in_=ident_f[:], pattern=[[-1, 128]],
                                                compare_op=ALU.is_equal, fill=0.0, base=0, channel_multiplier=1),
             w=[B_const])
        S.op("pool", lambda e: e.memset(ones_f[:], 1.0), w=[B_const])
        S.op("dve", lambda e: e.tensor_copy(out=ident_b[:], in_=ident_f[:]), r=[B_const], w=[B_const])
        S.dma("sp", flag_sb[:], flag, B_const, True)

        with ExitStack() as ps:
            cc = sbt(ps, "cc", [128, 8], F32)
            modrow = sbt(ps, "modrow", [1, 6 * D], F32)
            cact = sbt(ps, "cact", [128, 8], F32)
            wab = [sbt(ps, "wab%d" % i, [128, 8, 256], F32) for i in range(2)]
            gpm = sbt(ps, "gpm", [128, 8], F32)
            gpf = sbt(ps, "gpf", [128, 8], F32)
            grow = sbt(ps, "grow", [1, 2 * D], F32)
            rprod = sbt(ps, "rprod", [1, 2 * D], F32)
            pr = [pst(ps, "p0r%d" % i, [128, 512], F32) for i in range(2)]
            pc = pst(ps, "p0c", [128, 512], F32)
            B_cc, B_cact, B_brow, B_g = Buf("cc"), Buf("cact"), Buf("brow"), Buf("g")
            B_wab = [Buf("wab0"), Buf("wab1")]
            B_pr = [Buf("pr0"), Buf("pr1")]
            B_pc = Buf("pc")
            B_rp = Buf("rprod")
            S.dma("sp", cc[:], c_col, B_cc, True)
            S.dma("sp", modrow[:], b_ada, B_mod, True)
            S.dma("sp", gpm[:], g_pre_mix_c, B_g, True)
            S.dma("sp", gpf[:], g_pre_ffn_c, B_g, True)
            S.dma("sp", grow[:, 0:D], g_post_mix_r, B_g, True)
            S.dma("sp", grow[:, D:2 * D], g_post_ffn_r, B_g, True)
            S.op("act", lambda e: e.activation(out=cact[:], in_=cc[:], func=AF.Silu), r=[B_cc], w=[B_cact])
            wada_v = w_ada.rearrange("(k p) n -> p k n", p=128)
            W1t = sbt(ps, "W1t0", [128, 32, 128], BF16)
            Ktoep = sbt(ps, "Ktoep0", [128, 32, 128], BF16)
            Ctab = sbt(ps, "Ctab0", [128, 32, 128], BF16)
            cosT = sbt(ps, "cosT0", [128, 32, 64], F32)
            sinT = sbt(ps, "sinT0", [128, 32, 64], F32)
            rho = sbt(ps, "rho0", [128, 32], F32)
            Jt = sbt(ps, "Jt0", [128, 128], F32)
            B_tab = Buf("tab0")
            pss = Ring([pst(ps, "pss0_%d" % i, [128, 512], F32) for i in range(3)], "pss0")

            def T(eng, fn):
                S.op(eng, fn, r=[B_tab, B_const], w=[B_tab])

            def bc3(ap2, n):
                return ap2.unsqueeze(2).to_broadcast([ap2.shape[0], ap2.shape[1], n])

            def tab_gen():
                f32t = lambda nm, shp: sbt(ps, nm, shp, F32)
                are, aim, ldt = f32t("are", [128, 32]), f32t("aim", [128, 32]), f32t("ldt", [128, 32])
                bre, bim = f32t("bre", [128, 32, 16]), f32t("bim", [128, 32, 16])
                cre, cim = f32t("cre", [128, 32, 16]), f32t("cim", [128, 32, 16])
                dcol = f32t("dcol", [128, 32])
                for t_, src in ((are, s_are), (aim, s_aim), (ldt, s_ldt), (bre, s_bre), (bim, s_bim), (cre, s_cre),
                                (cim, s_cim), (dcol, s_dcol)):
                    S.dma("sp", t_[:], src, B_tab, True)
                dtt, xr, xi, mag = f32t("dtt", [128, 32]), f32t("xr", [128, 32]), f32t("xi", [128, 32]), f32t("mag", [128, 32])
                ys, sn, cs = f32t("ys", [128, 32]), f32t("sn", [128, 32]), f32t("cs", [128, 32])
                t1, t2, t3 = f32t("t1", [128, 32]), f32t("t2", [128, 32]), f32t("t3", [128, 32])
                cor, coi = f32t("cor", [128, 32]), f32t("coi", [128, 32])
                PWr, PWi = f32t("PWr", [128, 9, 32]), f32t("PWi", [128, 9, 32])
                NPr, NPi = f32t("NPr", [128, 8, 32]), f32t("NPi", [128, 8, 32])
                bbr, bbi = f32t("bbr", [128, 32, 16]), f32t("bbi", [128, 32, 16])
                u1, u2 = f32t("u1", [128, 32, 16]), f32t("u2", [128, 32, 16])
                BB = f32t("BB", [128, 32, 8, 16])
                WW = f32t("WW", [128, 32, 8, 16])
                CC = f32t("CC", [128, 32, 9, 16])
                maskLT = f32t("maskLT", [128, 8, 16])
                pi = float(np.pi)
                T("act", lambda e: e.activation(out=dtt[:], in_=ldt[:], func=AF.Exp))
                T("dve", lambda e: e.tensor_tensor(out=xr[:], in0=dtt[:], in1=are[:], op=ALU.mult))
                T("dve", lambda e: e.tensor_tensor(out=xi[:], in0=dtt[:], in1=aim[:], op=ALU.mult))
                T("act", lambda e: e.activation(out=mag[:], in_=xr[:], func=AF.Exp))
                T("act", lambda e: e.activation(out=rho[:], in_=xr[:], func=AF.Exp, scale=8.0))
                MAGIC = 12582912.0

                def sin_of(dst, src_ap, shift):
                    T("dve", lambda e: e.tensor_scalar(out=t1[:], in0=src_ap, scalar1=shift, scalar2=None, op0=ALU.add))
                    T("dve", lambda e: e.tensor_scalar(out=t2[:], in0=t1[:], scalar1=1.0 / (2 * pi), scalar2=MAGIC,
                                                       op0=ALU.mult, op1=ALU.add))
                    T("dve", lambda e: e.tensor_scalar(out=t2[:], in0=t2[:], scalar1=-MAGIC, scalar2=None, op0=ALU.add))
                    T("dve", lambda e: e.scalar_tensor_tensor(out=ys[:], in0=t2[:], scalar=-2 * pi, in1=t1[:],
                                                              op0=ALU.mult, op1=ALU.add))
                    T("dve", lambda e: e.tensor_scalar(out=ys[:], in0=ys[:], scalar1=-3.14159, scalar2=3.14159,
                                                       op0=ALU.max, op1=ALU.min))
                    T("act", lambda e: e.activation(out=dst, in_=ys[:], func=AF.Sin))

                sin_of(sn[:], xi[:], 0.0)
                sin_of(cs[:], xi[:], 0.5 * pi)
                yield
                T("dve", lambda e: e.memset(PWr[:, 0, :], 1.0))
                T("dve", lambda e: e.memset(PWi[:, 0, :], 0.0))
                T("dve", lambda e: e.memset(NPr[:, 0, :], 1.0))
                T("dve", lambda e: e.memset(NPi[:, 0, :], 0.0))
                T("dve", lambda e: e.tensor_tensor(out=PWr[:, 1, :], in0=mag[:], in1=cs[:], op=ALU.mult))
                T("dve", lambda e: e.tensor_tensor(out=PWi[:, 1, :], in0=mag[:], in1=sn[:], op=ALU.mult))
                abr, abi = PWr[:, 1, :], PWi[:, 1, :]
                T("dve", lambda e: e.tensor_tensor(out=t1[:], in0=are[:], in1=are[:], op=ALU.mult))
                T("dve", lambda e: e.tensor_tensor(out=t2[:], in0=aim[:], in1=aim[:], op=ALU.mult))
                T("dve", lambda e: e.tensor_tensor(out=t1[:], in0=t1[:], in1=t2[:], op=ALU.add))
                T("dve", lambda e: e.reciprocal(out=t1[:], in_=t1[:]))
                T("dve", lambda e: e.tensor_scalar(out=t2[:], in0=abr, scalar1=-1.0, scalar2=None, op0=ALU.add))
                T("dve", lambda e: e.tensor_tensor(out=cor[:], in0=t2[:], in1=are[:], op=ALU.mult))
                T("dve", lambda e: e.tensor_tensor(out=t3[:], in0=abi, in1=aim[:], op=ALU.mult))
                T("dve", lambda e: e.tensor_tensor(out=cor[:], in0=cor[:], in1=t3[:], op=ALU.add))
                T("dve", lambda e: e.tensor_tensor(out=cor[:], in0=cor[:], in1=t1[:], op=ALU.mult))
                T("dve", lambda e: e.tensor_tensor(out=coi[:], in0=abi, in1=are[:], op=ALU.mult))
                T("dve", lambda e: e.tensor_tensor(out=t3[:], in0=t2[:], in1=aim[:], op=ALU.mult))
                T("dve", lambda e: e.tensor_tensor(out=coi[:], in0=coi[:], in1=t3[:], op=ALU.subtract))
                T("dve", lambda e: e.tensor_tensor(out=coi[:], in0=coi[:], in1=t1[:], op=ALU.mult))
                T("dve", lambda e: e.tensor_tensor(out=bbr[:], in0=bre[:], in1=bc3(cor[:], 16), op=ALU.mult))
                T("dve", lambda e: e.tensor_tensor(out=u1[:], in0=bim[:], in1=bc3(coi[:], 16), op=ALU.mult))
                T("dve", lambda e: e.tensor_tensor(out=bbr[:], in0=bbr[:], in1=u1[:], op=ALU.subtract))
                T("dve", lambda e: e.tensor_tensor(out=bbi[:], in0=bim[:], in1=bc3(cor[:], 16), op=ALU.mult))
                T("dve", lambda e: e.tensor_tensor(out=u1[:], in0=bre[:], in1=bc3(coi[:], 16), op=ALU.mult))
                T("dve", lambda e: e.tensor_tensor(out=bbi[:], in0=bbi[:], in1=u1[:], op=ALU.add))
                T("dve", lambda e: e.tensor_tensor(out=t1[:], in0=abr, in1=abr, op=ALU.mult))
                T("dve", lambda e: e.tensor_tensor(out=t2[:], in0=abi, in1=abi, op=ALU.mult))
                T("dve", lambda e: e.tensor_tensor(out=t1[:], in0=t1[:], in1=t2[:], op=ALU.add))
                T("dve", lambda e: e.reciprocal(out=t1[:], in_=t1[:]))
                T("dve", lambda e: e.tensor_tensor(out=NPr[:, 1, :], in0=abr, in1=t1[:], op=ALU.mult))
                T("dve", lambda e: e.scalar_tensor_tensor(out=NPi[:, 1, :], in0=abi, scalar=-1.0, in1=t1[:], op0=ALU.mult, op1=ALU.mult))

                def cmul(orr, oii, ar_, ai_, br_, bi_, tA, tB):
                    T("dve", lambda e: e.tensor_tensor(out=tA, in0=ar_, in1=br_, op=ALU.mult))
                    T("dve", lambda e: e.tensor_tensor(out=tB, in0=ai_, in1=bi_, op=ALU.mult))
                    T("dve", lambda e: e.tensor_tensor(out=tA, in0=tA, in1=tB, op=ALU.subtract))
                    T("dve", lambda e: e.tensor_tensor(out=tB, in0=ar_, in1=bi_, op=ALU.mult))
                    T("dve", lambda e: e.tensor_tensor(out=oii, in0=ai_, in1=br_, op=ALU.mult))
                    T("dve", lambda e: e.tensor_tensor(out=oii, in0=oii, in1=tB, op=ALU.add))
                    T("dve", lambda e: e.tensor_copy(out=orr, in_=tA))

                for k in range(2, 9):
                    cmul(PWr[:, k, :], PWi[:, k, :], PWr[:, k - 1, :], PWi[:, k - 1, :], abr, abi, t1[:], t2[:])
                    yield
                for k in range(2, 8):
                    cmul(NPr[:, k, :], NPi[:, k, :], NPr[:, k - 1, :], NPi[:, k - 1, :], NPr[:, 1, :], NPi[:, 1, :], t1[:], t2[:])
                    yield
                lo, hi = slice(0, 64), slice(64, 128)
                for s_ in range(8):
                    for (dst, pr_, pi_) in ((BB, NPr[:, s_, :], NPi[:, s_, :]), (WW, PWr[:, 7 - s_, :], PWi[:, 7 - s_, :])):
                        T("dve", lambda e: e.tensor_tensor(out=u1[lo], in0=bbr[lo], in1=bc3(pr_[lo], 16), op=ALU.mult))
                        T("dve", lambda e: e.tensor_tensor(out=u2[lo], in0=bbi[lo], in1=bc3(pi_[lo], 16), op=ALU.mult))
                        T("dve", lambda e: e.tensor_tensor(out=dst[lo, :, s_, :], in0=u1[lo], in1=u2[lo], op=ALU.subtract))
                        T("dve", lambda e: e.tensor_tensor(out=u1[hi], in0=bbi[hi], in1=bc3(pr_[hi], 16), op=ALU.mult))
                        T("dve", lambda e: e.tensor_tensor(out=u2[hi], in0=bbr[hi], in1=bc3(pi_[hi], 16), op=ALU.mult))
                        T("dve", lambda e: e.tensor_tensor(out=dst[hi, :, s_, :], in0=u1[hi], in1=u2[hi], op=ALU.add))
                        yield
                for k in range(9):
                    pr_, pi_ = PWr[:, k, :], PWi[:, k, :]
                    T("dve", lambda e: e.tensor_tensor(out=u1[lo], in0=cre[lo], in1=bc3(pr_[lo], 16), op=ALU.mult))
                    T("dve", lambda e: e.tensor_tensor(out=u2[lo], in0=cim[lo], in1=bc3(pi_[lo], 16), op=ALU.mult))
                    T("dve", lambda e: e.tensor_tensor(out=CC[lo, :, k, :], in0=u1[lo], in1=u2[lo], op=ALU.subtract))
                    T("dve", lambda e: e.tensor_tensor(out=u1[hi], in0=cre[hi], in1=bc3(pi_[hi], 16), op=ALU.mult))
                    T("dve", lambda e: e.tensor_tensor(out=u2[hi], in0=cim[hi], in1=bc3(pr_[hi], 16), op=ALU.mult))
                    T("dve", lambda e: e.scalar_tensor_tensor(out=CC[hi, :, k, :], in0=u1[hi], scalar=-1.0, in1=u2[hi],
                                                              op0=ALU.mult, op1=ALU.subtract))
                    yield
                T("dve", lambda e: e.tensor_copy(out=Ctab[:].rearrange("p g (k c) -> p g k c", k=8), in_=CC[:, :, 1:9, :]))
                T("pool", lambda e: e.memset(maskLT[:], 1.0))
                T("pool", lambda e: e.affine_select(out=maskLT[:], in_=maskLT[:], pattern=[[16, 8], [0, 16]],
                                                    compare_op=ALU.is_ge, fill=0.0, base=15, channel_multiplier=-1))
                jt2 = f32t("jt2", [128, 128])
                T("pool", lambda e: e.memset(Jt[:], 1.0))
                T("pool", lambda e: e.affine_select(out=Jt[:], in_=Jt[:], pattern=[[1, 128]], compare_op=ALU.is_equal,
                                                    fill=0.0, base=-64, channel_multiplier=-1))
                T("pool", lambda e: e.memset(jt2[:], 1.0))
                T("pool", lambda e: e.affine_select(out=jt2[:], in_=jt2[:], pattern=[[1, 128]], compare_op=ALU.is_equal,
                                                    fill=0.0, base=64, channel_multiplier=-1))
                T("dve", lambda e: e.tensor_tensor(out=Jt[:], in0=Jt[:], in1=jt2[:], op=ALU.subtract))
                ktmp = f32t("ktmp", [128, 128])
                for g in range(32):
                    pk, B_pk = pss.next()
                    S.op("pe", lambda e: e.matmul(pk[:, 0:128], lhsT=BB[:, g, :, :].rearrange("p a b -> p (a b)"), rhs=CC[:, g, 0:8, :].rearrange("p a b -> p (a b)"), start=True, stop=True),
                         r=[B_tab], w=[B_pk])
                    S.op("dve", lambda e: e.tensor_tensor(out=ktmp[:], in0=pk[:, 0:128], in1=maskLT[:].rearrange("p a b -> p (a b)"),
                                                          op=ALU.mult), r=[B_pk, B_tab], w=[B_tab])
                    T("dve", lambda e: e.scalar_tensor_tensor(out=Ktoep[:, g, :], in0=ident_f[:], scalar=dcol[:, g:g + 1],
                                                              in1=ktmp[:], op0=ALU.mult, op1=ALU.add))
                    pw, B_pw = pss.next()
                    S.op("pe", lambda e: e.transpose(out=pw[:, 0:128], in_=WW[:, g, :, :].rearrange("p a b -> p (a b)"), identity=ident_f[:]),
                         r=[B_tab, B_const], w=[B_pw])
                    S.op("act", lambda e: e.activation(out=W1t[:, g, :], in_=pw[:, 0:128], func=AF.Copy), r=[B_pw], w=[B_tab])
                    yield
                T("dve", lambda e: e.reciprocal(out=t3[:], in_=rho[:]))
                T("dve", lambda e: e.tensor_tensor(out=cosT[:, :, 0], in0=PWr[:, 8, :], in1=t3[:], op=ALU.mult))
                T("dve", lambda e: e.tensor_tensor(out=sinT[:, :, 0], in0=PWi[:, 8, :], in1=t3[:], op=ALU.mult))
                e1, e2 = f32t("e1", [128, 32, 32]), f32t("e2", [128, 32, 32])
                m = 1
                while m < 64:
                    br_ = cosT[:, :, m - 1:m].to_broadcast([128, 32, m])
                    bi_ = sinT[:, :, m - 1:m].to_broadcast([128, 32, m])
                    cmul(cosT[:, :, m:2 * m], sinT[:, :, m:2 * m], cosT[:, :, 0:m], sinT[:, :, 0:m], br_, bi_,
                         e1[:, :, 0:m], e2[:, :, 0:m])
                    m *= 2
                    yield

            tgen = tab_gen()
            for nb in range(24):
                i = nb % 2
                S.dma("sp", wab[i][:], wada_v[:, :, nb * 256:(nb + 1) * 256], B_wab[i], True)
                S.op("pe", [(lambda e, k=k: e.matmul(pr[i][0:1, 0:256], lhsT=cact[:, k:k + 1], rhs=wab[i][:, k, :],
                                                     start=(k == 0), stop=(k == 7))) for k in range(8)],
                     r=[B_cact, B_wab[i]], w=[B_pr[i]])
                S.op("dve", lambda e: e.tensor_tensor(out=modrow[0:1, nb * 256:(nb + 1) * 256], in0=pr[i][0:1, 0:256],
                                                      in1=modrow[0:1, nb * 256:(nb + 1) * 256], op=ALU.add),
                     r=[B_pr[i]], w=[B_mod])
                for _ in range(4):
                    next(tgen, None)
            for _ in tgen:
                pass
            for t_, dst_ in ((W1t, scr_W1t), (Ktoep, scr_Ktoep), (Ctab, scr_Ctab), (cosT, scr_cosT), (sinT, scr_sinT),
                             (rho, scr_rho), (Jt, scr_Jt)):
                S.dma("pool", dst_, t_[:], B_tab, False)
            cols = [(0, 0), (1, 8), (3, 16), (4, 24)]
            S.op("pe", [(lambda e, j=j, o=o, k=k: e.matmul(pc[:, o + k:o + k + 1],
                                                           lhsT=modrow[0:1, j * D + k * 128:j * D + (k + 1) * 128],
                                                           rhs=ones_f[0:1, 0:1], start=True, stop=True))
                        for (j, o) in cols for k in range(8)], r=[B_mod, B_const], w=[B_pc])
            S.op("dve", lambda e: e.tensor_copy(out=sh_m[:], in_=pc[:, 0:8]), r=[B_pc], w=[B_mod])
            S.op("dve", lambda e: e.tensor_copy(out=sh_f[:], in_=pc[:, 16:24]), r=[B_pc], w=[B_mod])
            S.op("dve", lambda e: e.scalar_tensor_tensor(out=gmod_m[:], in0=pc[:, 8:16], scalar=1.0, in1=gpm[:],
                                                         op0=ALU.add, op1=ALU.mult), r=[B_pc, B_g], w=[B_mod])
            S.op("dve", lambda e: e.scalar_tensor_tensor(out=gmod_f[:], in0=pc[:, 24:32], scalar=1.0, in1=gpf[:],
                                                         op0=ALU.add, op1=ALU.mult), r=[B_pc, B_g], w=[B_mod])
            S.op("dve", lambda e: e.tensor_tensor(out=rprod[0:1, 0:D], in0=modrow[0:1, 2 * D:3 * D],
                                                  in1=grow[0:1, 0:D], op=ALU.mult), r=[B_mod, B_g], w=[B_rp])
            S.op("dve", lambda e: e.tensor_tensor(out=rprod[0:1, D:2 * D], in0=modrow[0:1, 5 * D:6 * D],
                                                  in1=grow[0:1, D:2 * D], op=ALU.mult), r=[B_mod, B_g], w=[B_rp])
            for j, dst in ((0, bc_m), (1, bc_f)):
                for hh in range(2):
                    S.op("pe", lambda e: e.matmul(pr[hh][:, :], lhsT=ones_f[0:1, :],
                                                  rhs=rprod[0:1, j * D + hh * 512:j * D + (hh + 1) * 512],
                                                  start=True, stop=True), r=[B_rp, B_const], w=[B_pr[hh]])
                    S.op("act", lambda e: e.activation(out=dst[:, hh * 512:(hh + 1) * 512], in_=pr[hh][:, :],
                                                       func=AF.Copy), r=[B_pr[hh]], w=[B_mod])
            if debug:
                d = dbg_out("modrow", [1, 6 * D])
                S.dma("sp", d, modrow[:], B_mod, False)
                d = dbg_out("bc_m", [128, D])
                S.dma("sp", d, bc_m[:], B_mod, False)
                d = dbg_out("gmod_m", [128, 8])
                S.dma("sp", d, gmod_m[:], B_mod, False)
            S.barrier()
            S.release([B_cc, B_g, B_tab, B_mod] + B_wab)


        def load_cast_weight(stack_tmp, dst, src_view, ncols, B_dst, stg, B_stg, engs=("dve", "act"), doff=0):
            nblk = (ncols + 255) // 256
            for cbk in range(nblk):
                c0 = cbk * 256
                c1 = min(ncols, c0 + 256)
                i = load_cast_weight.n % len(stg)
                load_cast_weight.n += 1
                kk = src_view.shape[1]
                S.dma("sp", stg[i][:, 0:kk, 0:c1 - c0], src_view[:, :, c0:c1], B_stg[i], True)
                eng = engs[cbk % len(engs)]
                if eng == "act":
                    S.op("act", lambda e: e.activation(out=dst[:, :, doff + c0:doff + c1], in_=stg[i][:, 0:kk, 0:c1 - c0], func=AF.Copy),
                         r=[B_stg[i]], w=[B_dst])
                else:
                    S.op(eng, lambda e: e.tensor_copy(out=dst[:, :, doff + c0:doff + c1], in_=stg[i][:, 0:kk, 0:c1 - c0]),
                         r=[B_stg[i]], w=[B_dst])
        load_cast_weight.n = 0

        if upto < 1:
            S.barrier()
            return nc, dbg

        with ExitStack() as ps:
            wukv = sbt(ps, "wukv", [128, 8, 1536], BF16)
            stg = [sbt(ps, "stg%d" % i, [128, 8, 256], F32) for i in range(2)]
            B_stg = [Buf("stg0"), Buf("stg1")]
            B_wukv = Buf("wukv")
            win_v = w_in.rearrange("(k p) n -> p k n", p=128)
            load_cast_weight(ps, wukv, win_v[:, :, 0:512], 512, B_wukv, stg, B_stg)
            load_cast_weight(ps, wukv, win_v[:, :, 1024:2048], 1024, B_wukv, stg, B_stg, doff=512)
            xt = [sbt(ps, "xt%d" % i, [128, 8, D], F32) for i in range(2)]
            B_xt = [Buf("xt0"), Buf("xt1")]
            junk = sbt(ps, "junk", [128, D], BF16)
            B_junk = Buf("junk")
            ssq = sbt(ps, "ssq", [128, 8], F32)
            rstd = sbt(ps, "rstd", [128, 8], F32)
            B_ssq, B_rstd = Buf("ssq"), Buf("rstd")
            xn = sbt(ps, "xn", [128, 8, D], BF16)
            B_xn = Buf("xn")
            hnT = sbt(ps, "hnT", [128, 8, TT], BF16)
            B_hnT = Buf("hnT")
            u_cm = sbt(ps, "u_cm", [128, 32, 8, 16], BF16)
            v_cm = sbt(ps, "v_cm", [128, 8, 512], BF16)
            kT = sbt(ps, "kT", [128, 4, TT], BF16)
            B_ucm, B_vcm, B_kT = Buf("ucm"), Buf("vcm"), Buf("kT")
            km_f = sbt(ps, "km_f", [128, 4, 4], F32)
            B_kmf = Buf("kmf")
            ptr = Ring([pst(ps, "ptr%d" % i, [128, TT], BF16) for i in range(2)], "ptr")
            pmm = Ring([pst(ps, "pmm%d" % i, [128, 512], F32) for i in range(4)], "pmm")
            xo_v = xo.rearrange("(t c s) d -> t c s d", c=128, s=8)
            xp_v = xp.rearrange("(t c s) d -> t c s d", c=128, s=8)
            evac_i = [0]

            def evac_copy(dst_ap, src_ap, r, w, scale=None):
                evac_i[0] += 1
                if evac_i[0] % 2 == 0:
                    if scale is None:
                        S.op("act", lambda e: e.activation(out=dst_ap, in_=src_ap, func=AF.Copy), r=r, w=w)
                    else:
                        S.op("act", lambda e: e.activation(out=dst_ap, in_=src_ap, func=AF.Copy, scale=scale), r=r, w=w)
                else:
                    if scale is None:
                        S.op("dve", lambda e: e.tensor_copy(out=dst_ap, in_=src_ap), r=r, w=w)
                    else:
                        S.op("dve", lambda e: e.tensor_scalar(out=dst_ap, in0=src_ap, scalar1=scale, scalar2=None,
                                                              op0=ALU.mult), r=r, w=w)

            def normA(xsrc_ap, xi):
                S.dma("sp", xt[xi][:], xsrc_ap, B_xt[xi], True)
                for s_ in range(8):
                    S.op("act", lambda e: e.activation(out=junk[:], in_=xt[xi][:, s_, :], func=AF.Square,
                                                       accum_out=ssq[:, s_:s_ + 1]), r=[B_xt[xi]], w=[B_junk, B_ssq])
                S.op("act", lambda e: e.activation(out=rstd[:], in_=ssq[:], func=AF.Sqrt, scale=1.0 / D, bias=EPS),
                     r=[B_ssq], w=[B_rstd])
                S.op("dve", lambda e: e.reciprocal(out=rstd[:], in_=rstd[:]), r=[B_rstd], w=[B_rstd])
                for s_ in range(8):
                    eng = "dve"
                    S.op(eng, lambda e: e.tensor_scalar(out=xn[:, s_, :], in0=xt[xi][:, s_, :],
                                                        scalar1=rstd[:, s_:s_ + 1], scalar2=None, op0=ALU.mult),
                         r=[B_xt[xi], B_rstd], w=[B_xn])

            def normT(gmod, shc):
                for k in range(8):
                    pt, B_pt = ptr.next()
                    S.op("pe", [(lambda e, s_=s_: e.transpose(out=pt[:, s_ * 128:(s_ + 1) * 128],
                                                              in_=xn[:, s_, k * 128:(k + 1) * 128], identity=ident_b[:]))
                                for s_ in range(8)], r=[B_xn, B_const], w=[B_pt])
                    if k % 2 == 0:
                        S.op("dve", lambda e: e.tensor_scalar(out=hnT[:, k, :], in0=pt[:, :], scalar1=gmod[:, k:k + 1],
                                                              scalar2=shc[:, k:k + 1], op0=ALU.mult, op1=ALU.add),
                             r=[B_pt, B_mod], w=[B_hnT])
                    else:
                        S.op("act", lambda e: e.activation(out=hnT[:, k, :], in_=pt[:, :], func=AF.Identity,
                                                           scale=gmod[:, k:k + 1], bias=shc[:, k:k + 1]),
                             r=[B_pt, B_mod], w=[B_hnT])

            for gt in range(2 * NT):
                own = gt >= NT
                ot = gt - NT
                if gt == 0:
                    normA(xp_v[0], 0)
                normT(gmod_m, sh_m)
                if gt + 1 < 2 * NT:
                    g2 = gt + 1
                    normA(xo_v[g2 - NT] if g2 >= NT else xp_v[g2], g2 % 2)
                if own:
                    S.dma("pool", scr_hn[ot], hnT[:], B_hnT, False)
                for s_ in range(8):
                    pu, B_pu = pmm.next()
                    S.op("pe", [(lambda e, k=k: e.matmul(pu[:, :], lhsT=hnT[:, k, s_ * 128:(s_ + 1) * 128],
                                                         rhs=wukv[:, k, 0:512], start=(k == 0), stop=(k == 7)))
                                for k in range(8)], r=[B_hnT, B_wukv], w=[B_pu])
                    evac_copy(u_cm[:, :, s_, :], pu[:, :].rearrange("p (g c) -> p g c", g=32), [B_pu], [B_ucm])
                    pv, B_pv = pmm.next()
                    S.op("pe", [(lambda e, k=k: e.matmul(pv[:, :], lhsT=hnT[:, k, s_ * 128:(s_ + 1) * 128],
                                                         rhs=wukv[:, k, 1024:1536], start=(k == 0), stop=(k == 7)))
                                for k in range(8)], r=[B_hnT, B_wukv], w=[B_pv])
                    evac_copy(v_cm[:, s_, :], pv[:, :], [B_pv], [B_vcm])
                S.dma("pool", scr_v[gt].rearrange("s c f -> c s f"), v_cm[:], B_vcm, False)
                S.dma("pool", scr_u[gt], u_cm[:], B_ucm, False)
                for cb in range(4):
                    for hf in range(2):
                        pk, B_pk = pmm.next()
                        S.op("pe", [(lambda e, k=k: e.matmul(pk[:, :], lhsT=wukv[:, k, 512 + cb * 128:512 + (cb + 1) * 128],
                                                             rhs=hnT[:, k, hf * 512:(hf + 1) * 512],
                                                             start=(k == 0), stop=(k == 7))) for k in range(8)],
                             r=[B_hnT, B_wukv], w=[B_pk])
                        evac_copy(kT[:, cb, hf * 512:(hf + 1) * 512], pk[:, :], [B_pk], [B_kT])
                S.dma("pool", scr_kt[:, :, gt * TT:(gt + 1) * TT].rearrange("b p n -> p b n"), kT[:], B_kT, False)
                S.op("dve", lambda e: e.tensor_reduce(out=km_f[:], in_=kT[:].rearrange("p b (s k c) -> p b k s c", s=8, k=4, c=32),
                                                      axis=AX.XY, op=ALU.add), r=[B_kT], w=[B_kmf])
                S.op("dve", lambda e: e.tensor_scalar(out=kmean[:, :, gt * 4:(gt + 1) * 4], in0=km_f[:], scalar1=1.0 / 256.0,
                                                      scalar2=None, op0=ALU.mult), r=[B_kmf], w=[B_kmean])
                if debug and gt == 0:
                    for nm, t_, B_, shp, dt_ in (("hnT", hnT, B_hnT, [128, 8, TT], BF16), ("u_cm", u_cm, B_ucm, [128, 32, 8, 16], BF16),
                                                 ("v_cm", v_cm, B_vcm, [128, 8, 512], BF16), ("kT", kT, B_kT, [128, 4, TT], BF16)):
                        S.dma("sp", dbg_out(nm, shp, dt_), t_[:], B_, False)
            if debug:
                S.dma("sp", dbg_out("kmean", [128, 4, 32], BF16), kmean[:], B_kmean, False)
            S.barrier()
            S.release([B_wukv] + B_stg + B_xt + [B_hnT, B_vcm, B_kT, B_ucm, B_kmean])


        if upto < 2:
            S.barrier()
            return nc, dbg

        with ExitStack() as ps:
            W1t = sbt(ps, "W1t", [128, 32, 128], BF16)
            Ktoep = sbt(ps, "Ktoep", [128, 32, 128], BF16)
            Ctab = sbt(ps, "Ctab", [128, 32, 128], BF16)
            cosT = sbt(ps, "cosT", [128, 32, 64], F32)
            sinT = sbt(ps, "sinT", [128, 32, 64], F32)
            rho = sbt(ps, "rho", [128, 32], F32)
            Jt = sbt(ps, "Jt", [128, 128], F32)
            B_tab = Buf("tab")
            pss = Ring([pst(ps, "pss%d" % i, [128, 512], F32) for i in range(6)], "pss")
            ptb = Ring([pst(ps, "ptb%d" % i, [128, 1024], BF16) for i in range(2)], "ptb")

            for t_, src_ in ((W1t, scr_W1t), (Ktoep, scr_Ktoep), (Ctab, scr_Ctab), (cosT, scr_cosT), (sinT, scr_sinT),
                             (rho, scr_rho), (Jt, scr_Jt)):
                S.dma("sp", t_[:], src_, B_tab, True)
            ucm = [sbt(ps, "ucm%d" % i, [128, 32, 128], BF16) for i in range(2)]
            B_ucm2 = [Buf("ucm0"), Buf("ucm1")]
            U = sbt(ps, "U", [128, 32, 128], BF16)
            B_U = [Buf("U%d" % i) for i in range(4)]
            SX = sbt(ps, "SX", [128, 32, 128], F32)
            B_SX = [Buf("SX%d" % i) for i in range(8)]
            Wh = sbt(ps, "Wh", [128, 32, 64], F32)
            B_Wh = [Buf("Wh%d" % i) for i in range(4)]
            Xprev = sbt(ps, "Xprev", [128, 32, 128], BF16)
            B_Xp = Buf("Xprev")
            carry = sbt(ps, "carry", [128, 32], F32)
            carry2 = sbt(ps, "carry2", [128, 32], F32)
            B_carry, B_carry2 = Buf("carry"), Buf("carry2")
            tmp2 = [sbt(ps, "tmp2_%d" % i, [128, 8, 64], F32) for i in range(2)]
            B_tmp2 = [Buf("tmp2_0"), Buf("tmp2_1")]
            zg = sbt(ps, "zg", [128, 32, 128], BF16)
            B_zg = [Buf("zg%d" % i) for i in range(8)]
            z_cm = sbt(ps, "z_cm", [128, 8, 512], BF16)
            B_zcm = Buf("z_cm")
            zT = sbt(ps, "zT", [128, 4, TT], BF16)
            B_zT = Buf("zT")
            S.op("dve", lambda e: e.memset(carry[:], 0.0), w=[B_carry])
            tcount = [0]
            Zh = [sbt(ps, "Zh%d" % i, [128, 32, 64], F32) for i in range(2)]
            B_Zh = [Buf("Zh0"), Buf("Zh1")]
            rhoT = sbt(ps, "rhoT", [128, 32, 64], F32)
            tmpc = sbt(ps, "tmpc", [128, 32], F32)
            B_tmpc = Buf("tmpc")
            S.op("dve", lambda e: e.tensor_copy(out=rhoT[:], in_=rho[:].unsqueeze(2).to_broadcast([128, 32, 64])), r=[B_tab], w=[B_tab])
            S.op("dve", lambda e: e.memset(rhoT[:, :, 0], 0.0), r=[B_tab], w=[B_tab])

            def scan_half(hf, init_buf_ap, B_init):
                S.op("dve", lambda e: e.tensor_tensor(out=tmpc[:], in0=rho[:], in1=init_buf_ap[:], op=ALU.mult),
                     r=[B_init, B_tab], w=[B_tmpc])
                S.op("dve", lambda e: e.tensor_tensor(out=Zh[hf][:, :, 0], in0=Zh[hf][:, :, 0], in1=tmpc[:], op=ALU.add),
                     r=[B_tmpc], w=[B_Zh[hf]])
                S.op("dve", lambda e: e.tensor_tensor_scan(out=Wh[:].rearrange("p g c -> p (g c)"),
                                                           data0=rhoT[:].rearrange("p g c -> p (g c)"),
                                                           data1=Zh[hf][:].rearrange("p g c -> p (g c)"), initial=0.0,
                                                           op0=ALU.mult, op1=ALU.add),
                     r=[B_Zh[hf], B_tab], w=B_Wh)

            def last_state(dst, B_dst):
                pj, B_pj = pss.next()
                S.op("pe", lambda e: e.matmul(pj[:, 0:32], lhsT=Jt[:], rhs=Wh[:, :, 63], start=True, stop=True),
                     r=B_Wh + [B_tab], w=[B_pj])
                S.op("dve", lambda e: e.tensor_tensor(out=tmpc[:], in0=pj[:, 0:32], in1=sinT[:, :, 63], op=ALU.mult),
                     r=[B_pj, B_tab], w=[B_tmpc])
                S.op("dve", lambda e: e.tensor_tensor(out=dst[:], in0=Wh[:, :, 63], in1=cosT[:, :, 63], op=ALU.mult),
                     r=B_Wh + [B_tab], w=[B_dst])
                S.op("dve", lambda e: e.tensor_tensor(out=dst[:], in0=dst[:], in1=tmpc[:], op=ALU.add), r=[B_tmpc], w=[B_dst])

            def rot_unrot(hf, init_buf_ap, B_init):
                c0 = hf * 64
                scan_half(hf, init_buf_ap, B_init)
                for q in range(4):
                    pj, B_pj = pss.next()
                    S.op("pe", lambda e: e.matmul(pj[:, :], lhsT=Jt[:], rhs=Wh[:, 8 * q:8 * q + 8, :].rearrange("p g c -> p (g c)"), start=True, stop=True),
                         r=[B_Wh[q], B_tab], w=[B_pj])
                    i2 = tcount[0] % 2
                    tcount[0] += 1
                    S.op("dve", lambda e: e.tensor_tensor(out=tmp2[i2][:], in0=pj[:, :].rearrange("p (g c) -> p g c", g=8),
                                                          in1=sinT[:, 8 * q:8 * q + 8, :], op=ALU.mult),
                         r=[B_pj, B_tab], w=[B_tmp2[i2]])
                    sxv = SX[:, 8 * q:8 * q + 8, c0:c0 + 64]
                    S.op("dve", lambda e: e.tensor_tensor(out=sxv, in0=Wh[:, 8 * q:8 * q + 8, :], in1=cosT[:, 8 * q:8 * q + 8, :],
                                                           op=ALU.mult), r=[B_Wh[q], B_tab], w=[B_SX[2 * q], B_SX[2 * q + 1]])
                    S.op("dve", lambda e: e.tensor_tensor(out=sxv, in0=sxv, in1=tmp2[i2][:], op=ALU.add),
                         r=[B_tmp2[i2]], w=[B_SX[2 * q], B_SX[2 * q + 1]])

            for gt in range(2 * NT):
                own = gt >= NT
                ot = gt - NT
                ui = gt % 2
                S.dma("sp", ucm[ui][:], scr_u[gt].rearrange("p g s c -> p g (s c)"), B_ucm2[ui], True)
                for q in range(4):
                    pt, B_pt = ptb.next()
                    S.op("pe", [(lambda e, j=j: e.transpose(out=pt[:, j * 128:(j + 1) * 128],
                                                            in_=ucm[ui][:, 8 * q + j, :],
                                                            identity=ident_b[:])) for j in range(8)],
                         r=[B_ucm2[ui], B_const], w=[B_pt])
                    evac_copy2 = "act" if q % 2 == 0 else "dve"
                    if evac_copy2 == "act":
                        S.op("act", lambda e: e.activation(out=U[:, 8 * q:8 * q + 8, :], in_=pt[:, :].rearrange("p (g c) -> p g c", g=8),
                                                           func=AF.Copy), r=[B_pt], w=[B_U[q]])
                    else:
                        S.op("dve", lambda e: e.tensor_copy(out=U[:, 8 * q:8 * q + 8, :], in_=pt[:, :].rearrange("p (g c) -> p g c", g=8)),
                             r=[B_pt], w=[B_U[q]])
                S.op("dve", lambda e: e.tensor_copy(out=Xprev[:, :, 0], in_=carry[:]), r=[B_carry], w=[B_Xp])
                for gb in range(8):
                    pa, B_pa = pss.next()
                    S.op("pe", [(lambda e, j=j: e.matmul(pa[:, j * 128:(j + 1) * 128], lhsT=W1t[:, 4 * gb + j, :],
                                                         rhs=U[:, 4 * gb + j, :], start=True, stop=True)) for j in range(4)],
                         r=[B_U[gb // 2], B_tab], w=[B_pa])
                    S.op("act", lambda e: e.activation(out=SX[:, 4 * gb:4 * gb + 4, :], in_=pa[:, :].rearrange("p (g c) -> p g c", g=4),
                                                       func=AF.Copy), r=[B_pa], w=[B_SX[gb]])
                    pj, B_pj = pss.next()
                    S.op("pe", lambda e: e.matmul(pj[:, :], lhsT=Jt[:], rhs=SX[:, 4 * gb:4 * gb + 4, :].rearrange("p g c -> p (g c)"), start=True, stop=True),
                         r=[B_SX[gb], B_tab], w=[B_pj])
                    for hf in range(2):
                        i2 = tcount[0] % 2
                        tcount[0] += 1
                        pjv = pj[:, :].rearrange("p (g c) -> p g c", g=4)[:, :, hf * 64:(hf + 1) * 64]
                        S.op("dve", lambda e: e.tensor_tensor(out=tmp2[i2][:, 0:4, :], in0=pjv, in1=sinT[:, 4 * gb:4 * gb + 4, :],
                                                              op=ALU.mult), r=[B_pj, B_tab], w=[B_tmp2[i2]])
                        sxv = SX[:, 4 * gb:4 * gb + 4, hf * 64:(hf + 1) * 64]
                        S.op("dve", lambda e: e.tensor_tensor(out=sxv, in0=sxv, in1=cosT[:, 4 * gb:4 * gb + 4, :], op=ALU.mult),
                             r=[B_tab], w=[B_SX[gb]])
                        S.op("dve", lambda e: e.tensor_tensor(out=Zh[hf][:, 4 * gb:4 * gb + 4, :], in0=sxv, in1=tmp2[i2][:, 0:4, :], op=ALU.subtract),
                             r=[B_tmp2[i2], B_SX[gb]], w=[B_Zh[hf]])
                if own:
                    rot_unrot(0, carry, B_carry)
                    S.op("dve", lambda e: e.tensor_copy(out=carry2[:], in_=SX[:, :, 63]), r=B_SX, w=[B_carry2])
                    rot_unrot(1, carry2, B_carry2)
                    S.op("dve", lambda e: e.tensor_copy(out=carry[:], in_=SX[:, :, 127]), r=B_SX, w=[B_carry])
                else:
                    scan_half(0, carry, B_carry)
                    last_state(carry2, B_carry2)
                    scan_half(1, carry2, B_carry2)
                    last_state(carry, B_carry)
                if gt == NT - 1:
                    S.op("dve", lambda e: e.tensor_scalar(out=carry[:], in0=carry[:], scalar1=flag_sb[:, 0:1], scalar2=None,
                                                          op0=ALU.mult), r=[B_carry, B_const], w=[B_carry])
                if not own:
                    continue
                S.op("act", lambda e: e.activation(out=Xprev[:, :, 1:128], in_=SX[:, :, 0:127], func=AF.Copy), r=B_SX, w=[B_Xp])
                for gb in range(8):
                    py, B_py = pss.next()
                    fns = []
                    for j in range(4):
                        g = 4 * gb + j
                        fns.append(lambda e, j=j, g=g: e.matmul(py[:, j * 128:(j + 1) * 128], lhsT=Ktoep[:, g, :], rhs=U[:, g, :],
                                                                start=True, stop=False))
                        fns.append(lambda e, j=j, g=g: e.matmul(py[:, j * 128:(j + 1) * 128], lhsT=Ctab[:, g, :], rhs=Xprev[:, g, :],
                                                                start=False, stop=True))
                    S.op("pe", fns, r=[B_U[gb // 2], B_Xp, B_tab], w=[B_py])
                    S.op("act", lambda e: e.activation(out=zg[:, 4 * gb:4 * gb + 4, :], in_=py[:, :].rearrange("p (g c) -> p g c", g=4),
                                                       func=AF.Gelu), r=[B_py], w=[B_zg[gb]])
                for q in range(4):
                    pt, B_pt = ptb.next()
                    S.op("pe", [(lambda e, j=j: e.transpose(out=pt[:, j * 128:(j + 1) * 128], in_=zg[:, 8 * q + j, :],
                                                            identity=ident_b[:])) for j in range(8)],
                         r=[B_zg[2 * q], B_zg[2 * q + 1], B_const], w=[B_pt])
                    S.op("dve", lambda e: e.tensor_copy(out=z_cm[:, :, q * 128:(q + 1) * 128].rearrange("p t (g c) -> p g t c", g=8),
                                                        in_=pt[:, :].rearrange("p (g t c) -> p g t c", g=8, t=8)),
                         r=[B_pt], w=[B_zcm])
                for blk in range(4):
                    pt, B_pt = ptb.next()
                    S.op("pe", [(lambda e, t_=t_: e.transpose(out=pt[:, t_ * 128:(t_ + 1) * 128],
                                                              in_=z_cm[:, t_, blk * 128:(blk + 1) * 128], identity=ident_b[:]))
                                for t_ in range(8)], r=[B_zcm, B_const], w=[B_pt])
                    if blk % 2 == 0:
                        S.op("act", lambda e: e.activation(out=zT[:, blk, :], in_=pt[:, :], func=AF.Copy), r=[B_pt], w=[B_zT])
                    else:
                        S.op("dve", lambda e: e.tensor_copy(out=zT[:, blk, :], in_=pt[:, :]), r=[B_pt], w=[B_zT])
                S.dma("pool", scr_z[:, :, ot * TT:(ot + 1) * TT].rearrange("b p n -> p b n"), zT[:], B_zT, False)
                if debug and ot == 0:
                    S.dma("sp", dbg_out("z_cm", [128, 8, 512], BF16), z_cm[:], B_zcm, False)
                    S.dma("sp", dbg_out("SX", [128, 32, 128], F32), SX[:], B_SX[0], False, extra_r=B_SX)
            S.barrier()
            S.release([B_tab, B_zT, B_zcm] + B_ucm2 + B_SX)


        if upto < 3:
            S.barrier()
            return nc, dbg

        ps12 = ExitStack()
        st12 = ExitStack()
        Kaug = [sbt(ps12, "Kaug%d" % i, [128, 2 * TOK], BF16) for i in range(2)]
        QAbase = sbt(ps12, "QAbase", [128, TOK], BF16)
        CB = sbt(ps12, "CB", [128, 8, 8, 128], BF16)
        VB = sbt(ps12, "VB", [128, NT, 32], F32)
        OWNM = sbt(ps12, "OWNM", [128, NT, 32], F32)
        B_st = Buf("p2static")
        wqg = sbt(st12, "wqg", [128, 8, 2560], BF16)
        B_wqg = Buf("wqg")
        with ExitStack() as st:
            stg = [sbt(st, "stgb%d" % i, [128, 8, 256], F32) for i in range(2)]
            B_stg = [Buf("stgb0"), Buf("stgb1")]
            load_cast_weight(st, wqg, win_v[:, :, 512:1024], 512, B_wqg, stg, B_stg)
            load_cast_weight(st, wqg, win_v[:, :, 2048:4096], 2048, B_wqg, stg, B_stg, doff=512)
            S.barrier()
            S.release(B_stg)
        def P2s(eng, fn):
            S.op(eng, fn, r=[B_st, B_const], w=[B_st])

        st = st12
        f32t = lambda nm, shp: sbt(st, nm, shp, F32)
        pidx = f32t("pidx", [128, 1])
        cA, cB_, cC = f32t("cA", [128, 1]), f32t("cB", [128, 1]), f32t("cC", [128, 1])
        qA, qB, qC = f32t("qA", [128, 1]), f32t("qB", [128, 1]), f32t("qC", [128, 1])
        p64 = f32t("p64", [128, 1])
        shi, slo, blk = f32t("shi", [128, TT]), f32t("slo", [128, TT]), f32t("blkt", [128, TT])
        ka = f32t("ka", [128, TT])
        cble, cblt = f32t("cble", [128, 128]), f32t("cblt", [128, 128])
        P2s("pool", lambda e: e.iota(pidx[:], pattern=[[0, 1]], base=0, channel_multiplier=1,
                                     allow_small_or_imprecise_dtypes=True))
        P2s("dve", lambda e: e.tensor_scalar(out=p64[:], in0=pidx[:], scalar1=-64.0, scalar2=None, op0=ALU.add))
        for (dst, val) in ((cA, 98.0), (cB_, 99.0), (qA, 96.0), (qB, 97.0)):
            P2s("dve", lambda e: e.tensor_scalar(out=dst[:], in0=pidx[:], scalar1=val, scalar2=None, op0=ALU.is_equal))
        P2s("dve", lambda e: e.tensor_tensor(out=cC[:], in0=qA[:], in1=qB[:], op=ALU.add))
        P2s("dve", lambda e: e.tensor_tensor(out=qC[:], in0=cA[:], in1=cB_[:], op=ALU.add))
        P2s("dve", lambda e: e.tensor_scalar(out=qA[:], in0=qA[:], scalar1=-1.0, scalar2=None, op0=ALU.mult))
        P2s("dve", lambda e: e.tensor_scalar(out=qB[:], in0=qB[:], scalar1=-1.0, scalar2=None, op0=ALU.mult))
        P2s("pool", lambda e: e.iota(slo[:], pattern=[[1, 8], [0, 16], [8, 8]], base=0, channel_multiplier=0,
                                     allow_small_or_imprecise_dtypes=True))
        for gt in range(2 * NT):
            P2s("pool", lambda e: e.iota(shi[:], pattern=[[0, 8], [64, 16], [0, 8]], base=gt * TT - TOK, channel_multiplier=0,
                                         allow_small_or_imprecise_dtypes=True))
            P2s("pool", lambda e: e.iota(blk[:], pattern=[[0, 8], [1, 4], [0, 32]], base=gt * 4, channel_multiplier=0,
                                         allow_small_or_imprecise_dtypes=True))
            P2s("dve", lambda e: e.tensor_scalar(out=ka[:], in0=blk[:], scalar1=p64[:, 0:1], scalar2=cC[:, 0:1],
                                                 op0=ALU.is_equal, op1=ALU.add))
            P2s("dve", lambda e: e.scalar_tensor_tensor(out=ka[:], in0=shi[:], scalar=cA[:, 0:1], in1=ka[:],
                                                        op0=ALU.mult, op1=ALU.add))
            P2s("dve", lambda e: e.scalar_tensor_tensor(out=ka[:], in0=slo[:], scalar=cB_[:, 0:1], in1=ka[:],
                                                        op0=ALU.mult, op1=ALU.add))
            for i in range(2):
                P2s("dve", lambda e: e.tensor_copy(out=Kaug[i][64:128, gt * TT:(gt + 1) * TT], in_=ka[64:128, :]))
            if gt >= NT:
                ot = gt - NT
                P2s("dve", lambda e: e.tensor_scalar(out=ka[:], in0=shi[:], scalar1=qA[:, 0:1], scalar2=qC[:, 0:1],
                                                     op0=ALU.mult, op1=ALU.add))
                P2s("dve", lambda e: e.scalar_tensor_tensor(out=QAbase[:, ot * TT:(ot + 1) * TT], in0=slo[:], scalar=qB[:, 0:1],
                                                            in1=ka[:], op0=ALU.mult, op1=ALU.add))
        P2s("pool", lambda e: e.memset(cble[:], 0.0))
        P2s("pool", lambda e: e.memset(cblt[:], 0.0))
        for i in range(4):
            sl = slice(32 * i, 32 * i + 32)
            P2s("pool", lambda e: e.affine_select(out=cble[sl, sl], in_=cble[sl, sl], pattern=[[1, 32]], compare_op=ALU.is_ge,
                                                  fill=NEG, base=0, channel_multiplier=-1))
            P2s("pool", lambda e: e.affine_select(out=cblt[sl, sl], in_=cblt[sl, sl], pattern=[[1, 32]], compare_op=ALU.is_ge,
                                                  fill=NEG, base=-1, channel_multiplier=-1))
        for sk in range(8):
            for sq in range(8):
                src = cble if sk <= sq else cblt
                P2s("dve", lambda e: e.tensor_copy(out=CB[:, sk, sq, :], in_=src[:]))
        P2s("pool", lambda e: e.memset(VB[:], 0.0))
        P2s("pool", lambda e: e.memset(OWNM[:], 1.0))
        P2s("dve", lambda e: e.tensor_scalar(out=VB[:, :, 0:16], in0=ones_f[:, 0:64].rearrange("p (a b) -> p a b", a=NT),
                                             scalar1=flag_sb[:, 0:1], scalar2=-1.0, op0=ALU.mult, op1=ALU.add))
        P2s("dve", lambda e: e.tensor_scalar(out=VB[:, :, 0:16], in0=VB[:, :, 0:16], scalar1=1e30, scalar2=None, op0=ALU.mult))
        for ot in range(NT):
            for i in range(4):
                j = 4 * ot + i
                sl = slice(32 * i, 32 * i + 32)
                P2s("pool", lambda e: e.affine_select(out=VB[sl, ot, 16:32], in_=VB[sl, ot, 16:32], pattern=[[-1, 16]],
                                                      compare_op=ALU.is_ge, fill=-1e30, base=j - 1, channel_multiplier=0))
                P2s("pool", lambda e: e.affine_select(out=OWNM[sl, ot, :], in_=OWNM[sl, ot, :], pattern=[[1, 32]],
                                                      compare_op=ALU.not_equal, fill=0.0, base=-(16 + j), channel_multiplier=0))

        with ExitStack() as ps:
            hnl = [sbt(ps, "hnl%d" % i, [128, 8, TT], BF16) for i in range(2)]
            B_hnl = [Buf("hnl0"), Buf("hnl1")]
            qT = sbt(ps, "qT", [128, 4, TT], BF16)
            gT = sbt(ps, "gT", [128, 16, TT], BF16)
            B_qT, B_gT = Buf("qT"), Buf("gT")
            pmm = Ring([pst(ps, "pmb%d" % i, [128, 512], F32) for i in range(6)], "pmb")
            for ot in range(NT):
                hi_ = ot % 2
                S.dma("sp", hnl[hi_][:], scr_hn[ot], B_hnl[hi_], True)
                for cb in range(20):
                    for hf in range(2):
                        pq, B_pq = pmm.next()
                        S.op("pe", [(lambda e, k=k: e.matmul(pq[:, :], lhsT=wqg[:, k, cb * 128:(cb + 1) * 128],
                                                             rhs=hnl[hi_][:, k, hf * 512:(hf + 1) * 512],
                                                             start=(k == 0), stop=(k == 7))) for k in range(8)],
                             r=[B_hnl[hi_], B_wqg], w=[B_pq])
                        if cb < 4:
                            S.op("act", lambda e: e.activation(out=qT[:, cb, hf * 512:(hf + 1) * 512], in_=pq[:, :],
                                                               func=AF.Copy, scale=0.125), r=[B_pq], w=[B_qT])
                        elif cb < 12:
                            S.op("act", lambda e: e.activation(out=gT[:, cb - 4, hf * 512:(hf + 1) * 512], in_=pq[:, :],
                                                               func=AF.Sigmoid), r=[B_pq], w=[B_gT])
                        else:
                            S.op("dve", lambda e: e.tensor_copy(out=gT[:, cb - 4, hf * 512:(hf + 1) * 512], in_=pq[:, :]),
                                 r=[B_pq], w=[B_gT])
                S.dma("pool", scr_q[:, :, ot * TT:(ot + 1) * TT].rearrange("b p n -> p b n"), qT[:], B_qT, False)
                S.dma("pool", scr_g[:, :, ot * TT:(ot + 1) * TT].rearrange("b p n -> p b n"), gT[:], B_gT, False)
            S.barrier()
            S.release([B_qT, B_gT, B_wqg] + B_hnl)
        st12.close()

        if upto < 4:
            S.barrier()
            return nc, dbg

        with ExitStack() as ps:
            Qaug = [sbt(ps, "Qaug%d" % i, [128, TOK], BF16) for i in range(2)]
            Vh = [sbt(ps, "Vh%d" % i, [128, 64, 128], BF16) for i in range(2)]
            B_K = [Buf("K0"), Buf("K1")]
            B_Q = [Buf("Q0"), Buf("Q1")]
            B_Qs = [Buf("Qs0"), Buf("Qs1")]
            B_V = [Buf("V0"), Buf("V1")]
            oT = sbt(ps, "oT", [128, TOK], BF16)
            B_oT = Buf("oT")
            kmh = sbt(ps, "kmh", [128, 32], F32)
            kmb = sbt(ps, "kmb", [128, 32], BF16)
            B_kmh = Buf("kmh")
            gm = sbt(ps, "gm", [128, 8, 32], F32)
            m8 = sbt(ps, "m8", [128, 8, 8], F32)
            thr = sbt(ps, "thr", [128, 8], F32)
            selb = sbt(ps, "selb", [128, 8, 32], BF16)
            B_gm, B_m8, B_thr, B_selb = Buf("gm"), Buf("m8"), Buf("thr"), Buf("selb")
            pTs = [sbt(ps, "pTs%d" % i, [128, 512], BF16) for i in range(5)]
            B_pTs = [Buf("pTs%d" % i) for i in range(5)]
            rrow = sbt(ps, "rrow", [128, 512], F32)
            bcs = sbt(ps, "bcs", [128, 512], F32)
            B_rrow, B_bcs = Buf("rrow"), Buf("bcs")
            pS = Ring([pst(ps, "pS%d" % i, [128, 512], F32) for i in range(4)], "pS")
            pAcc = Ring([pst(ps, "pAcc%d" % i, [128, 512], F32) for i in range(2)], "pAcc")
            pG = Ring([pst(ps, "pG%d" % i, [128, 512], F32) for i in range(1)], "pG")
            pX = Ring([pst(ps, "pX%d" % i, [128, 1024], BF16) for i in range(1)], "pX")

            for i in range(2):
                S.op("dve", lambda e: e.memset(Vh[i][:, :, 64:128], 0.0), w=[B_V[i]])
                S.op("dve", lambda e: e.memset(Vh[i][:, :, 64:65], 1.0), w=[B_V[i]])

            scr_v_r = scr_v.rearrange("t s c f -> c (t s) f")
            LAG = 3
            pend = []
            step = [0]
            seq = [0]

            def sched(due, fn):
                seq[0] += 1
                pend.append((due, seq[0], fn))
                pend.sort(key=lambda t_: (t_[0], t_[1]))

            def flush(upto_step):
                while pend and pend[0][0] <= upto_step:
                    pend.pop(0)[2]()

            def emit_loads(hd):
                cb, h2, hb = hd // 2, hd % 2, hd % 2
                slope = 2.0 ** (-(hd + 1))
                S.dma("sp", Kaug[hb][0:64, :], scr_kt[cb, h2 * 64:(h2 + 1) * 64, :], B_K[hb], True)
                S.dma("sp", Qaug[hb][0:64, :], scr_q[cb, h2 * 64:(h2 + 1) * 64, :], B_Q[hb], True)
                S.dma("sp", Vh[hb][:, :, 0:64], scr_v_r[:, :, hd * 64:(hd + 1) * 64], B_V[hb], True)

            def sel_items(hd):
                cb, h2, hb = hd // 2, hd % 2, hd % 2
                slope = 2.0 ** (-(hd + 1))
                items = []

                def st0():
                    S.op("dve", lambda e: e.tensor_scalar(out=Qaug[hb][96:128, :], in0=QAbase[96:128, :], scalar1=slope, scalar2=None,
                                                           op0=ALU.mult), r=[B_st], w=[B_Qs[hb]])
                    S.op("dve", lambda e: e.tensor_reduce(out=kmh[0:64, :].rearrange("p (t k) -> p t k", t=2 * NT),
                                                          in_=Kaug[hb][0:64, :].rearrange("p (t s k c) -> p t k s c", t=2 * NT, s=8, k=4, c=32),
                                                          axis=AX.XY, op=ALU.add), r=[B_K[hb]], w=[B_kmh])
                    S.op("dve", lambda e: e.tensor_scalar(out=kmb[0:64, :], in0=kmh[0:64, :], scalar1=1.0 / 256.0, scalar2=None, op0=ALU.mult),
                         r=[B_kmh], w=[B_kmh])
                items.append((4, st0))
                for ot in range(NT):
                    base = 30 + 60 * ot
                    holder = {}

                    def stA(ot=ot, holder=holder):
                        pg, B_pg = pG.next()
                        holder["pg"] = (pg, B_pg)
                        S.op("pe", [(lambda e, s_=s_: e.matmul(pg[:, s_ * 32:(s_ + 1) * 32],
                                                               lhsT=Qaug[hb][0:64, ot * TT + s_ * 128:ot * TT + (s_ + 1) * 128],
                                                               rhs=kmb[0:64, :], start=True, stop=True)) for s_ in range(8)],
                             r=[B_Q[hb], B_kmh], w=[B_pg])

                    def stB(ot=ot, holder=holder):
                        pg, B_pg = holder["pg"]
                        S.op("dve", lambda e: e.tensor_tensor(out=gm[:], in0=pg[:, 0:256].rearrange("p (s n) -> p s n", s=8),
                                                              in1=VB[:, ot:ot + 1, :].to_broadcast([128, 8, 32]), op=ALU.add),
                             r=[B_pg, B_st], w=[B_gm])
                        S.op("dve", [(lambda e, s_=s_: e.max(out=m8[:, s_, :], in_=gm[:, s_, :])) for s_ in range(8)], r=[B_gm], w=[B_m8])
                        S.op("dve", lambda e: e.tensor_scalar(out=thr[:], in0=m8[:, :, 2], scalar1=-1e29, scalar2=None, op0=ALU.max),
                             r=[B_m8], w=[B_thr])
                        S.op("dve", lambda e: e.tensor_tensor(out=gm[:], in0=gm[:], in1=thr[:].unsqueeze(2).to_broadcast([128, 8, 32]),
                                                              op=ALU.subtract), r=[B_thr], w=[B_gm])
                        S.op("dve", lambda e: e.tensor_scalar(out=gm[:], in0=gm[:], scalar1=0.0, scalar2=NEG, op0=ALU.is_lt, op1=ALU.mult),
                             r=[B_gm], w=[B_gm])
                        S.op("dve", lambda e: e.tensor_tensor(out=selb[:], in0=gm[:], in1=OWNM[:, ot:ot + 1, :].to_broadcast([128, 8, 32]),
                                                              op=ALU.mult), r=[B_gm, B_st], w=[B_selb])

                    def stC(ot=ot, holder=holder):
                        px, B_px = pX.next()
                        holder["px"] = (px, B_px)
                        S.op("pe", [(lambda e, s_=s_: e.transpose(out=px[64:96, s_ * 128:(s_ + 1) * 128], in_=selb[:, s_, :],
                                                                  identity=ident_b[:])) for s_ in range(8)],
                             r=[B_selb, B_const], w=[B_px])

                    def stD(ot=ot, holder=holder):
                        px, B_px = holder["px"]
                        S.op("dve", lambda e: e.tensor_copy(out=Qaug[hb][64:96, ot * TT:(ot + 1) * TT], in_=px[64:96, :]),
                             r=[B_px], w=[B_Qs[hb]])
                    items += [(base, stA), (base, stB), (base + 24, stC), (base + 32, stD)]
                return items

            cvA = [sbt(ps, "cvA%d" % i, [128, 8, 256], F32) for i in range(3)]
            cvB = [sbt(ps, "cvB%d" % i, [128, 8, 256], BF16) for i in range(3)]
            B_cvA = [Buf("cvA%d" % i) for i in range(3)]
            B_cvB = [Buf("cvB%d" % i) for i in range(3)]
            cv_jobs = []
            kp = lambda w_: w_.rearrange("(k p) n -> p k n", p=128)
            for src, dst, K_, N_ in ((kp(w_glu_a), scr_wga, 4, D), (kp(w_glu_b), scr_wgb, 4, D), (kp(w_attn_out), scr_wao, 4, D),
                                     (kp(w_out), scr_wo, 8, D), (kp(w_ff_gate), scr_wg, 8, DFF), (kp(w_ff_up), scr_wu, 8, DFF),
                                     (kp(w_ff_down), scr_wd, NF, D)):
                for k0 in range(0, K_, 8):
                    kk = min(8, K_ - k0)
                    for c0 in range(0, N_, 256):
                        cv_jobs.append((src, dst, k0, kk, c0, min(256, N_ - c0)))
            for ji, (src, dst, k0, kk, c0, w_) in enumerate(cv_jobs):
                i3 = ji % 3
                t_in = 60 + 36 * ji

                def cv_in(src=src, k0=k0, kk=kk, c0=c0, w_=w_, i3=i3):
                    S.dma("sp", cvA[i3][:, 0:kk, 0:w_], src[:, k0:k0 + kk, c0:c0 + w_], B_cvA[i3], True)

                def cv_cast(kk=kk, w_=w_, i3=i3):
                    S.op("dve", lambda e: e.tensor_copy(out=cvB[i3][:, 0:kk, 0:w_], in_=cvA[i3][:, 0:kk, 0:w_]),
                         r=[B_cvA[i3]], w=[B_cvB[i3]])

                def cv_out(dst=dst, k0=k0, kk=kk, c0=c0, w_=w_, i3=i3):
                    S.dma("pool", dst[:, k0:k0 + kk, c0:c0 + w_], cvB[i3][:, 0:kk, 0:w_], B_cvB[i3], False)
                sched(t_in, cv_in)
                sched(t_in + 30, cv_cast)
                sched(t_in + 34, cv_out)

            emit_loads(0)
            for (_, fn) in sel_items(0):
                fn()
            for hd in range(8):
                cb, h2 = hd // 2, hd % 2
                hb = hd % 2
                if hd + 1 < 8:
                    emit_loads(hd + 1)
                    for (rel, fn) in sel_items(hd + 1):
                        sched(step[0] + rel, fn)
                for ot in range(NT):
                    for qh in range(2):
                        q0 = ot * TT + qh * 512
                        acc, B_acc = pAcc.next()
                        kts = [(gt, sk) for gt in range(NT + ot + 1) for sk in range(8)]
                        for ki, (gt, sk) in enumerate(kts):
                            pS_, B_pS = pS.next()
                            diag = (gt == NT + ot)
                            k0 = gt * TT + sk * 128
                            fns = [lambda e, pS_=pS_, k0=k0, q0=q0, diag=diag: e.matmul(pS_[:, :], lhsT=Kaug[hb][:, k0:k0 + 128],
                                                                                        rhs=Qaug[hb][:, q0:q0 + 512], start=True, stop=not diag)]
                            if diag:
                                fns.append(lambda e, pS_=pS_, sk=sk, qh=qh: e.matmul(pS_[:, :], lhsT=ident_b[:],
                                                                                      rhs=CB[:, sk, 4 * qh:4 * qh + 4, :].rearrange("p a b -> p (a b)"),
                                                                                      start=False, stop=True))
                            S.op("pe", fns, r=[B_K[hb], B_Q[hb], B_Qs[hb], B_st, B_const], w=[B_pS])
                            pi_ = step[0] % 5
                            S.op("act", lambda e, pS_=pS_, pi_=pi_: e.activation(out=pTs[pi_][:], in_=pS_[:, :], func=AF.Exp),
                                 r=[B_pS], w=[B_pTs[pi_]])

                            def pv(acc=acc, B_acc=B_acc, gt=gt, sk=sk, pi_=pi_, first=(ki == 0), last=(ki == len(kts) - 1), hb=hb):
                                S.op("pe", lambda e: e.matmul(acc[:, :], lhsT=Vh[hb][:, gt * 8 + sk, :], rhs=pTs[pi_][:],
                                                              start=first, stop=last), r=[B_V[hb], B_pTs[pi_]], w=[B_acc])
                            sched(step[0] + LAG, pv)
                            flush(step[0])
                            step[0] += 1

                        def tail1(acc=acc, B_acc=B_acc):
                            S.op("dve", lambda e: e.reciprocal(out=rrow[64:65, :], in_=acc[64:65, :]), r=[B_acc], w=[B_rrow])

                        def tail2(acc=acc, B_acc=B_acc, q0=q0):
                            pb, B_pb = pG.next()
                            S.op("pe", lambda e: e.matmul(pb[0:64, :], lhsT=ones_f[64:65, 0:64], rhs=rrow[64:65, :], start=True, stop=True),
                                 r=[B_rrow, B_const], w=[B_pb])
                            S.op("dve", lambda e: e.tensor_copy(out=bcs[0:64, :], in_=pb[0:64, :]), r=[B_pb], w=[B_bcs])
                            S.op("dve", lambda e: e.tensor_tensor(out=oT[0:64, q0:q0 + 512], in0=acc[0:64, :], in1=bcs[0:64, :], op=ALU.mult),
                                 r=[B_acc, B_bcs], w=[B_oT])
                        sched(step[0] + LAG + 1, tail1)
                        sched(step[0] + LAG + 8, tail2)
                flush(step[0] + LAG + 8)
                S.dma("pool", scr_o[cb, h2 * 64:(h2 + 1) * 64, :], oT[0:64, :], B_oT, False)
            flush(10 ** 9)
            S.barrier()
            S.release(B_K + B_Q + B_V + [B_oT] + B_cvA + B_cvB)
        ps12.close()


        if upto < 5:
            S.barrier()
            return nc, dbg

        def post_norm_residual(py2, B_py2, xres, B_xres, s_loc, bc, ssq2, rs2, B_ssq2, B_rs2, junkf, B_junkf):
            for nh in range(2):
                S.op("act", lambda e: e.activation(out=junkf[:], in_=py2[nh][:, :], func=AF.Square, accum_out=ssq2[:, nh:nh + 1]),
                     r=[B_py2[nh]], w=[B_junkf, B_ssq2])
            S.op("dve", lambda e: e.tensor_tensor(out=rs2[:, 0:1], in0=ssq2[:, 0:1], in1=ssq2[:, 1:2], op=ALU.add), r=[B_ssq2], w=[B_rs2])
            S.op("act", lambda e: e.activation(out=rs2[:, 1:2], in_=rs2[:, 0:1], func=AF.Sqrt, scale=1.0 / D, bias=EPS), r=[B_rs2], w=[B_rs2])
            S.op("dve", lambda e: e.reciprocal(out=rs2[:, 2:3], in_=rs2[:, 1:2]), r=[B_rs2], w=[B_rs2])
            for nh in range(2):
                S.op("dve", lambda e: e.scalar_tensor_tensor(out=junkf[:, 0:512] if False else py2_sb[nh][:], in0=py2[nh][:, :], scalar=rs2[:, 2:3],
                                                             in1=bc[:, nh * 512:(nh + 1) * 512], op0=ALU.mult, op1=ALU.mult),
                     r=[B_py2[nh], B_rs2, B_mod], w=[B_py2sb[nh]])
                S.op("dve", lambda e: e.tensor_tensor(out=xres[:, s_loc, nh * 512:(nh + 1) * 512], in0=xres[:, s_loc, nh * 512:(nh + 1) * 512],
                                                       in1=py2_sb[nh][:], op=ALU.add), r=[B_py2sb[nh]], w=[B_xres])

        with ExitStack() as ps:
            Wga = sbt(ps, "Wga", [128, 4, D], BF16)
            Wgb = sbt(ps, "Wgb", [128, 4, D], BF16)
            Wao = sbt(ps, "Wao", [128, 4, D], BF16)
            Wo = sbt(ps, "Wo", [128, 8, D], BF16)
            B_w3 = Buf("w3")
            for dst, src in ((Wga, scr_wga), (Wgb, scr_wgb), (Wao, scr_wao), (Wo, scr_wo)):
                S.dma("sp", dst[:], src, B_w3, True)
            zT3 = sbt(ps, "zT3", [128, 4, TT], BF16)
            oT3 = sbt(ps, "oT3", [128, 4, TT], BF16)
            gT3 = sbt(ps, "gT3", [128, 16, TT], BF16)
            x3 = sbt(ps, "x3", [128, 8, D], F32)
            mT = sbt(ps, "mT", [128, 8, TT], BF16)
            B_zT3, B_oT3, B_gT3, B_x3, B_mT = Buf("zT3"), Buf("oT3"), Buf("gT3"), Buf("x3"), Buf("mT")
            sg = [sbt(ps, "sg%d" % i, [128, 512], F32) for i in range(2)]
            B_sg = [Buf("sg0"), Buf("sg1")]
            ta = [sbt(ps, "ta%d" % i, [128, 512], BF16) for i in range(2)]
            tb_ = [sbt(ps, "tb%d" % i, [128, 512], BF16) for i in range(2)]
            B_ta, B_tb = [Buf("ta0"), Buf("ta1")], [Buf("tb0"), Buf("tb1")]
            gbs = [sbt(ps, "gbs%d" % i, [128, 512], BF16) for i in range(2)]
            B_gbs = [Buf("gbs0"), Buf("gbs1")]
            py2_sb = [sbt(ps, "py2sb%d" % i, [128, 512], F32) for i in range(2)]
            B_py2sb = [Buf("py2sb0"), Buf("py2sb1")]
            junkf = sbt(ps, "junkf", [128, 512], BF16)
            B_junkf = Buf("junkf")
            ssq2 = sbt(ps, "ssq2", [128, 2], F32)
            rs2 = sbt(ps, "rs2", [128, 3], F32)
            B_ssq2, B_rs2 = Buf("ssq2"), Buf("rs2")
            p3 = Ring([pst(ps, "p3_%d" % i, [128, 512], F32) for i in range(8)], "p3")
            xo_v3 = xo.rearrange("(t c s) d -> t c s d", c=128, s=8)
            out_v3 = out.rearrange("(t c s) d -> t c s d", c=128, s=8)
            it = 0
            for ot in range(NT):
                S.dma("sp", zT3[:], scr_z[:, :, ot * TT:(ot + 1) * TT].rearrange("b p n -> p b n"), B_zT3, True)
                S.dma("sp", oT3[:], scr_o[:, :, ot * TT:(ot + 1) * TT].rearrange("b p n -> p b n"), B_oT3, True)
                S.dma("sp", gT3[:], scr_g[:, :, ot * TT:(ot + 1) * TT].rearrange("b p n -> p b n"), B_gT3, True)
                S.dma("sp", x3[:], xo_v3[ot], B_x3, True)
                for ncb in range(8):
                    for hf in range(2):
                        sl = slice(hf * 512, (hf + 1) * 512)
                        i2 = it % 2
                        it += 1
                        pa, B_pa = p3.next()
                        pb, B_pb = p3.next()
                        pc, B_pc = p3.next()
                        S.op("pe", [(lambda e, k=k: e.matmul(pa[:, :], lhsT=Wga[:, k, ncb * 128:(ncb + 1) * 128], rhs=zT3[:, k, sl],
                                                             start=(k == 0), stop=(k == 3))) for k in range(4)], r=[B_w3, B_zT3], w=[B_pa])
                        S.op("pe", [(lambda e, k=k: e.matmul(pb[:, :], lhsT=Wgb[:, k, ncb * 128:(ncb + 1) * 128], rhs=zT3[:, k, sl],
                                                             start=(k == 0), stop=(k == 3))) for k in range(4)], r=[B_w3, B_zT3], w=[B_pb])
                        S.op("pe", [(lambda e, k=k: e.matmul(pc[:, :], lhsT=Wao[:, k, ncb * 128:(ncb + 1) * 128], rhs=oT3[:, k, sl],
                                                             start=(k == 0), stop=(k == 3))) for k in range(4)], r=[B_w3, B_oT3], w=[B_pc])
                        S.op("act", lambda e: e.activation(out=sg[i2][:], in_=pb[:, :], func=AF.Sigmoid), r=[B_pb], w=[B_sg[i2]])
                        S.op("dve", lambda e: e.tensor_tensor(out=sg[i2][:], in0=pa[:, :], in1=sg[i2][:], op=ALU.mult), r=[B_pa, B_sg[i2]], w=[B_sg[i2]])
                        S.op("dve", lambda e: e.tensor_tensor(out=ta[i2][:], in0=sg[i2][:], in1=gT3[:, ncb, sl], op=ALU.mult),
                             r=[B_sg[i2], B_gT3], w=[B_ta[i2]])
                        S.op("act", lambda e: e.activation(out=gbs[i2][:], in_=gT3[:, 8 + ncb, sl], func=AF.Sigmoid), r=[B_gT3], w=[B_gbs[i2]])
                        S.op("dve", lambda e: e.tensor_tensor(out=tb_[i2][:], in0=pc[:, :], in1=gbs[i2][:], op=ALU.mult),
                             r=[B_pc, B_gbs[i2]], w=[B_tb[i2]])
                        S.op("dve", lambda e: e.tensor_tensor(out=mT[:, ncb, sl], in0=ta[i2][:], in1=tb_[i2][:], op=ALU.add),
                             r=[B_ta[i2], B_tb[i2]], w=[B_mT])
                for s_ in range(8):
                    py2, B_py2 = [], []
                    for nh in range(2):
                        p_, B_p = p3.next()
                        S.op("pe", [(lambda e, k=k: e.matmul(p_[:, :], lhsT=mT[:, k, s_ * 128:(s_ + 1) * 128], rhs=Wo[:, k, nh * 512:(nh + 1) * 512],
                                                             start=(k == 0), stop=(k == 7))) for k in range(8)], r=[B_mT, B_w3], w=[B_p])
                        py2.append(p_)
                        B_py2.append(B_p)
                    post_norm_residual(py2, B_py2, x3, B_x3, s_, bc_m, ssq2, rs2, B_ssq2, B_rs2, junkf, B_junkf)
                S.dma("pool", out_v3[ot], x3[:], B_x3, False)
            S.barrier()
            S.release([B_w3, B_zT3, B_oT3, B_gT3, B_x3])

        if upto < 6:
            S.barrier()
            return nc, dbg

        with ExitStack() as ps:
            Wg = sbt(ps, "Wg", [128, 8, DFF], BF16)
            Wu = sbt(ps, "Wu", [128, 8, DFF], BF16)
            Wd = sbt(ps, "Wd", [128, NF, D], BF16)
            B_w4 = Buf("w4")
            for dst, src in ((Wg, scr_wg), (Wu, scr_wu), (Wd, scr_wd)):
                S.dma("sp", dst[:], src, B_w4, True)
            x4 = sbt(ps, "x4", [128, 4, D], F32)
            xn4 = sbt(ps, "xn4", [128, 4, D], BF16)
            hn4 = sbt(ps, "hn4", [128, 8, 512], BF16)
            aT = sbt(ps, "aT", [128, NF, 512], BF16)
            B_x4, B_xn4, B_hn4, B_aT = Buf("x4"), Buf("xn4"), Buf("hn4"), Buf("aT")
            sl4 = [sbt(ps, "sl4_%d" % i, [128, 512], BF16) for i in range(2)]
            B_sl4 = [Buf("sl4_0"), Buf("sl4_1")]
            py2_sb = [sbt(ps, "py4sb%d" % i, [128, 512], F32) for i in range(2)]
            B_py2sb = [Buf("py4sb0"), Buf("py4sb1")]
            junk4 = sbt(ps, "junk4", [128, D], BF16)
            junkf = sbt(ps, "junkf4", [128, 512], BF16)
            B_junk4, B_junkf = Buf("junk4"), Buf("junkf4")
            ssq4 = sbt(ps, "ssq4", [128, 4], F32)
            rstd4 = sbt(ps, "rstd4", [128, 4], F32)
            ssq2 = sbt(ps, "ssq2b", [128, 2], F32)
            rs2 = sbt(ps, "rs2b", [128, 3], F32)
            B_ssq4, B_rstd4, B_ssq2, B_rs2 = Buf("ssq4"), Buf("rstd4"), Buf("ssq2b"), Buf("rs2b")
            ptr4 = Ring([pst(ps, "ptr4_%d" % i, [128, 512], BF16) for i in range(2)], "ptr4")
            p4 = Ring([pst(ps, "p4_%d" % i, [128, 512], F32) for i in range(6)], "p4")
            out_v4 = out.rearrange("(t c s) d -> t c s d", c=128, s=8)
            it = 0
            for ot in range(NT):
                for sh in range(2):
                    S.dma("sp", x4[:], out_v4[ot][:, 4 * sh:4 * sh + 4, :], B_x4, True)
                    for s_ in range(4):
                        S.op("act", lambda e: e.activation(out=junk4[:], in_=x4[:, s_, :], func=AF.Square, accum_out=ssq4[:, s_:s_ + 1]),
                             r=[B_x4], w=[B_junk4, B_ssq4])
                    S.op("act", lambda e: e.activation(out=rstd4[:], in_=ssq4[:], func=AF.Sqrt, scale=1.0 / D, bias=EPS), r=[B_ssq4], w=[B_rstd4])
                    S.op("dve", lambda e: e.reciprocal(out=rstd4[:], in_=rstd4[:]), r=[B_rstd4], w=[B_rstd4])
                    for s_ in range(4):
                        S.op("dve",
                             lambda e: e.tensor_scalar(out=xn4[:, s_, :], in0=x4[:, s_, :], scalar1=rstd4[:, s_:s_ + 1], scalar2=None, op0=ALU.mult),
                             r=[B_x4, B_rstd4], w=[B_xn4])
                    for k in range(8):
                        pt, B_pt = ptr4.next()
                        S.op("pe", [(lambda e, s_=s_: e.transpose(out=pt[:, s_ * 128:(s_ + 1) * 128], in_=xn4[:, s_, k * 128:(k + 1) * 128],
                                                                  identity=ident_b[:])) for s_ in range(4)], r=[B_xn4, B_const], w=[B_pt])
                        if k % 2 == 0:
                            S.op("dve", lambda e: e.tensor_scalar(out=hn4[:, k, :], in0=pt[:, :], scalar1=gmod_f[:, k:k + 1], scalar2=sh_f[:, k:k + 1],
                                                                  op0=ALU.mult, op1=ALU.add), r=[B_pt, B_mod], w=[B_hn4])
                        else:
                            S.op("act", lambda e: e.activation(out=hn4[:, k, :], in_=pt[:, :], func=AF.Identity, scale=gmod_f[:, k:k + 1],
                                                               bias=sh_f[:, k:k + 1]), r=[B_pt, B_mod], w=[B_hn4])
                    for f in range(NF):
                        i2 = it % 2
                        it += 1
                        pg_, B_pg = p4.next()
                        pu_, B_pu = p4.next()
                        S.op("pe", [(lambda e, k=k: e.matmul(pg_[:, :], lhsT=Wg[:, k, f * 128:(f + 1) * 128], rhs=hn4[:, k, :],
                                                             start=(k == 0), stop=(k == 7))) for k in range(8)], r=[B_w4, B_hn4], w=[B_pg])
                        S.op("pe", [(lambda e, k=k: e.matmul(pu_[:, :], lhsT=Wu[:, k, f * 128:(f + 1) * 128], rhs=hn4[:, k, :],
                                                             start=(k == 0), stop=(k == 7))) for k in range(8)], r=[B_w4, B_hn4], w=[B_pu])
                        S.op("act", lambda e: e.activation(out=sl4[i2][:], in_=pg_[:, :], func=AF.Silu), r=[B_pg], w=[B_sl4[i2]])
                        S.op("dve", lambda e: e.tensor_tensor(out=aT[:, f, :], in0=pu_[:, :], in1=sl4[i2][:], op=ALU.mult),
                             r=[B_pu, B_sl4[i2]], w=[B_aT])
                    for s_ in range(4):
                        py2, B_py2 = [], []
                        for nh in range(2):
                            p_, B_p = p4.next()
                            S.op("pe", [(lambda e, f=f: e.matmul(p_[:, :], lhsT=aT[:, f, s_ * 128:(s_ + 1) * 128], rhs=Wd[:, f, nh * 512:(nh + 1) * 512],
                                                                 start=(f == 0), stop=(f == NF - 1))) for f in range(NF)], r=[B_aT, B_w4], w=[B_p])
                            py2.append(p_)
                            B_py2.append(B_p)
                        post_norm_residual(py2, B_py2, x4, B_x4, s_, bc_f, ssq2, rs2, B_ssq2, B_rs2, junkf, B_junkf)
                    S.dma("pool", out_v4[ot][:, 4 * sh:4 * sh + 4, :], x4[:], B_x4, False)
            S.barrier()

        S.barrier()
    return nc, dbg


def _prep_inputs(inputs):
    f = lambda a: np.ascontiguousarray(np.asarray(a, dtype=np.float32))
    x = f(inputs["x"])
    c = f(inputs["c"])
    shared = {}
    shared["w_ada"] = f(inputs["w_ada"][0])
    shared["b_ada"] = f(inputs["b_ada"][0]).reshape(1, -1)
    shared["g_pre_mix_c"] = f(inputs["g_pre_mix"][0].reshape(8, 128).T)
    shared["g_pre_ffn_c"] = f(inputs["g_pre_ffn"][0].reshape(8, 128).T)
    shared["g_post_mix_r"] = f(inputs["g_post_mix"][0]).reshape(1, -1)
    shared["g_post_ffn_r"] = f(inputs["g_post_ffn"][0]).reshape(1, -1)
    shared["w_in"] = f(inputs["w_in"][0])
    st2 = lambda a: f(np.concatenate([a, a], axis=0))
    shared["s_are"] = st2(np.asarray(inputs["ssm_a_re"][0]).T)
    shared["s_aim"] = st2(np.asarray(inputs["ssm_a_im"][0]).T)
    shared["s_ldt"] = f(np.broadcast_to(np.asarray(inputs["ssm_log_dt"][0])[None, :], (128, 32)))
    shared["s_bre"] = st2(np.asarray(inputs["ssm_b_re"][0]).transpose(1, 0, 2))
    shared["s_bim"] = st2(np.asarray(inputs["ssm_b_im"][0]).transpose(1, 0, 2))
    shared["s_cre"] = st2(np.asarray(inputs["ssm_c_re"][0]).transpose(2, 0, 1))
    shared["s_cim"] = st2(np.asarray(inputs["ssm_c_im"][0]).transpose(2, 0, 1))
    shared["s_dcol"] = f(np.tile(np.asarray(inputs["ssm_d"][0]).T, (8, 1)))
    for k in ("w_glu_a", "w_glu_b", "w_attn_out", "w_out", "w_ff_gate", "w_ff_up", "w_ff_down"):
        shared[k] = f(inputs[k][0])
    in_maps = []
    for core in range(8):
        b, h = core // 2, core % 2
        m = dict(shared)
        m["xo"] = f(x[b, h * TOK:(h + 1) * TOK])
        m["xp"] = f(x[b, 0:TOK])
        m["flag"] = np.full((128, 1), float(h), np.float32)
        m["c_col"] = f(c[b].reshape(8, 128).T)
        in_maps.append(m)
    return in_maps


_CACHE = {}


def kernel(**inputs):
    in_maps = _prep_inputs(inputs)
    if "nc" not in _CACHE:
        _CACHE["nc"] = build_program()[0]
    nc = _CACHE["nc"]
    res = run_bass_kernel_spmd(nc, in_maps, core_ids=list(range(8)))
    outp = np.empty((4, 8192, D), np.float32)
    for core in range(8):
        b, h = core // 2, core % 2
        outp[b, h * TOK:(h + 1) * TOK] = res.results[core]["out"]
    return outp
```
